# Optimizing a Trainium2 kernel written in Bass

```python
import math
import jax, jax.numpy as jnp
from jax import lax
import numpy as np

D_MODEL = 1024
BATCH = 2
SEQ = 8192
DEPTH = 1

CHUNK = 64
M_HEADS = 4
M_HEAD_DIM = D_MODEL // M_HEADS
M_WIDTH = M_HEADS * M_HEAD_DIM
CONV_WIDTH = 4
A_HEADS = 8
A_HEAD_DIM = D_MODEL // A_HEADS
A_WIDTH = A_HEADS * A_HEAD_DIM
IDX_HEADS = 8
IDX_DIM = 64
TOPK_MAX = 256
Q_BLOCK = 128
ROPE_THETA = 10000.0
N_EXPERTS = 32
TOP_K = 4
D_FF = D_MODEL
SWIGLU_LIMIT = 7.0
SWIGLU_ALPHA = 1.702
LN_EPS = 1e-5
DEEPNORM_ALPHA = (2.0 * DEPTH) ** 0.25
DEEPNORM_BETA = (8.0 * DEPTH) ** -0.25

COLUMN_SIZES = (M_WIDTH, M_WIDTH, M_WIDTH, M_WIDTH, M_HEADS, M_HEADS,
                A_WIDTH, A_WIDTH, A_WIDTH, IDX_HEADS * IDX_DIM, IDX_DIM, IDX_HEADS,
                D_MODEL, D_MODEL)
IN_WIDTH = 4 * M_WIDTH + 2 * M_HEADS + 3 * A_WIDTH + IDX_HEADS * IDX_DIM + IDX_DIM + IDX_HEADS + 2 * D_MODEL
F_GATE_OFFSET = 4 * M_WIDTH + M_HEADS

kernel_name = 'hybrid_mlstm_dsa_moe_block'


def _split_columns(p):
    out, start = [], 0
    for s in COLUMN_SIZES:
        out.append(p[..., start:start + s])
        start += s
    return out


def _layer_norm(x, g, b):
    xf = x.astype(jnp.float32)
    mu = xf.mean(-1, keepdims=True)
    var = jnp.square(xf - mu).mean(-1, keepdims=True)
    return ((xf - mu) * lax.rsqrt(var + LN_EPS) * g + b).astype(x.dtype)


def _head_norm(h, g):
    mu = h.mean(-1, keepdims=True)
    var = jnp.square(h - mu).mean(-1, keepdims=True)
    return (h - mu) * lax.rsqrt(var + LN_EPS) * g


def _rope(x, pos):
    half = x.shape[-1] // 2
    inv = ROPE_THETA ** (-jnp.arange(half, dtype=jnp.float32) / half)
    ang = pos.astype(jnp.float32)[:, None] * inv[None, :]
    cos = jnp.cos(ang)[None, :, None, :]
    sin = jnp.sin(ang)[None, :, None, :]
    x1 = x[..., :half].astype(jnp.float32)
    x2 = x[..., half:].astype(jnp.float32)
    return jnp.concatenate([x1 * cos - x2 * sin, x2 * cos + x1 * sin], -1).astype(x.dtype)


def _causal_conv(x, w, b):
    K, T = w.shape[0], x.shape[1]
    xp = jnp.pad(x, ((0, 0), (K - 1, 0), (0, 0)))
    y = b
    for j in range(K):
        y = y + xp[:, j:j + T] * w[j]
    return y


def _mlstm(q, k, v, i_pre, f_pre):
    B, T, H, d = q.shape
    nc = T // CHUNK

    def to_chunks(a):
        a = a.reshape((B, nc, CHUNK) + a.shape[2:])
        return jnp.moveaxis(a, (1, 3), (0, 2))

    logf = jax.nn.log_sigmoid(f_pre)
    xs = tuple(to_chunks(a) for a in (q, k, v, i_pre, logf))
    causal = jnp.tril(jnp.ones((CHUNK, CHUNK), dtype=bool))

    def step(carry, xs_c):
        C, n, m = carry
        qb, kb, vb, ib, fb = xs_c
        b = jnp.cumsum(fb, axis=-1)
        Dm = b[..., :, None] - b[..., None, :] + ib[..., None, :]
        Dm = jnp.where(causal, Dm, -jnp.inf)
        m_inter = b + m[..., None]
        m_t = jnp.maximum(m_inter, Dm.max(-1))
        S = jnp.einsum('bhtd,bhsd->bhts', qb, kb) * jnp.exp(Dm - m_t[..., None])
        w_inter = jnp.exp(m_inter - m_t)
        num = jnp.einsum('bhts,bhsd->bhtd', S, vb) + w_inter[..., None] * jnp.einsum('bhtk,bhkv->bhtv', qb, C)
        den = S.sum(-1) + w_inter * jnp.einsum('bhtk,bhk->bht', qb, n)
        h = num / jnp.maximum(jnp.abs(den), jnp.exp(-m_t))[..., None]
        bL = b[..., -1]
        g = bL[..., None] - b + ib
        m_new = jnp.maximum(bL + m, g.max(-1))
        wg = jnp.exp(g - m_new[..., None])
        decay = jnp.exp(bL + m - m_new)
        C_new = decay[..., None, None] * C + jnp.einsum('bhs,bhsk,bhsv->bhkv', wg, kb, vb)
        n_new = decay[..., None] * n + jnp.einsum('bhs,bhsk->bhk', wg, kb)
        return (C_new, n_new, m_new), h

    init = (jnp.zeros((B, H, d, d), jnp.float32), jnp.zeros((B, H, d), jnp.float32),
            jnp.zeros((B, H), jnp.float32))
    _, hs = lax.scan(step, init, xs)
    return hs.transpose(1, 0, 3, 2, 4).reshape(B, T, H, d)


def _dsa(q, k, v, q_idx, k_idx, w_idx):
    B, T, H, dh = q.shape
    top_k = min(TOPK_MAX, T // 4)
    key_chunk = jnp.arange(T) // CHUNK
    bidx = jnp.arange(B)[:, None, None]
    k_idx_f = k_idx.astype(jnp.float32)

    def block(start):
        qb = lax.dynamic_slice_in_dim(q, start, Q_BLOCK, axis=1)
        qib = lax.dynamic_slice_in_dim(q_idx, start, Q_BLOCK, axis=1).astype(jnp.float32)
        wib = lax.dynamic_slice_in_dim(w_idx, start, Q_BLOCK, axis=1).astype(jnp.float32)
        q_chunk = (start + jnp.arange(Q_BLOCK)) // CHUNK
        s_idx = jnp.einsum('bqhd,bsd->bqhs', qib, k_idx_f) * IDX_DIM ** -0.5
        scores = jnp.einsum('bqh,bqhs->bqs', wib * IDX_HEADS ** -0.5, jax.nn.relu(s_idx))
        admissible = key_chunk[None, :] <= q_chunk[:, None]
        scores = jnp.where(admissible[None], scores, -jnp.inf)
        _, sel = lax.top_k(scores, top_k)
        valid = (sel // CHUNK) <= q_chunk[None, :, None]
        k_sel = k[bidx, sel]
        v_sel = v[bidx, sel]
        logits = jnp.einsum('bqhd,bqkhd->bqhk', qb, k_sel).astype(jnp.float32) * dh ** -0.5
        logits = jnp.where(valid[:, :, None, :], logits, -jnp.inf)
        p = jax.nn.softmax(logits, axis=-1)
        return jnp.einsum('bqhk,bqkhd->bqhd', p.astype(v.dtype), v_sel)

    starts = jnp.arange(T // Q_BLOCK) * Q_BLOCK
    out = lax.map(block, starts)
    return out.transpose(1, 0, 2, 3, 4).reshape(B, T, H, dh)


def _moe(h, w_router, b_router, w_gate, b_gate, w_up, b_up, w_down, b_down):
    B, T, D = h.shape
    xt = h.reshape(B * T, D)
    logits = (xt @ w_router + b_router).astype(jnp.float32)
    top_vals, top_idx = lax.top_k(logits, TOP_K)
    probs = jax.nn.softmax(top_vals, axis=-1)
    gates = jnp.sum(jax.nn.one_hot(top_idx, N_EXPERTS, dtype=jnp.float32) * probs[..., None], axis=1)
    y = jnp.zeros((B * T, D), jnp.float32)
    for e in range(N_EXPERTS):
        g = jnp.minimum(xt @ w_gate[e] + b_gate[e], SWIGLU_LIMIT)
        u = jnp.clip(xt @ w_up[e] + b_up[e], -SWIGLU_LIMIT, SWIGLU_LIMIT)
        act = (u + 1.0) * g * jax.nn.sigmoid(SWIGLU_ALPHA * g)
        y = y + gates[:, e:e + 1] * (act @ w_down[e] + b_down[e])
    return y.astype(h.dtype).reshape(B, T, D)


def setup_inputs(seed: int = 0) -> dict:
    key = jax.random.key(seed)
    ks = jax.random.split(key, 24)
    f32 = jnp.float32
    nrm = lambda k, s: jax.random.normal(k, s, f32)
    b_in = 0.02 * nrm(ks[4], (DEPTH, IN_WIDTH))
    b_in = b_in.at[:, F_GATE_OFFSET:F_GATE_OFFSET + M_HEADS].add(jnp.linspace(3.0, 6.0, M_HEADS))
    return {
        'x': nrm(ks[0], (BATCH, SEQ, D_MODEL)),
        'ln_in_g': 1.0 + 0.02 * nrm(ks[1], (D_MODEL,)),
        'ln_in_b': 0.02 * nrm(ks[2], (D_MODEL,)),
        'w_in': nrm(ks[3], (DEPTH, D_MODEL, IN_WIDTH)) * D_MODEL ** -0.5,
        'b_in': b_in,
        'conv_w': nrm(ks[5], (DEPTH, CONV_WIDTH, 2 * M_WIDTH)) * CONV_WIDTH ** -0.5,
        'conv_b': 0.02 * nrm(ks[6], (DEPTH, 2 * M_WIDTH)),
        'm_norm_g': 1.0 + 0.02 * nrm(ks[7], (DEPTH, M_WIDTH)),
        'w_out': nrm(ks[8], (DEPTH, D_MODEL, D_MODEL)) * D_MODEL ** -0.5 * DEEPNORM_BETA,
        'ln1_g': 1.0 + 0.02 * nrm(ks[9], (DEPTH, D_MODEL)),
        'ln1_b': 0.02 * nrm(ks[10], (DEPTH, D_MODEL)),
        'w_router': nrm(ks[11], (DEPTH, D_MODEL, N_EXPERTS)) * D_MODEL ** -0.5,
        'b_router': 0.01 * nrm(ks[12], (DEPTH, N_EXPERTS)),
        'w_gate': nrm(ks[13], (DEPTH, N_EXPERTS, D_MODEL, D_FF)) * D_MODEL ** -0.5,
        'b_gate': 0.01 * nrm(ks[14], (DEPTH, N_EXPERTS, D_FF)),
        'w_up': nrm(ks[15], (DEPTH, N_EXPERTS, D_MODEL, D_FF)) * D_MODEL ** -0.5,
        'b_up': 0.01 * nrm(ks[16], (DEPTH, N_EXPERTS, D_FF)),
        'w_down': nrm(ks[17], (DEPTH, N_EXPERTS, D_FF, D_MODEL)) * D_FF ** -0.5 * DEEPNORM_BETA,
        'b_down': 0.01 * nrm(ks[18], (DEPTH, N_EXPERTS, D_MODEL)),
        'ln2_g': 1.0 + 0.02 * nrm(ks[19], (DEPTH, D_MODEL)),
        'ln2_b': 0.02 * nrm(ks[20], (DEPTH, D_MODEL)),
    }


def reference(x, ln_in_g, ln_in_b, w_in, b_in, conv_w, conv_b, m_norm_g, w_out,
              ln1_g, ln1_b, w_router, b_router, w_gate, b_gate, w_up, b_up,
              w_down, b_down, ln2_g, ln2_b):
    B, T, _ = x.shape
    f32 = jnp.float32
    pos = jnp.arange(T, dtype=jnp.int32)
    h = _layer_norm(x, ln_in_g, ln_in_b)
    for l in range(DEPTH):
        proj = h @ w_in[l] + b_in[l]
        (m_q, m_k, m_v, m_o, m_i, m_f, a_q, a_k, a_v, x_q, x_k, x_w, g_m, g_a) = _split_columns(proj)
        qk = jax.nn.silu(_causal_conv(jnp.concatenate([m_q, m_k], -1), conv_w[l], conv_b[l]))
        mq = qk[..., :M_WIDTH].reshape(B, T, M_HEADS, M_HEAD_DIM) * M_HEAD_DIM ** -0.5
        mk = qk[..., M_WIDTH:].reshape(B, T, M_HEADS, M_HEAD_DIM)
        mv = m_v.reshape(B, T, M_HEADS, M_HEAD_DIM)
        hm = _mlstm(mq.astype(f32), mk.astype(f32), mv.astype(f32), m_i.astype(f32), m_f.astype(f32))
        hm = _head_norm(hm, m_norm_g[l].reshape(M_HEADS, M_HEAD_DIM))
        y_m = jax.nn.sigmoid(m_o) * hm.reshape(B, T, M_WIDTH).astype(h.dtype)
        aq = _rope(a_q.reshape(B, T, A_HEADS, A_HEAD_DIM), pos)
        ak = _rope(a_k.reshape(B, T, A_HEADS, A_HEAD_DIM), pos)
        av = a_v.reshape(B, T, A_HEADS, A_HEAD_DIM)
        iq = _rope(x_q.reshape(B, T, IDX_HEADS, IDX_DIM), pos)
        ik = _rope(x_k[:, :, None, :], pos)[:, :, 0, :]
        y_a = _dsa(aq, ak, av, iq, ik, x_w).reshape(B, T, A_WIDTH)
        merged = jax.nn.sigmoid(g_m) * y_m + jax.nn.sigmoid(g_a) * y_a
        h = _layer_norm(DEEPNORM_ALPHA * h + merged @ w_out[l], ln1_g[l], ln1_b[l])
        moe_out = _moe(h, w_router[l], b_router[l], w_gate[l], b_gate[l], w_up[l], b_up[l], w_down[l], b_down[l])
        h = _layer_norm(DEEPNORM_ALPHA * h + moe_out, ln2_g[l], ln2_b[l])
    return h
```

```python
import numpy as np
from contextlib import ExitStack
import concourse.bass as bass
import concourse.mybir as mybir
from concourse.bass_utils import run_bass_kernel_spmd

F32 = mybir.dt.float32
F32R = mybir.dt.float32r
BF16 = mybir.dt.bfloat16
I32 = mybir.dt.int32
U32 = mybir.dt.uint32
AF = mybir.ActivationFunctionType
ALU = mybir.AluOpType
AX = mybir.AxisListType

D = 1024
INW = 9808
NEXP = 32
CAP = 384
NBIS = 22
NDMASEM = 40
LN_EPS = 1e-5
ALPHA = 2.0 ** 0.25

O_MQ, O_MK, O_MV, O_MO, O_MI, O_MF = 0, 1024, 2048, 3072, 4096, 4100
O_AQ, O_AK, O_AV, O_XQ, O_XK, O_XW, O_GM, O_GA = 4104, 5128, 6152, 7176, 7688, 7752, 7760, 8784


class Buf:
    __slots__ = ("w", "r")

    def __init__(self):
        self.w = {}
        self.r = {}


class Tl:
    def __init__(self, t):
        self.t = t
        self.b = Buf()

    def __getitem__(self, k):
        return self.t[k]


class KB:
    def __init__(self, nc, es):
        self.nc = nc
        self.engs = {"pe": nc.tensor, "act": nc.scalar, "dve": nc.vector, "pool": nc.gpsimd, "sp": nc.sync}
        self.sem = {k: es.enter_context(nc.semaphore("sem_" + k)) for k in self.engs}
        self.cnt = {k: 0 for k in self.engs}
        self.seen = {}
        self.dsem = [es.enter_context(nc.semaphore("dq%d" % i)) for i in range(NDMASEM)]
        self.dval = [0] * NDMASEM
        self.qpool = {"sp": list(range(0, 20)), "pool": list(range(20, 36)), "act": list(range(36, 40))}
        self.qrr = {"sp": 0, "pool": 0, "act": 0}
        self.uid = 0

    def sb(self, es, shape, dt, name=None):
        self.uid += 1
        return Tl(es.enter_context(self.nc.sbuf_tensor("%s_%d" % (name or "t", self.uid), list(shape), dt)))

    def ps(self, es, shape, dt=F32, name=None):
        self.uid += 1
        return Tl(es.enter_context(self.nc.psum_tensor("%s_%d" % (name or "p", self.uid), list(shape), dt)))

    def _wait(self, e, tok):
        sem, val, key = tok
        if self.seen.get((e, key), 0) >= val:
            return
        self.seen[(e, key)] = val
        self.engs[e].wait_ge(sem, val)

    def _deps(self, e, reads, writes):
        toks = []
        for b in reads:
            toks.extend(b.b.w.values())
        for b in writes:
            toks.extend(b.b.w.values())
            toks.extend(b.b.r.values())
        for t in toks:
            if t[2] == "pe" and e == "pe":
                continue
            self._wait(e, t)

    def _mark(self, tok, reads, writes):
        for b in reads:
            b.b.r[tok[2]] = tok
        for b in writes:
            b.b.w[tok[2]] = tok
            b.b.r = {}

    def op(self, e, fn, reads=(), writes=()):
        self._deps(e, reads, writes)
        ins = fn(self.engs[e])
        self.cnt[e] += 1
        ins.then_inc(self.sem[e], 1)
        self._mark((self.sem[e], self.cnt[e], e), reads, writes)

    def dma(self, q, out, in_, reads=(), writes=(), indirect=None):
        pl = self.qpool[q]
        i = pl[self.qrr[q] % len(pl)]
        self.qrr[q] += 1
        key = "d%d" % i
        if self.dval[i] > 0:
            self._wait(q, (self.dsem[i], self.dval[i], key))
        self._deps(q, reads, writes)
        if indirect is None:
            ins = self.engs[q].dma_start(out=out, in_=in_)
        else:
            ins = indirect(self.engs[q])
        self.dval[i] += 16
        ins.then_inc(self.dsem[i], 16)
        self._mark((self.dsem[i], self.dval[i], key), reads, writes)

    def barrier_all(self, bufs):
        for e in self.engs:
            self._deps(e, bufs, ())


def _r(ap):
    return ap.bitcast(F32R)


def _f(ap):
    return ap.bitcast(F32)


def build(T, debug=None, upto="all"):
    NT = T // 128
    NG = T // 512
    NO = T // 4
    NOT = NO // 128
    NCH = T // 64
    nc = bass.Bass("TRN2", target_bir_lowering=False)

    used_inputs = []

    def din(name, shape, dt=F32):
        used_inputs.append(name)
        return nc.dram_tensor(name, list(shape), dt, kind="ExternalInput").ap()

    def dscr(name, shape, dt=F32):
        return nc.dram_tensor(name, list(shape), dt, kind="Internal").ap()

    x_all = din("x_all", [T, D])
    x_own = din("x_own", [NO, D])
    w_in = din("w_in", [D, INW], F32R)
    b_in = din("b_in", [1, INW])
    cols = din("cols", [128, 128])
    conv_wc = din("conv_wc", [128, 16, 4])
    rope_a = din("rope_a", [T, 2, 64])
    rope_i = din("rope_i", [T, 2, 32])
    rope_ao = din("rope_ao", [NO, 2, 64])
    rope_io = din("rope_io", [NO, 2, 32])
    sel = din("sel", [128, 32], F32R)
    trisel = din("trisel", [8, 64, 128])
    tri64 = din("tri64", [64, 64])
    cmask = din("cmask", [64, 16])
    ident = din("ident", [128, 128])
    pw2 = din("pw2", [128, NBIS])
    admb = din("admb", [128, 512])
    m_norm_g = din("m_norm_g", [1, D])
    w_out = din("w_out", [D, D], F32R)
    vecs = din("vecs", [6, D])
    w_router = din("w_router", [D, NEXP])
    b_router = din("b_router", [1, NEXP])
    if upto == "all":
        w_gate = din("w_gate", [NEXP, D, D], F32R)
        w_up = din("w_up", [NEXP, D, D], F32R)
        w_down = din("w_down", [NEXP, D, D], F32R)
    bgu_col = din("bgu_col", [128, NEXP, 16])
    b_down = din("b_down", [NEXP, D])
    eoff = din("eoff", [128, NEXP])
    ustri = din("ustri", [128, 128])
    out = nc.dram_tensor("out", [NO, D], F32, kind="ExternalOutput").ap()

    xnT_d = dscr("xnT_d", [8, 128, T])
    xnTo_d = dscr("xnTo_d", [8, 128, NO])
    kT_d = dscr("kT_d", [8, 128, T])
    ktok_d = dscr("ktok_d", [T, D])
    qTo_d = dscr("qTo_d", [8, 128, NO])
    v_d = dscr("v_d", [T, D])
    ikT_d = dscr("ikT_d", [64, T])
    g_d = dscr("g_d", [T, 8])
    mg_d = dscr("mg_d", [NO, D])
    akT_d = dscr("akT_d", [8, 128, T], BF16)
    av_d = dscr("av_d", [8, 128, T // 128, 130], BF16)
    aqT_d = dscr("aqT_d", [NOT, 128, 8, 128], BF16)
    iqT_d = dscr("iqT_d", [NOT, 64, 8, 128])
    og_d = dscr("og_d", [NO, D])
    ga_d = dscr("ga_d", [NO, D])
    ym_d = dscr("ym_d", [NO, D])
    h1_d = dscr("h1_d", [NO, D])
    xs_d = dscr("xs_d", [NEXP * CAP, D])
    ys_d = dscr("ys_d", [NEXP * CAP, D])
    dbg = {}
    if debug:
        for nm, shp in debug.items():
            dbg[nm] = nc.dram_tensor("dbg_" + nm, list(shp), F32, kind="ExternalOutput").ap()

    with ExitStack() as es0:
        kb = KB(nc, es0)
        V = nc.vector
        c_cols = kb.sb(es0, [128, 128], F32, "cols")
        c_ident = kb.sb(es0, [128, 128], F32, "ident")
        c_identb = kb.sb(es0, [128, 128], BF16, "identb")
        kb.dma("sp", c_cols[:], cols, writes=[c_cols])
        kb.dma("sp", c_ident[:], ident, writes=[c_ident])
        kb.op("dve", lambda e: e.tensor_copy(out=c_identb[:], in_=c_ident[:]), [c_ident], [c_identb])
        GCOL, BCOL, BQK, CBC = 0, 8, 16, 32

        def layernorm_stats(es, xt, tmp_pool):
            st, mv = tmp_pool
            kb.op("dve", lambda e: e.bn_stats(out=st[:, 0, :], in_=xt[:, 0:512]), [xt], [st])
            kb.op("dve", lambda e: e.bn_stats(out=st[:, 1, :], in_=xt[:, 512:1024]), [xt], [st])
            kb.op("dve", lambda e: e.bn_aggr(out=mv[:, 0:2], in_=st[:].rearrange("p a b -> p (a b)")), [st], [mv])
            kb.op("dve", lambda e: e.tensor_scalar(out=mv[:, 2:3], in0=mv[:, 1:2], scalar1=LN_EPS, scalar2=None,
                                                  op0=ALU.add), [mv], [mv])
            kb.op("act", lambda e: e.activation(out=mv[:, 2:3], in_=mv[:, 2:3], func=AF.Sqrt), [mv], [mv])
            kb.op("dve", lambda e: e.reciprocal(out=mv[:, 3:4], in_=mv[:, 2:3]), [mv], [mv])
            return mv[:, 0:1], mv[:, 3:4]

        def phase_ln_T(xsrc, ntiles, dst):
            with ExitStack() as es:
                xts = [kb.sb(es, [128, D], F32, "xt") for _ in range(3)]
                xhs = [kb.sb(es, [128, D], F32, "xh") for _ in range(2)]
                sts = [(kb.sb(es, [128, 2, 6], F32, "st"), kb.sb(es, [128, 4], F32, "mv")) for _ in range(2)]
                xns = [kb.sb(es, [128, 8, 512], F32, "xn") for _ in range(2)]
                pts = [kb.ps(es, [128, 512], F32, "pt") for _ in range(4)]
                for tt in range(ntiles):
                    xt, xh, stp, xn = xts[tt % 3], xhs[tt % 2], sts[tt % 2], xns[(tt // 4) % 2]
                    t4 = tt % 4
                    kb.dma("pool", xt[:], xsrc[tt * 128:(tt + 1) * 128, :], writes=[xt])
                    mean, rstd = layernorm_stats(es, xt, stp)
                    kb.op("dve", lambda e: e.tensor_scalar(out=xh[:], in0=xt[:], scalar1=mean, scalar2=rstd,
                                                          op0=ALU.subtract, op1=ALU.mult), [xt, stp[1]], [xh])
                    for half in range(2):
                        pt = pts[(tt % 2) * 2 + half]
                        for k in range(4):
                            dc = half * 4 + k
                            kb.op("pe", lambda e: e.transpose(out=pt[:, k * 128:(k + 1) * 128],
                                                              in_=xh[:, dc * 128:(dc + 1) * 128], identity=c_ident[:]),
                                  [xh, c_ident], [pt])
                        for k in range(4):
                            dc = half * 4 + k
                            kb.op("act", lambda e: e.activation(out=xn[:, dc, t4 * 128:(t4 + 1) * 128], in_=pt[:, k * 128:(k + 1) * 128],
                                                                func=AF.Identity, scale=c_cols[:, GCOL + dc:GCOL + dc + 1],
                                                                bias=c_cols[:, BCOL + dc:BCOL + dc + 1]),
                                  [pt, c_cols], [xn])
                    if t4 == 3:
                        g4 = tt // 4
                        kb.dma("sp", dst[:, :, g4 * 512:(g4 + 1) * 512].rearrange("c p t -> p c t"), xn[:], reads=[xn])

        def fence():
            for e in kb.engs:
                for i in range(NDMASEM):
                    if kb.dval[i] > 0:
                        kb._wait(e, (kb.dsem[i], kb.dval[i], "d%d" % i))

        def barrier():
            fence()
            for e in kb.engs:
                for e2 in kb.engs:
                    if kb.cnt[e2] > 0 and e2 != e:
                        kb._wait(e, (kb.sem[e2], kb.cnt[e2], e2))

        phase_ln_T(x_all, NT, xnT_d)
        barrier()
        phase_ln_T(x_own, NOT, xnTo_d)

        barrier()

        with ExitStack() as es:
            W = kb.sb(es, [128, 8, 2048], F32R, "Wqk")
            for dc in range(8):
                kb.dma("pool", W[:, dc, :], w_in[dc * 128:(dc + 1) * 128, 0:2048], writes=[W])
            c_cw = kb.sb(es, [128, 16, 4], F32, "cw")
            kb.dma("sp", c_cw[:], conv_wc, writes=[c_cw])
            c_sel = kb.sb(es, [128, 32], F32R, "sel")
            kb.dma("pool", c_sel[:], sel, writes=[c_sel])
            xgs = [kb.sb(es, [128, 8, 512], F32R, "xg") for _ in range(2)]
            pre = kb.sb(es, [128, 16, 516], F32, "pre")
            preb = [Tl(None) for _ in range(16)]
            kb.op("dve", lambda e: e.memset(pre[:], 0.0), [], [pre] + preb)
            accs = [kb.sb(es, [128, 512], F32, "acc") for _ in range(2)]
            sqs = [kb.sb(es, [128, 512], F32, "sq") for _ in range(2)]
            qtok = kb.sb(es, [128, 4, D], F32R, "qtok")
            ktok = kb.sb(es, [128, 4, D], F32, "ktok")
            qTo = kb.sb(es, [128, 8, 128], F32, "qTo")
            pms = [kb.ps(es, [128, 512], F32, "pm") for _ in range(3)]
            ptr = [kb.ps(es, [128, 512], F32, "ptr") for _ in range(2)]
            psels = [kb.ps(es, [128, 512], F32, "psel") for _ in range(2)]
            pend_a1 = []
            for g in range(NG):
                xg = xgs[g % 2]
                kb.dma("pool", xg[:], _r(xnT_d[:, :, g * 512:(g + 1) * 512].rearrange("c p t -> p c t")), writes=[xg])
                pend_silu = []

                def emit_silu(cc):
                    acc, sq = accs[cc % 2], sqs[cc % 2]
                    kb.op("act", lambda e: e.activation(out=sq[:], in_=acc[:], func=AF.Silu), [acc], [sq])
                    if cc >= 8:
                        kb.dma("sp", kT_d[cc - 8, :, g * 512:(g + 1) * 512], sq[:], reads=[sq])

                    def tail(cc=cc, sq=sq):
                        pt = ptr[cc % 2]
                        for tk in range(4):
                            kb.op("pe", lambda e: e.transpose(out=pt[:, tk * 128:(tk + 1) * 128],
                                                              in_=sq[:, tk * 128:(tk + 1) * 128], identity=c_ident[:]),
                                  [sq, c_ident], [pt])
                        dst = qtok if cc < 8 else ktok
                        c8 = cc % 8
                        kb.op("dve", lambda e: e.tensor_copy(out=dst[:, :, c8 * 128:(c8 + 1) * 128],
                                                             in_=pt[:].rearrange("p (k c) -> p k c", k=4)), [pt], [dst])
                    pend_a1.append(tail)

                for cc in range(16):
                    pm = pms[cc % 3]
                    acc = accs[cc % 2]
                    pb = preb[cc]
                    for dc in range(8):
                        kb.op("pe", lambda e: e.matmul(pm[:], W[:, dc, cc * 128:(cc + 1) * 128], xg[:, dc, :],
                                                       start=(dc == 0), stop=(dc == 7)), [W, xg], [pm])
                    while pend_a1:
                        pend_a1.pop(0)()
                    kb.op("act", lambda e: e.activation(out=pre[:, cc, 0:4], in_=pre[:, cc, 512:516], func=AF.Identity),
                          [pb], [pb])
                    kb.op("act", lambda e: e.activation(out=pre[:, cc, 4:516], in_=pm[:], func=AF.Identity,
                                                        bias=c_cols[:, BQK + cc:BQK + cc + 1]), [pm, c_cols], [pb])
                    kb.op("act", lambda e: e.activation(out=acc[:], in_=pre[:, cc, 4:516], func=AF.Identity,
                                                        scale=c_cw[:, cc, 3:4], bias=c_cols[:, CBC + cc:CBC + cc + 1]),
                          [pb, c_cw, c_cols], [acc])
                    for j in range(3):
                        kb.op("dve", lambda e: e.scalar_tensor_tensor(out=acc[:], in0=pre[:, cc, 1 + j:513 + j],
                                                                      scalar=c_cw[:, cc, j:j + 1], in1=acc[:],
                                                                      op0=ALU.mult, op1=ALU.add), [pb, c_cw, acc], [acc])
                    while pend_silu:
                        emit_silu(pend_silu.pop(0))
                    pend_silu.append(cc)
                while pend_silu:
                    emit_silu(pend_silu.pop(0))
                while pend_a1:
                    pend_a1.pop(0)()
                kb.dma("sp", ktok_d[g * 512:(g + 1) * 512, :].rearrange("(k p) n -> p k n", p=128), ktok[:], reads=[ktok])
                for tk in range(4):
                    psl = psels[tk % 2]
                    for c8 in range(8):
                        kb.op("pe", lambda e: e.matmul(psl[:, c8 * 32:(c8 + 1) * 32], qtok[:, tk, c8 * 128:(c8 + 1) * 128],
                                                       c_sel[:], start=True, stop=True), [qtok, c_sel], [psl])
                    kb.op("act", lambda e: e.activation(out=qTo[:, :, tk * 32:(tk + 1) * 32],
                                                        in_=psl[:, 0:256].rearrange("p (c m) -> p c m", c=8),
                                                        func=AF.Identity), [psl], [qTo])
                kb.dma("sp", qTo_d[:, :, g * 128:(g + 1) * 128].rearrange("c p t -> p c t"), qTo[:], reads=[qTo])
        fence()
        def finish():
            fence()
            for e in kb.engs:
                for e2 in kb.engs:
                    if kb.cnt[e2] > 0:
                        kb._wait(e, (kb.sem[e2], kb.cnt[e2], e2))

        if upto == "A":
            for nm, src in (("kT", kT_d[:, :, :].rearrange("c p t -> (c p) t")), ("qTo", qTo_d[:, :, :].rearrange("c p t -> (c p) t")),
                            ("ktok", ktok_d)):
                if nm in dbg:
                    kb.dma("sp", dbg[nm], src)
            finish()
            nc.used_inputs = used_inputs
            return nc
        def bcast_load(es, src_row_ap, n, name):
            t = kb.sb(es, [128, n], F32, name)
            kb.dma("sp", t[:], src_row_ap.partition_broadcast(128), writes=[t])
            return t

        def rope(es, src, dst, cos, sin, nh, half, tmp):
            shp = [128, nh, half]
            cb = cos.unsqueeze(1).to_broadcast(shp)
            sb_ = sin.unsqueeze(1).to_broadcast(shp)
            x1, x2 = src[0], src[1]
            t = tmp
            kb.op("dve", lambda e: e.tensor_tensor(out=t[:, :, 0:half], in0=x1, in1=cb, op=ALU.mult), src[2], [t])
            kb.op("dve", lambda e: e.tensor_tensor(out=t[:, :, half:2 * half], in0=x2, in1=sb_, op=ALU.mult), src[2], [t])
            kb.op("dve", lambda e: e.tensor_tensor(out=dst[0], in0=t[:, :, 0:half], in1=t[:, :, half:2 * half],
                                                   op=ALU.subtract), [t], dst[2])
            kb.op("dve", lambda e: e.tensor_tensor(out=t[:, :, 0:half], in0=x2, in1=cb, op=ALU.mult), src[2], [t])
            kb.op("dve", lambda e: e.tensor_tensor(out=t[:, :, half:2 * half], in0=x1, in1=sb_, op=ALU.mult), src[2], [t])
            kb.op("dve", lambda e: e.tensor_tensor(out=dst[1], in0=t[:, :, 0:half], in1=t[:, :, half:2 * half],
                                                   op=ALU.add), [t], dst[2])

        WQ = kb.sb(es0, [128, NOT, 8], F32, "WQ")
        barrier()
        with ExitStack() as es:
            NC2 = 1096
            W = kb.sb(es, [128, 8, NC2], F32R, "W2")
            for dc in range(8):
                r0 = slice(dc * 128, (dc + 1) * 128)
                kb.dma("pool", W[:, dc, 0:1024], w_in[r0, O_MV:O_MV + 1024], writes=[W])
                kb.dma("pool", W[:, dc, 1024:1032], w_in[r0, O_MI:O_MI + 8], writes=[W])
                kb.dma("pool", W[:, dc, 1032:1096], w_in[r0, O_XK:O_XK + 64], writes=[W])
            bias = kb.sb(es, [128, NC2], F32, "bias2")
            kb.dma("sp", bias[:, 0:1024], b_in[0, O_MV:O_MV + 1024].partition_broadcast(128), writes=[bias])
            kb.dma("sp", bias[:, 1024:1032], b_in[0, O_MI:O_MI + 8].partition_broadcast(128), writes=[bias])
            kb.dma("sp", bias[:, 1032:1096], b_in[0, O_XK:O_XK + 64].partition_broadcast(128), writes=[bias])
            xgs = [kb.sb(es, [128, 8, 512], F32R, "xg") for _ in range(2)]
            vts = [kb.sb(es, [128, 4, D], F32, "vt") for _ in range(2)]
            gxs = [kb.sb(es, [128, 4, 72], F32, "gx") for _ in range(2)]
            rps = [kb.sb(es, [128, 4, 2, 32], F32, "rp") for _ in range(2)]
            xkr = kb.sb(es, [128, 4, 64], F32, "xkr")
            rtmp = kb.sb(es, [128, 4, 64], F32, "rtmp")
            ikg = kb.sb(es, [64, 512], F32, "ikg")
            pA = [kb.ps(es, [128, 512], F32, "pA") for _ in range(6)]
            pT = kb.ps(es, [128, 512], F32, "pT")
            for g in range(NG):
                xg, vt, gx, rp = xgs[g % 2], vts[g % 2], gxs[g % 2], rps[g % 2]
                kb.dma("pool", xg[:], _r(xnT_d[:, :, g * 512:(g + 1) * 512].rearrange("c p t -> p c t")), writes=[xg])
                kb.dma("sp", rp[:], rope_i[g * 512:(g + 1) * 512].rearrange("(k p) c d -> p k c d", p=128), writes=[rp])
                for tk in range(4):
                    ps3 = [pA[(tk % 2) * 3 + q] for q in range(3)]
                    for q, (c0, c1) in enumerate(((0, 512), (512, 1024), (1024, 1096))):
                        for dc in range(8):
                            kb.op("pe", lambda e: e.matmul(ps3[q][:, 0:c1 - c0], xg[:, dc, tk * 128:(tk + 1) * 128],
                                                           W[:, dc, c0:c1], start=(dc == 0), stop=(dc == 7)),
                                  [xg, W], [ps3[q]])
                    kb.op("dve", lambda e: e.tensor_tensor(out=vt[:, tk, 0:512], in0=ps3[0][:], in1=bias[:, 0:512],
                                                           op=ALU.add), [ps3[0], bias], [vt])
                    kb.op("dve", lambda e: e.tensor_tensor(out=vt[:, tk, 512:1024], in0=ps3[1][:], in1=bias[:, 512:1024],
                                                           op=ALU.add), [ps3[1], bias], [vt])
                    kb.op("dve", lambda e: e.tensor_tensor(out=gx[:, tk, :], in0=ps3[2][:, 0:72], in1=bias[:, 1024:1096],
                                                           op=ALU.add), [ps3[2], bias], [gx])
                kb.dma("sp", v_d[g * 512:(g + 1) * 512, :].rearrange("(k p) n -> p k n", p=128), vt[:], reads=[vt])
                kb.dma("sp", g_d[g * 512:(g + 1) * 512, :].rearrange("(k p) n -> p k n", p=128), gx[:, :, 0:8], reads=[gx])
                for tk in range(4):
                    cosv, sinv = rp[:, tk, 0, :], rp[:, tk, 1, :]
                    x1, x2 = gx[:, tk, 8:40], gx[:, tk, 40:72]
                    kb.op("dve", lambda e: e.tensor_tensor(out=rtmp[:, tk, 0:32], in0=x1, in1=cosv, op=ALU.mult), [gx, rp], [rtmp])
                    kb.op("dve", lambda e: e.tensor_tensor(out=rtmp[:, tk, 32:64], in0=x2, in1=sinv, op=ALU.mult), [gx, rp], [rtmp])
                    kb.op("dve", lambda e: e.tensor_tensor(out=xkr[:, tk, 0:32], in0=rtmp[:, tk, 0:32], in1=rtmp[:, tk, 32:64],
                                                           op=ALU.subtract), [rtmp], [xkr])
                    kb.op("dve", lambda e: e.tensor_tensor(out=rtmp[:, tk, 0:32], in0=x2, in1=cosv, op=ALU.mult), [gx, rp], [rtmp])
                    kb.op("dve", lambda e: e.tensor_tensor(out=rtmp[:, tk, 32:64], in0=x1, in1=sinv, op=ALU.mult), [gx, rp], [rtmp])
                    kb.op("dve", lambda e: e.tensor_tensor(out=xkr[:, tk, 32:64], in0=rtmp[:, tk, 0:32], in1=rtmp[:, tk, 32:64],
                                                           op=ALU.add), [rtmp], [xkr])
                for tk in range(4):
                    kb.op("pe", lambda e: e.transpose(out=pT[0:64, tk * 128:(tk + 1) * 128], in_=xkr[:, tk, :],
                                                      identity=c_ident[:]), [xkr, c_ident], [pT])
                kb.op("act", lambda e: e.activation(out=ikg[:], in_=pT[0:64, :], func=AF.Identity), [pT], [ikg])
                kb.dma("sp", ikT_d[:, g * 512:(g + 1) * 512], ikg[:], reads=[ikg])
        barrier()
        with ExitStack() as es:
            W = kb.sb(es, [128, 8, 2048], F32R, "W3")
            for dc in range(8):
                kb.dma("pool", W[:, dc, :], w_in[dc * 128:(dc + 1) * 128, O_AK:O_AK + 2048], writes=[W])
            bias = bcast_load(es, b_in[0, O_AK:O_AK + 2048], 2048, "bias3")
            xgs = [kb.sb(es, [128, 8, 512], F32R, "xg") for _ in range(2)]
            rps = [kb.sb(es, [128, 4, 2, 64], F32, "rpa") for _ in range(2)]
            ak = kb.sb(es, [128, 8, 128], F32, "ak")
            akrs = [kb.sb(es, [128, 8, 128], BF16, "akr") for _ in range(2)]
            pend_a3 = []
            rtmp = kb.sb(es, [128, 8, 128], F32, "rtmp3")
            akTs = [kb.sb(es, [128, 8, 512], BF16, "akT") for _ in range(2)]
            vaugs = [kb.sb(es, [128, 8, 4, 130], BF16, "vaug") for _ in range(2)]
            for va in vaugs:
                kb.op("dve", lambda e: e.memset(va[:], 1.0), [], [va])
            pA = [kb.ps(es, [128, 512], F32, "pA3") for _ in range(4)]
            pTb = [kb.ps(es, [128, 1024], BF16, "pTb") for _ in range(2)]
            for g in range(NG):
                xg, rp, akT, va = xgs[g % 2], rps[g % 2], akTs[g % 2], vaugs[g % 2]
                kb.dma("pool", xg[:], _r(xnT_d[:, :, g * 512:(g + 1) * 512].rearrange("c p t -> p c t")), writes=[xg])
                kb.dma("sp", rp[:], rope_a[g * 512:(g + 1) * 512].rearrange("(k p) c d -> p k c d", p=128), writes=[rp])
                for tk in range(4):
                    akr = akrs[tk % 2]
                    for q in range(4):
                        for dc in range(8):
                            kb.op("pe", lambda e: e.matmul(pA[q][:], xg[:, dc, tk * 128:(tk + 1) * 128],
                                                           W[:, dc, q * 512:(q + 1) * 512], start=(dc == 0), stop=(dc == 7)),
                                  [xg, W], [pA[q]])
                    while pend_a3:
                        pend_a3.pop(0)()
                    akf = ak[:].rearrange("p h d -> p (h d)")
                    for q in range(2):
                        kb.op("dve", lambda e: e.tensor_tensor(out=akf[:, q * 512:(q + 1) * 512], in0=pA[q][:],
                                                               in1=bias[:, q * 512:(q + 1) * 512], op=ALU.add),
                              [pA[q], bias], [ak])
                    for q in range(2):
                        kb.op("dve", lambda e: e.tensor_tensor(out=va[:, q * 4:(q + 1) * 4, tk, 0:128],
                                                               in0=pA[2 + q][:].rearrange("p (h d) -> p h d", h=4),
                                                               in1=bias[:, 1024 + q * 512:1024 + (q + 1) * 512].rearrange("p (h d) -> p h d", h=4),
                                                               op=ALU.add), [pA[2 + q], bias], [va])
                    rope(es, (ak[:, :, 0:64], ak[:, :, 64:128], [ak, rp]), (akr[:, :, 0:64], akr[:, :, 64:128], [akr]),
                         rp[:, tk, 0, :], rp[:, tk, 1, :], 8, 64, rtmp)
                    def tail(g=g, tk=tk, akr=akr, akT=akT, va=va):
                        ptb = pTb[tk % 2]
                        for h in range(8):
                            kb.op("pe", lambda e: e.transpose(out=ptb[:, h * 128:(h + 1) * 128], in_=akr[:, h, :],
                                                              identity=c_identb[:]), [akr, c_identb], [ptb])
                        kb.op("act", lambda e: e.activation(out=akT[:, :, tk * 128:(tk + 1) * 128],
                                                            in_=ptb[:].rearrange("p (h t) -> p h t", h=8), func=AF.Identity),
                              [ptb], [akT])
                        if tk == 3:
                            kb.dma("sp", akT_d[:, :, g * 512:(g + 1) * 512].rearrange("h p t -> p h t"), akT[:], reads=[akT])
                            kb.dma("sp", av_d[:, :, g * 4:(g + 1) * 4, :].rearrange("h p k c -> p h k c"), va[:], reads=[va])
                    pend_a3.append(tail)
            while pend_a3:
                pend_a3.pop(0)()
        def q_phase(col_specs, ncols, consume):
            barrier()
            with ExitStack() as es:
                W = kb.sb(es, [128, 8, ncols], F32R, "WQp")
                bias = kb.sb(es, [128, ncols], F32, "biasq")
                for (src0, n, dst0) in col_specs:
                    for dc in range(8):
                        kb.dma("pool", W[:, dc, dst0:dst0 + n], w_in[dc * 128:(dc + 1) * 128, src0:src0 + n], writes=[W])
                    kb.dma("sp", bias[:, dst0:dst0 + n], b_in[0, src0:src0 + n].partition_broadcast(128), writes=[bias])
                xos = [kb.sb(es, [128, 8, 128], F32R, "xo") for _ in range(2)]
                nb = (ncols + 511) // 512
                pQ = [kb.ps(es, [128, 512], F32, "pQ") for _ in range(4)]
                pend_q = []
                for ot in range(NOT):
                    xo = xos[ot % 2]
                    kb.dma("pool", xo[:], _r(xnTo_d[:, :, ot * 128:(ot + 1) * 128].rearrange("c p t -> p c t")), writes=[xo])
                    for q in range(nb):
                        c0, c1 = q * 512, min(ncols, (q + 1) * 512)
                        for dc in range(8):
                            kb.op("pe", lambda e: e.matmul(pQ[q][:, 0:c1 - c0], xo[:, dc, :], W[:, dc, c0:c1],
                                                           start=(dc == 0), stop=(dc == 7)), [xo, W], [pQ[q]])
                    while pend_q:
                        pend_q.pop(0)()
                    tl = consume(es, ot, pQ, bias)
                    if tl is not None:
                        pend_q.append(tl)
                while pend_q:
                    pend_q.pop(0)()

        barrier()
        with ExitStack() as esq:
            aq = kb.sb(esq, [128, 8, 128], F32, "aq")
            aqrs = [kb.sb(esq, [128, 8, 128], BF16, "aqr") for _ in range(2)]
            xq = kb.sb(esq, [128, 8, 64], F32, "xq")
            xqrs = [kb.sb(esq, [128, 8, 64], F32, "xqr") for _ in range(2)]
            rtq = kb.sb(esq, [128, 8, 128], F32, "rtq")
            rpa = kb.sb(esq, [128, 2, 64], F32, "rpao")
            rpi = kb.sb(esq, [128, 2, 32], F32, "rpio")
            aqT = kb.sb(esq, [128, 8, 128], BF16, "aqT")
            iqT = kb.sb(esq, [64, 8, 128], F32, "iqT")
            ptb = kb.ps(esq, [128, 1024], BF16, "ptbq")
            pti = [kb.ps(esq, [128, 512], F32, "ptiq") for _ in range(2)]

            def consume_q1(es, ot, pQ, bias):
                aqr, xqr = aqrs[ot % 2], xqrs[ot % 2]
                kb.dma("sp", rpa[:], rope_ao[ot * 128:(ot + 1) * 128], writes=[rpa])
                kb.dma("sp", rpi[:], rope_io[ot * 128:(ot + 1) * 128], writes=[rpi])
                aqf = aq[:].rearrange("p h d -> p (h d)")
                for q in range(2):
                    kb.op("dve", lambda e: e.tensor_tensor(out=aqf[:, q * 512:(q + 1) * 512], in0=pQ[q][:],
                                                           in1=bias[:, q * 512:(q + 1) * 512], op=ALU.add), [pQ[q], bias], [aq])
                kb.op("dve", lambda e: e.tensor_tensor(out=xq[:].rearrange("p h d -> p (h d)"), in0=pQ[2][:],
                                                       in1=bias[:, 1024:1536], op=ALU.add), [pQ[2], bias], [xq])
                kb.op("dve", lambda e: e.tensor_tensor(out=WQ[:, ot, :], in0=pQ[3][:, 0:8], in1=bias[:, 1536:1544],
                                                       op=ALU.add), [pQ[3], bias], [WQ])
                kb.op("dve", lambda e: e.tensor_scalar(out=WQ[:, ot, :], in0=WQ[:, ot, :], scalar1=float(8 ** -0.5 * 64 ** -0.5),
                                                      scalar2=None, op0=ALU.mult), [WQ], [WQ])
                rope(es, (aq[:, :, 0:64], aq[:, :, 64:128], [aq, rpa]), (aqr[:, :, 0:64], aqr[:, :, 64:128], [aqr]),
                     rpa[:, 0, :], rpa[:, 1, :], 8, 64, rtq)
                rope(es, (xq[:, :, 0:32], xq[:, :, 32:64], [xq, rpi]), (xqr[:, :, 0:32], xqr[:, :, 32:64], [xqr]),
                     rpi[:, 0, :], rpi[:, 1, :], 8, 32, rtq)
                def tail(ot=ot, aqr=aqr, xqr=xqr):
                    for h in range(8):
                        kb.op("pe", lambda e: e.transpose(out=ptb[:, h * 128:(h + 1) * 128], in_=aqr[:, h, :],
                                                          identity=c_identb[:]), [aqr, c_identb], [ptb])
                    kb.op("act", lambda e: e.activation(out=aqT[:], in_=ptb[:].rearrange("p (h t) -> p h t", h=8),
                                                        func=AF.Identity), [ptb], [aqT])
                    kb.dma("sp", aqT_d[ot], aqT[:], reads=[aqT])
                    for h in range(8):
                        kb.op("pe", lambda e: e.transpose(out=pti[h // 4][0:64, (h % 4) * 128:(h % 4 + 1) * 128],
                                                          in_=xqr[:, h, :], identity=c_ident[:]), [xqr, c_ident], [pti[h // 4]])
                    for q in range(2):
                        kb.op("act", lambda e: e.activation(out=iqT[:, q * 4:(q + 1) * 4, :],
                                                            in_=pti[q][0:64, :].rearrange("p (h t) -> p h t", h=4),
                                                            func=AF.Identity), [pti[q]], [iqT])
                    kb.dma("sp", iqT_d[ot], iqT[:], reads=[iqT])
                return tail

            q_phase([(O_AQ, 1024, 0), (O_XQ, 512, 1024), (O_XW, 8, 1536)], 1544, consume_q1)

        barrier()
        with ExitStack() as esq:
            t1 = kb.sb(esq, [128, 2048], F32, "t1")
            ogt = kb.sb(esq, [128, D], F32, "ogt")

            def consume_q2(es, ot, pQ, bias):
                for q in range(4):
                    kb.op("dve", lambda e: e.tensor_tensor(out=t1[:, q * 512:(q + 1) * 512], in0=pQ[q][:],
                                                           in1=bias[:, q * 512:(q + 1) * 512], op=ALU.add), [pQ[q], bias], [t1])
                kb.op("act", lambda e: e.activation(out=t1[:], in_=t1[:], func=AF.Sigmoid), [t1], [t1])
                kb.op("dve", lambda e: e.tensor_tensor(out=ogt[:], in0=t1[:, 0:1024], in1=t1[:, 1024:2048], op=ALU.mult),
                      [t1], [ogt])
                kb.dma("sp", og_d[ot * 128:(ot + 1) * 128, :], ogt[:], reads=[ogt])

            q_phase([(O_MO, 1024, 0), (O_GM, 1024, 1024)], 2048, consume_q2)

            def consume_q3(es, ot, pQ, bias):
                for q in range(2):
                    kb.op("dve", lambda e: e.tensor_tensor(out=t1[:, q * 512:(q + 1) * 512], in0=pQ[q][:],
                                                           in1=bias[:, q * 512:(q + 1) * 512], op=ALU.add), [pQ[q], bias], [t1])
                kb.op("act", lambda e: e.activation(out=ogt[:], in_=t1[:, 0:1024], func=AF.Sigmoid), [t1], [ogt])
                kb.dma("sp", ga_d[ot * 128:(ot + 1) * 128, :], ogt[:], reads=[ogt])

            q_phase([(O_GA, 1024, 0)], 1024, consume_q3)
        fence()
        if upto == "Q":
            for nm, src in (("v", v_d), ("ikT", ikT_d), ("og", og_d), ("ga", ga_d)):
                if nm in dbg:
                    kb.dma("sp", dbg[nm], src)
            finish()
            nc.used_inputs = used_inputs
            return nc
        barrier()
        with ExitStack() as es:
            G = kb.sb(es, [64, NCH, 8], F32, "G64")
            for c0 in range(0, NCH, 16):
                c1 = min(NCH, c0 + 16)
                kb.dma("sp", G[:, c0:c1, :], g_d[c0 * 64:c1 * 64, :].rearrange("(c s) n -> s c n", s=64), writes=[G])
            c_tri = kb.sb(es, [64, 64], F32, "tri64")
            kb.dma("sp", c_tri[:], tri64, writes=[c_tri])
            c_trs = kb.sb(es, [64, 8, 128], F32, "trisel")
            kb.dma("sp", c_trs[:], trisel.rearrange("c s m -> s c m"), writes=[c_trs])
            c_cm = kb.sb(es, [64, 16], F32, "cmask")
            kb.dma("sp", c_cm[:], cmask, writes=[c_cm])
            c_ones = kb.sb(es, [64, 128], F32, "ones64")
            kb.op("dve", lambda e: e.memset(c_ones[:], 1.0), [], [c_ones])
            mng = bcast_load(es, m_norm_g[0, :], D, "mng")
            NL = kb.sb(es, [64, NCH, 4], F32, "NL")
            NB = kb.sb(es, [64, NCH, 4], F32, "NB")
            NBL = kb.sb(es, [128, NCH, 4], F32, "NBL")
            DEC = kb.sb(es, [128, NCH, 4], F32, "DEC")
            WG = kb.sb(es, [64, NCH, 4], F32, "WG")
            CF = kb.sb(es, [64, NCH, 4], F32, "CF")
            RF = kb.sb(es, [128, NG, 4], F32, "RF")
            pg = [kb.ps(es, [128, 512], F32, "pgate") for _ in range(2)]
            kb.op("act", lambda e: e.activation(out=NL[:], in_=G[:, :, 4:8], func=AF.Exp, scale=-1.0), [G], [NL])
            kb.op("act", lambda e: e.activation(out=NL[:], in_=NL[:], func=AF.Ln, bias=1.0), [NL], [NL])
            NLf = NL[:].rearrange("s c h -> s (c h)")
            for c0 in range(0, NCH * 4, 512):
                c1 = min(NCH * 4, c0 + 512)
                kb.op("pe", lambda e: e.matmul(pg[0][0:64, 0:c1 - c0], c_tri[:], NLf[:, c0:c1], start=True, stop=True),
                      [c_tri, NL], [pg[0]])
                kb.op("dve", lambda e: e.tensor_copy(out=NB[:].rearrange("s c h -> s (c h)")[:, c0:c1], in_=pg[0][0:64, 0:c1 - c0]),
                      [pg[0]], [NB])
                kb.op("pe", lambda e: e.matmul(pg[1][:, 0:c1 - c0], c_ones[:], NLf[:, c0:c1], start=True, stop=True),
                      [c_ones, NL], [pg[1]])
                kb.op("dve", lambda e: e.tensor_copy(out=NBL[:].rearrange("s c h -> s (c h)")[:, c0:c1], in_=pg[1][:, 0:c1 - c0]),
                      [pg[1]], [NBL])
            kb.op("act", lambda e: e.activation(out=DEC[:], in_=NBL[:], func=AF.Exp, scale=-1.0), [NBL], [DEC])
            kb.op("dve", lambda e: e.tensor_tensor(out=CF[:], in0=G[:, :, 0:4], in1=NB[:], op=ALU.add), [G, NB], [CF])
            kb.op("dve", lambda e: e.tensor_tensor(out=WG[:], in0=CF[:], in1=NBL[0:64], op=ALU.subtract), [CF, NBL], [WG])
            kb.op("act", lambda e: e.activation(out=CF[:], in_=CF[:], func=AF.Exp), [CF], [CF])
            kb.op("act", lambda e: e.activation(out=WG[:], in_=WG[:], func=AF.Exp), [WG], [WG])
            for g in range(NG):
                for cp in range(8):
                    kb.op("pe", lambda e: e.matmul(pg[0][:, 0:4], c_trs[:, cp, :], NL[:, g * 8 + cp, :],
                                                   start=(cp == 0), stop=(cp == 7)), [c_trs, NL], [pg[0]])
                kb.op("act", lambda e: e.activation(out=RF[:, g, :], in_=pg[0][:, 0:4], func=AF.Exp, scale=-1.0), [pg[0]], [RF])
            kTs = [kb.sb(es, [128, 8, 512], F32R, "kTg") for _ in range(2)]
            kts = [kb.sb(es, [64, D], F32, "ktg") for _ in range(2)]
            kws = [kb.sb(es, [64, 4, 256], F32R, "kwg") for _ in range(3)]
            vgs = [kb.sb(es, [64, 4, 258], F32R, "vg") for _ in range(3)]
            qgs = [kb.sb(es, [128, 8, 128], F32R, "qg") for _ in range(2)]
            for vg in vgs:
                kb.op("dve", lambda e: e.memset(_f(vg[:]), 1.0), [], [vg])
            qpad = kb.sb(es, [128, 8, 8, 128], F32R, "qpad")
            spad = kb.sb(es, [64, 4, 8, 128], F32R, "spad")
            kb.op("dve", lambda e: e.memset(_f(qpad[:]), 0.0), [], [qpad])
            kb.op("dve", lambda e: e.memset(_f(spad[:]), 0.0), [], [spad])
            C32 = [kb.sb(es, [128, 2, 258], F32, "C32_%d" % h) for h in range(4)]
            Cr = [kb.sb(es, [128, 2, 258], F32R, "Cr_%d" % h) for h in range(4)]
            for h in range(4):
                kb.op("dve", lambda e: e.memset(C32[h][:], 0.0), [], [C32[h]])
                kb.op("dve", lambda e: e.memset(_f(Cr[h][:]), 0.0), [], [Cr[h]])
            spb = [[Tl(None) for _ in range(8)] for _ in range(4)]
            hm = kb.sb(es, [128, 4, 256], F32, "hm")
            ogts = [kb.sb(es, [128, D], F32, "ogtB") for _ in range(2)]
            zt = kb.sb(es, [128, D], F32, "zt")
            kb.op("dve", lambda e: e.memset(zt[:], 0.0), [], [zt])
            zrows = list(range(0, NEXP * CAP, 128))
            ymt = kb.sb(es, [128, D], F32, "ymt")
            sc = kb.sb(es, [128, 16], F32, "scB")
            stB = (kb.sb(es, [128, 1, 6], F32, "stB"), kb.sb(es, [128, 4], F32, "mvB"))
            pacc = [kb.ps(es, [128, 512], F32, "pacc") for _ in range(4)]
            pU = pg
            pS = kb.ps(es, [128, 512], F32, "pS")
            pS_s = [Tl(None), Tl(None)]
            pS_n = [Tl(None), Tl(None)]
            c_one2 = kb.sb(es, [64, 2], F32R, "one2")
            kb.op("dve", lambda e: e.memset(_f(c_one2[:]), 1.0), [], [c_one2])
            def group_loads(g):
                kT, qg = kTs[g % 2], qgs[g % 2]
                kb.dma("pool", kT[:], _r(kT_d[:, :, g * 512:(g + 1) * 512].rearrange("c p t -> p c t")), writes=[kT])
                kb.dma("pool", qg[:], _r(qTo_d[:, :, g * 128:(g + 1) * 128].rearrange("c p t -> p c t")), writes=[qg])
                kb.dma("sp", ogts[g % 2][:], og_d[g * 128:(g + 1) * 128, :], writes=[ogts[g % 2]])

            for g in range(NG):
                kT, qg, ogt = kTs[g % 2], qgs[g % 2], ogts[g % 2]
                if g == 0:
                    group_loads(0)
                for cp in range(8):
                    kb.op("pool", lambda e: e.tensor_copy(out=qpad[:, :, cp, cp * 16:(cp + 1) * 16], in_=qg[:, :, cp * 16:(cp + 1) * 16]),
                          [qg], [qpad])
                if g + 1 < NG:
                    group_loads(g + 1)
                nz = (len(zrows) + NG - 1) // NG
                for r0 in zrows[g * nz:(g + 1) * nz]:
                    kb.dma("sp", xs_d[r0:r0 + 128, :], zt[:], reads=[zt])
                def chunk_loads(c):
                    kt, kw, vg = kts[c % 2], kws[c % 3], vgs[c % 3]
                    kb.dma("sp", kt[:], ktok_d[c * 64:(c + 1) * 64, :], writes=[kt])
                    kb.dma("pool", vg[:, :, 0:256], _r(v_d[c * 64:(c + 1) * 64, :].rearrange("s (h d) -> s h d", h=4)), writes=[vg])
                    kb.op("pool", lambda e: e.tensor_tensor(out=kw[:], in0=kt[:].rearrange("s (h d) -> s h d", h=4),
                                                            in1=WG[:, c, :].unsqueeze(2).to_broadcast([64, 4, 256]),
                                                            op=ALU.mult), [kt, WG], [kw])

                def emit_S(cp, h):
                    c = g * 8 + cp
                    par = h % 2
                    for half in range(2):
                        kb.op("pe", lambda e: e.matmul(pS[0:64, par * 16:(par + 1) * 16], kT[:, h * 2 + half, cp * 64:(cp + 1) * 64],
                                                       qg[:, h * 2 + half, cp * 16:(cp + 1) * 16], start=(half == 0), stop=(half == 1)),
                              [kT, qg], [pS_s[par]])
                    kb.op("dve", lambda e: e.scalar_tensor_tensor(out=spad[:, h, cp, cp * 16:(cp + 1) * 16],
                                                                  in0=pS[0:64, par * 16:(par + 1) * 16],
                                                                  scalar=CF[:, c, h:h + 1], in1=c_cm[:], op0=ALU.mult, op1=ALU.mult),
                          [pS_s[par], CF, c_cm], [spb[h][cp]])

                steps = [(cp, h) for cp in range(8) for h in range(4)]
                emit_S(*steps[0])
                for si, (cp, h) in enumerate(steps):
                    c = g * 8 + cp
                    kt, kw, vg = kts[c % 2], kws[c % 3], vgs[c % 3]
                    if h == 0:
                        if c == 0:
                            chunk_loads(0)
                        if c + 1 < NCH:
                            chunk_loads(c + 1)
                    if si + 1 < len(steps):
                        emit_S(*steps[si + 1])
                    par = h % 2
                    kb.op("pe", lambda e: e.matmul(pacc[h][:, 0:258], spad[:, h, cp, :], vg[:, h, :],
                                                   start=(cp == 0), stop=False), [spb[h][cp], vg], [pacc[h]])
                    for half in range(2):
                        kb.op("pe", lambda e: e.matmul(pacc[h][:, 0:258], qpad[:, h * 2 + half, cp, :], Cr[h][:, half, :],
                                                       start=False, stop=(cp == 7 and half == 1)), [qpad, Cr[h]], [pacc[h]])
                    for half in range(2):
                        kb.op("pe", lambda e: e.matmul(pU[par][:, half * 256:(half + 1) * 256], kw[:, h, half * 128:(half + 1) * 128],
                                                       vg[:, h, 0:256], start=True, stop=True), [kw, vg], [pU[par]])
                        kb.op("pe", lambda e: e.matmul(pS[:, 64 + par * 4 + half * 2:64 + par * 4 + half * 2 + 2],
                                                       kw[:, h, half * 128:(half + 1) * 128], c_one2[:], start=True, stop=True),
                              [kw, c_one2], [pS_n[par]])
                    kb.op("dve", lambda e: e.scalar_tensor_tensor(out=C32[h][:, :, 0:256], in0=C32[h][:, :, 0:256],
                                                                  scalar=DEC[:, c, h:h + 1],
                                                                  in1=pU[par][:].rearrange("p (a d) -> p a d", a=2),
                                                                  op0=ALU.mult, op1=ALU.add), [C32[h], DEC, pU[par]], [C32[h]])
                    kb.op("dve", lambda e: e.scalar_tensor_tensor(out=C32[h][:, :, 256:258], in0=C32[h][:, :, 256:258],
                                                                  scalar=DEC[:, c, h:h + 1],
                                                                  in1=pS[:, 64 + par * 4:64 + par * 4 + 4].rearrange("p (a d) -> p a d", a=2),
                                                                  op0=ALU.mult, op1=ALU.add), [C32[h], DEC, pS_n[par]], [C32[h]])
                    kb.op("act", lambda e: e.activation(out=Cr[h][:], in_=C32[h][:], func=AF.Identity), [C32[h]], [Cr[h]])
                for h in range(4):
                    kb.op("dve", lambda e: e.tensor_scalar(out=sc[:, 0:1], in0=pacc[h][:, 256:257], scalar1=RF[:, g, h:h + 1],
                                                          scalar2=None, op0=ALU.mult), [pacc[h], RF], [sc])
                    kb.op("dve", lambda e: e.tensor_scalar(out=sc[:, 3:4], in0=sc[:, 0:1], scalar1=-1.0, scalar2=None,
                                                          op0=ALU.mult), [sc], [sc])
                    kb.op("dve", lambda e: e.tensor_tensor(out=sc[:, 0:1], in0=sc[:, 0:1], in1=sc[:, 3:4], op=ALU.max), [sc], [sc])
                    kb.op("dve", lambda e: e.tensor_scalar(out=sc[:, 0:1], in0=sc[:, 0:1], scalar1=1.0, scalar2=None,
                                                          op0=ALU.max), [sc], [sc])
                    kb.op("dve", lambda e: e.reciprocal(out=sc[:, 1:2], in_=sc[:, 0:1]), [sc], [sc])
                    kb.op("dve", lambda e: e.tensor_tensor(out=sc[:, 2:3], in0=sc[:, 1:2], in1=RF[:, g, h:h + 1], op=ALU.mult),
                          [sc, RF], [sc])
                    kb.op("act", lambda e: e.activation(out=hm[:, h, :], in_=pacc[h][:, 0:256], func=AF.Identity, scale=sc[:, 2:3]),
                          [pacc[h], sc], [hm])
                    kb.op("dve", lambda e: e.bn_stats(out=stB[0][:, 0, :], in_=hm[:, h, :]), [hm], [stB[0]])
                    kb.op("dve", lambda e: e.bn_aggr(out=stB[1][:, 0:2], in_=stB[0][:, 0, :]), [stB[0]], [stB[1]])
                    kb.op("dve", lambda e: e.tensor_scalar(out=stB[1][:, 2:3], in0=stB[1][:, 1:2], scalar1=LN_EPS, scalar2=None,
                                                          op0=ALU.add), [stB[1]], [stB[1]])
                    kb.op("act", lambda e: e.activation(out=stB[1][:, 2:3], in_=stB[1][:, 2:3], func=AF.Sqrt), [stB[1]], [stB[1]])
                    kb.op("dve", lambda e: e.reciprocal(out=stB[1][:, 3:4], in_=stB[1][:, 2:3]), [stB[1]], [stB[1]])
                    kb.op("dve", lambda e: e.tensor_scalar(out=ymt[:, h * 256:(h + 1) * 256], in0=hm[:, h, :], scalar1=stB[1][:, 0:1],
                                                          scalar2=stB[1][:, 3:4], op0=ALU.subtract, op1=ALU.mult), [hm, stB[1]], [ymt])
                kb.op("dve", lambda e: e.tensor_tensor(out=ymt[:], in0=ymt[:], in1=mng[:], op=ALU.mult), [ymt, mng], [ymt])
                kb.op("dve", lambda e: e.tensor_tensor(out=ymt[:], in0=ymt[:], in1=ogt[:], op=ALU.mult), [ymt, ogt], [ymt])
                kb.dma("sp", ym_d[g * 128:(g + 1) * 128, :], ymt[:], reads=[ymt])
        fence()
        if upto == "B":
            if "ym" in dbg:
                kb.dma("sp", dbg["ym"], ym_d)
            finish()
            nc.used_inputs = used_inputs
            return nc
        barrier()
        with ExitStack() as es:
            c_adm = kb.sb(es, [128, 512], F32, "admb")
            kb.dma("sp", c_adm[:], admb, writes=[c_adm])
            c_pw = kb.sb(es, [128, NBIS], F32, "pw2")
            kb.dma("sp", c_pw[:], pw2, writes=[c_pw])
            NMAX = T
            score = kb.sb(es, [128, NMAX], F32, "score")
            msk = kb.sb(es, [128, NMAX], BF16, "msk")
            mbT = kb.sb(es, [128, NMAX // 128, 128], BF16, "mbT")
            Rb = [kb.sb(es, [128, 2, 512], F32R, "Rb") for _ in range(4)]
            Dw = kb.sb(es, [128, 8, 128], F32R, "Dw")
            iqs = [kb.sb(es, [64, 8, 128], F32R, "iq") for _ in range(2)]
            aqs = [kb.sb(es, [128, 8, 128], BF16, "aqs") for _ in range(2)]
            iks = [kb.sb(es, [64, 512], F32R, "ik") for _ in range(2)]
            KhT = [kb.sb(es, [128, NMAX], BF16, "KhT") for _ in range(2)]
            Vh = [kb.sb(es, [128, NMAX // 128, 130], BF16, "Vh") for _ in range(2)]
            PT = [kb.sb(es, [128, 4, 128], BF16, "PT") for _ in range(2)]
            bs = kb.sb(es, [128, 8 + NBIS], F32, "bs")
            rs8 = kb.sb(es, [128, 8], F32, "rs8")
            ya = kb.sb(es, [128, 8, 128], F32, "ya")
            gat = kb.sb(es, [128, D], F32, "gat")
            ymt = kb.sb(es, [128, D], F32, "ymtD")
            PS = [kb.ps(es, [128, 512], F32, "PDs") for _ in range(2)]
            PSC = [kb.ps(es, [128, 512], F32, "PDsc") for _ in range(2)]
            LG = [kb.ps(es, [128, 512], F32, "PDlg") for _ in range(2)]
            PO = kb.ps(es, [128, 512], F32, "PDo")
            PO_t = [Tl(None), Tl(None)]
            Pb = kb.ps(es, [128, 1024], BF16, "PDb")
            qscale = float(128 ** -0.5)

            def kv_load(i, h):
                N = 512 * (i + 1)
                Kt, Vt = KhT[h % 2], Vh[h % 2]
                kb.dma("sp", Kt[:, 0:N], akT_d[h, :, 0:N], writes=[Kt])
                kb.dma("sp", Vt[:, 0:N // 128, :], av_d[h, :, 0:N // 128, :], writes=[Vt])

            def stage1(i):
                N = 512 * (i + 1)
                iq = iqs[i % 2]
                th = []

                def t_load():
                    kb.dma("pool", iq[:], _r(iqT_d[i]), writes=[iq])
                    for h in range(8):
                        kb.op("pool", lambda e: e.tensor_scalar(out=Dw[:, h, :], in0=c_ident[:], scalar1=WQ[:, i, h:h + 1], scalar2=None,
                                                               op0=ALU.mult), [c_ident, WQ], [Dw])
                th.append((1.0, t_load))
                units = [(kt, hp) for kt in range(i + 1) for hp in range(4)]

                def emit_S(u):
                    kt, hp = units[u]
                    ik = iks[kt % 2]
                    if hp == 0:
                        kb.dma("pool", ik[:], _r(ikT_d[:, kt * 512:(kt + 1) * 512]), writes=[ik])
                    R_ = Rb[u % 4]
                    for j in range(2):
                        h = hp * 2 + j
                        pS_ = PS[j]
                        kb.op("pe", lambda e: e.matmul(pS_[:], iq[:, h, :], ik[:], start=True, stop=True), [iq, ik], [pS_])
                        if j == 0:
                            kb.op("act", lambda e: e.activation(out=R_[:, j, :], in_=pS_[:], func=AF.Relu), [pS_], [R_])
                        else:
                            kb.op("dve", lambda e: e.tensor_scalar(out=R_[:, j, :], in0=pS_[:], scalar1=0.0, scalar2=None,
                                                                  op0=ALU.max), [pS_], [R_])

                def emit_Sc(u):
                    kt, hp = units[u]
                    psc = PSC[kt % 2]
                    R_ = Rb[u % 4]
                    for j in range(2):
                        h = hp * 2 + j
                        kb.op("pe", lambda e: e.matmul(psc[:], Dw[:, h, :], R_[:, j, :], start=(h == 0), stop=(h == 7)),
                              [Dw, R_], [psc])
                    if hp == 3:
                        kb.op("act", lambda e: e.activation(out=score[:, kt * 512:(kt + 1) * 512], in_=psc[:], func=AF.Identity),
                              [psc], [score])

                th.append((1.0, lambda: emit_S(0)))
                for u in range(len(units)):
                    def t_unit(u=u):
                        if u + 1 < len(units):
                            emit_S(u + 1)
                        emit_Sc(u)
                    th.append((1.3, t_unit))

                def t_prep():
                    kb.op("dve", lambda e: e.tensor_reduce(out=bs[:, 0:1], in_=score[:, 0:N], axis=AX.X, op=ALU.max), [score], [bs])
                    kb.op("dve", lambda e: e.tensor_reduce(out=bs[:, 1:2], in_=score[:, 0:N], axis=AX.X, op=ALU.min), [score], [bs])
                    kb.op("dve", lambda e: e.tensor_scalar(out=bs[:, 2:3], in0=bs[:, 1:2], scalar1=-1.0, scalar2=None, op0=ALU.add), [bs], [bs])
                    kb.op("dve", lambda e: e.tensor_tensor(out=bs[:, 3:4], in0=bs[:, 0:1], in1=bs[:, 2:3], op=ALU.subtract), [bs], [bs])
                    kb.op("dve", lambda e: e.tensor_scalar(out=bs[:, 8:8 + NBIS], in0=c_pw[:], scalar1=bs[:, 3:4], scalar2=None,
                                                          op0=ALU.mult), [bs, c_pw], [bs])
                    kb.op("dve", lambda e: e.tensor_tensor(out=score[:, N - 512:N], in0=score[:, N - 512:N], in1=c_adm[:], op=ALU.add),
                          [score, c_adm], [score])
                th.append((2.0 * N / 960.0 + 1.0, t_prep))

                def t_bis(n):
                    kb.op("dve", lambda e: e.tensor_tensor(out=bs[:, 4:5], in0=bs[:, 2:3], in1=bs[:, 8 + n:9 + n], op=ALU.add), [bs], [bs])
                    kb.op("dve", lambda e: e.tensor_scalar(out=msk[:, 0:N], in0=score[:, 0:N], scalar1=bs[:, 4:5], scalar2=0.0,
                                                          op0=ALU.is_gt, op1=ALU.add, accum_out=bs[:, 5:6]), [score, bs], [msk, bs])
                    kb.op("dve", lambda e: e.tensor_scalar(out=bs[:, 6:7], in0=bs[:, 5:6], scalar1=255.5, scalar2=None, op0=ALU.is_gt),
                          [bs], [bs])
                    kb.op("dve", lambda e: e.scalar_tensor_tensor(out=bs[:, 2:3], in0=bs[:, 8 + n:9 + n], scalar=bs[:, 6:7],
                                                                  in1=bs[:, 2:3], op0=ALU.mult, op1=ALU.add), [bs], [bs])
                for n in range(NBIS):
                    th.append((N / 960.0 + 0.5, lambda n=n: t_bis(n)))

                def t_final():
                    kb.op("dve", lambda e: e.tensor_scalar(out=msk[:, 0:N], in0=score[:, 0:N], scalar1=bs[:, 2:3], scalar2=None,
                                                          op0=ALU.is_gt), [score, bs], [msk])
                th.append((N / 960.0 + 0.2, t_final))
                return th

            def mask_T(i):
                N = 512 * (i + 1)
                for k8 in range(0, N // 128, 8):
                    for kb_ in range(8):
                        kk = k8 + kb_
                        if kk >= N // 128:
                            break
                        kb.op("pe", lambda e: e.transpose(out=Pb[:, kb_ * 128:(kb_ + 1) * 128], in_=msk[:, kk * 128:(kk + 1) * 128],
                                                          identity=c_identb[:]), [msk, c_identb], [Pb])
                    nn = min(8, N // 128 - k8)
                    kb.op("act", lambda e: e.activation(out=mbT[:, k8:k8 + nn, :],
                                                        in_=Pb[:, 0:nn * 128].rearrange("p (k q) -> p k q", k=nn),
                                                        func=AF.Identity, scale=30000.0, bias=-30000.0), [Pb], [mbT])

            def stage2(i):
                N = 512 * (i + 1)
                aq_ = aqs[i % 2]
                th = []

                def t_loads():
                    if i == 0:
                        kb.dma("sp", aqs[0][:], aqT_d[0], writes=[aqs[0]])
                        kv_load(0, 0)
                    if i + 1 < NG:
                        kb.dma("sp", aqs[(i + 1) % 2][:], aqT_d[i + 1], writes=[aqs[(i + 1) % 2]])
                    kb.dma("sp", gat[:], ga_d[i * 128:(i + 1) * 128, :], writes=[gat])
                    kb.dma("sp", ymt[:], ym_d[i * 128:(i + 1) * 128, :], writes=[ymt])
                th.append((0.2, t_loads))

                def emit_lg(h, k4):
                    Kt = KhT[h % 2]
                    lg = LG[k4 % 2]
                    pt = PT[k4 % 2]
                    for j4 in range(4):
                        kk = k4 * 4 + j4
                        kb.op("pe", lambda e: e.matmul(lg[:, j4 * 128:(j4 + 1) * 128], Kt[:, kk * 128:(kk + 1) * 128], aq_[:, h, :],
                                                       start=True, stop=False), [Kt, aq_], [lg])
                        kb.op("pe", lambda e: e.matmul(lg[:, j4 * 128:(j4 + 1) * 128], c_identb[:], mbT[:, kk, :],
                                                       start=False, stop=True), [c_identb, mbT], [lg])
                    kb.op("act", lambda e: e.activation(out=pt[:].rearrange("p k q -> p (k q)"), in_=lg[:], func=AF.Exp, scale=qscale),
                          [lg], [pt])

                def emit_pv(h, k4):
                    Vt = Vh[h % 2]
                    pt = PT[k4 % 2]
                    par = h % 2
                    O = PO[:, par * 256:par * 256 + 130]
                    for j4 in range(4):
                        kk = k4 * 4 + j4
                        kb.op("pe", lambda e: e.matmul(O, pt[:, j4, :], Vt[:, kk, :], start=(kk == 0),
                                                       stop=(kk == N // 128 - 1)), [pt, Vt], [PO_t[par]])
                    if k4 == N // 512 - 1:
                        kb.op("dve", lambda e: e.reciprocal(out=rs8[:, h:h + 1], in_=PO[:, par * 256 + 128:par * 256 + 129]),
                              [PO_t[par]], [rs8])
                        kb.op("dve", lambda e: e.tensor_scalar(out=ya[:, h, :], in0=PO[:, par * 256:par * 256 + 128],
                                                              scalar1=rs8[:, h:h + 1], scalar2=None, op0=ALU.mult),
                              [PO_t[par], rs8], [ya])

                for h in range(8):
                    def t_first(h=h):
                        if h < 7:
                            kv_load(i, h + 1)
                        elif i + 1 < NG:
                            kv_load(i + 1, 0)
                        emit_lg(h, 0)
                    th.append((0.9, t_first))
                    for k4 in range(N // 512):
                        def t_blk(h=h, k4=k4):
                            if k4 + 1 < N // 512:
                                emit_lg(h, k4 + 1)
                            emit_pv(h, k4)
                        th.append((0.75, t_blk))

                def t_merge():
                    kb.op("dve", lambda e: e.tensor_tensor(out=gat[:], in0=gat[:], in1=ya[:].rearrange("p h d -> p (h d)"), op=ALU.mult),
                          [gat, ya], [gat])
                    kb.op("dve", lambda e: e.tensor_tensor(out=gat[:], in0=gat[:], in1=ymt[:], op=ALU.add), [gat, ymt], [gat])
                    kb.dma("sp", mg_d[i * 128:(i + 1) * 128, :], gat[:], reads=[gat])
                th.append((2.0, t_merge))
                return th

            def run_merged(A, Bl):
                ta = sum(w for w, _ in A) or 1.0
                tb = sum(w for w, _ in Bl) or 1.0
                ia = ib = 0
                da = db = 0.0
                while ia < len(A) or ib < len(Bl):
                    if ib >= len(Bl) or (ia < len(A) and da / ta <= db / tb):
                        w, f = A[ia]
                        ia += 1
                        da += w
                    else:
                        w, f = Bl[ib]
                        ib += 1
                        db += w
                    f()

            for _, f in stage1(0):
                f()
            mask_T(0)
            for i in range(NG):
                run_merged(stage2(i), stage1(i + 1) if i + 1 < NG else [])
                if i + 1 < NG:
                    mask_T(i + 1)
        fence()
        if upto == "D":
            if "mg" in dbg:
                kb.dma("sp", dbg["mg"], mg_d)
            finish()
            nc.used_inputs = used_inputs
            return nc
        barrier()
        NSL = NEXP * CAP
        GK = kb.sb(es0, [128, NOT, 4], F32, "GK")
        bc_reg = nc.gpsimd.to_reg(NSL - 1)
        SLI = kb.sb(es0, [128, NOT, 4], I32, "SLI")

        def ln_full(xt, stp, gt, bt, outt):
            mean, rstd = layernorm_stats(None, xt, stp)
            kb.op("dve", lambda e: e.tensor_scalar(out=outt[:], in0=xt[:], scalar1=mean, scalar2=rstd,
                                                  op0=ALU.subtract, op1=ALU.mult), [xt, stp[1]], [outt])
            kb.op("dve", lambda e: e.tensor_tensor(out=outt[:], in0=outt[:], in1=gt[:], op=ALU.mult), [outt, gt], [outt])
            kb.op("dve", lambda e: e.tensor_tensor(out=outt[:], in0=outt[:], in1=bt[:], op=ALU.add), [outt, bt], [outt])

        with ExitStack() as es:
            Wo = kb.sb(es, [128, 8, D], F32R, "Wo")
            for dc in range(8):
                kb.dma("pool", Wo[:, dc, :], w_out[dc * 128:(dc + 1) * 128, :], writes=[Wo])
            g0, b0, g1, b1 = [bcast_load(es, vecs[k, :], D, "lnv%d" % k) for k in range(4)]
            Wr = kb.sb(es, [128, 8, NEXP], F32, "Wr")
            kb.dma("sp", Wr[:], w_router.rearrange("(c p) n -> p c n", p=128), writes=[Wr])
            br = bcast_load(es, b_router[0, :], NEXP, "br")
            c_us = kb.sb(es, [128, 128], F32, "ustri")
            kb.dma("sp", c_us[:], ustri, writes=[c_us])
            c_on = kb.sb(es, [128, 128], F32, "ones128")
            kb.op("dve", lambda e: e.memset(c_on[:], 1.0), [], [c_on])
            c_eo = kb.sb(es, [128, NEXP], F32, "eoff")
            kb.dma("sp", c_eo[:], eoff, writes=[c_eo])
            MK = kb.sb(es, [128, NOT, NEXP], F32, "MK")
            mgt = kb.sb(es, [128, D], F32, "mgt")
            mT = kb.sb(es, [128, 8, 128], F32R, "mT")
            xt = kb.sb(es, [128, D], F32, "xtE")
            h0 = kb.sb(es, [128, D], F32, "h0")
            rs = kb.sb(es, [128, D], F32, "rs")
            h1 = kb.sb(es, [128, D], F32, "h1")
            h1T = kb.sb(es, [128, 8, 128], F32, "h1T")
            stp = (kb.sb(es, [128, 2, 6], F32, "stE"), kb.sb(es, [128, 4], F32, "mvE"))
            lgt = kb.sb(es, [128, NEXP], F32, "lgt")
            ex = kb.sb(es, [128, NEXP], F32, "ex")
            gts = kb.sb(es, [128, NEXP], F32, "gts")
            slf = kb.sb(es, [128, NEXP], F32, "slf")
            m8 = kb.sb(es, [128, 8], F32, "m8")
            sm = kb.sb(es, [128, 8], F32, "sm")
            slk = kb.sb(es, [128, 4], F32, "slk")
            junk = kb.sb(es, [128, NEXP], F32, "junk")
            pt = [kb.ps(es, [128, 512], F32, "ptE") for _ in range(2)]
            po = [kb.ps(es, [128, 512], F32, "poE") for _ in range(2)]
            pr = kb.ps(es, [128, 512], F32, "prE")
            pp = kb.ps(es, [128, 512], F32, "ppE")
            fence()
            for ot in range(NOT):
                kb.dma("sp", mgt[:], mg_d[ot * 128:(ot + 1) * 128, :], writes=[mgt])
                kb.dma("sp", xt[:], x_own[ot * 128:(ot + 1) * 128, :], writes=[xt])
                for half in range(2):
                    for k in range(4):
                        dc = half * 4 + k
                        kb.op("pe", lambda e: e.transpose(out=pt[half][:, k * 128:(k + 1) * 128], in_=mgt[:, dc * 128:(dc + 1) * 128],
                                                          identity=c_ident[:]), [mgt, c_ident], [pt[half]])
                    kb.op("act", lambda e: e.activation(out=mT[:, half * 4:(half + 1) * 4, :],
                                                        in_=pt[half][:].rearrange("p (k t) -> p k t", k=4), func=AF.Identity),
                          [pt[half]], [mT])
                for q in range(2):
                    for dc in range(8):
                        kb.op("pe", lambda e: e.matmul(po[q][:], mT[:, dc, :], Wo[:, dc, q * 512:(q + 1) * 512],
                                                       start=(dc == 0), stop=(dc == 7)), [mT, Wo], [po[q]])
                ln_full(xt, stp, g0, b0, h0)
                for q in range(2):
                    kb.op("dve", lambda e: e.scalar_tensor_tensor(out=rs[:, q * 512:(q + 1) * 512], in0=h0[:, q * 512:(q + 1) * 512],
                                                                  scalar=float(ALPHA), in1=po[q][:], op0=ALU.mult, op1=ALU.add),
                          [h0, po[q]], [rs])
                ln_full(rs, stp, g1, b1, h1)
                kb.dma("sp", h1_d[ot * 128:(ot + 1) * 128, :], h1[:], reads=[h1])
                for half in range(2):
                    for k in range(4):
                        dc = half * 4 + k
                        kb.op("pe", lambda e: e.transpose(out=pt[half][:, k * 128:(k + 1) * 128], in_=h1[:, dc * 128:(dc + 1) * 128],
                                                          identity=c_ident[:]), [h1, c_ident], [pt[half]])
                    kb.op("act", lambda e: e.activation(out=h1T[:, half * 4:(half + 1) * 4, :],
                                                        in_=pt[half][:].rearrange("p (k t) -> p k t", k=4), func=AF.Identity),
                          [pt[half]], [h1T])
                for dc in range(8):
                    kb.op("pe", lambda e: e.matmul(pr[:, 0:NEXP], h1T[:, dc, :], Wr[:, dc, :], start=(dc == 0), stop=(dc == 7)),
                          [h1T, Wr], [pr])
                kb.op("dve", lambda e: e.tensor_tensor(out=lgt[:], in0=pr[:, 0:NEXP], in1=br[:], op=ALU.add), [pr, br], [lgt])
                kb.op("dve", lambda e: e.max(out=m8[:], in_=lgt[:]), [lgt], [m8])
                kb.op("dve", lambda e: e.tensor_scalar(out=MK[:, ot, :], in0=lgt[:], scalar1=m8[:, 3:4], scalar2=None, op0=ALU.is_ge),
                      [lgt, m8], [MK])
                kb.op("dve", lambda e: e.tensor_scalar(out=sm[:, 0:1], in0=m8[:, 0:1], scalar1=-1.0, scalar2=None, op0=ALU.mult), [m8], [sm])
                kb.op("act", lambda e: e.activation(out=ex[:], in_=lgt[:], func=AF.Exp, bias=sm[:, 0:1]), [lgt, sm], [ex])
                kb.op("dve", lambda e: e.scalar_tensor_tensor(out=ex[:], in0=ex[:], scalar=1.0, in1=MK[:, ot, :], op0=ALU.mult, op1=ALU.mult,
                                                              accum_out=sm[:, 1:2]), [ex, MK], [ex, sm])
                kb.op("dve", lambda e: e.reciprocal(out=sm[:, 2:3], in_=sm[:, 1:2]), [sm], [sm])
                kb.op("dve", lambda e: e.tensor_scalar(out=gts[:], in0=ex[:], scalar1=sm[:, 2:3], scalar2=None, op0=ALU.mult), [ex, sm], [gts])
                kb.op("pe", lambda e: e.matmul(pp[:, 0:NEXP], c_us[:], MK[:, ot, :], start=True, stop=(ot == 0)), [c_us, MK], [pp])
                for o2 in range(ot):
                    kb.op("pe", lambda e: e.matmul(pp[:, 0:NEXP], c_on[:], MK[:, o2, :], start=False, stop=(o2 == ot - 1)), [c_on, MK], [pp])
                kb.op("dve", lambda e: e.tensor_scalar(out=junk[:], in0=pp[:, 0:NEXP], scalar1=float(CAP) - 0.5, scalar2=None, op0=ALU.is_lt),
                      [pp], [junk])
                kb.op("dve", lambda e: e.tensor_tensor(out=junk[:], in0=junk[:], in1=MK[:, ot, :], op=ALU.mult), [junk, MK], [junk])
                kb.op("dve", lambda e: e.tensor_tensor(out=slf[:], in0=pp[:, 0:NEXP], in1=c_eo[:], op=ALU.add), [pp, c_eo], [slf])
                kb.op("dve", lambda e: e.tensor_scalar(out=slf[:], in0=slf[:], scalar1=-1.0e6, scalar2=None, op0=ALU.add), [slf], [slf])
                kb.op("dve", lambda e: e.tensor_tensor(out=slf[:], in0=slf[:], in1=junk[:], op=ALU.mult), [slf, junk], [slf])
                kb.op("dve", lambda e: e.tensor_scalar(out=slf[:], in0=slf[:], scalar1=1.0e6, scalar2=None, op0=ALU.add), [slf], [slf])
                for k in range(4):
                    kb.op("dve", lambda e: e.scalar_tensor_tensor(out=junk[:], in0=lgt[:], scalar=m8[:, k:k + 1], in1=slf[:],
                                                                  op0=ALU.is_equal, op1=ALU.mult, accum_out=slk[:, k:k + 1]),
                          [lgt, m8, slf], [junk, slk])
                    kb.op("dve", lambda e: e.scalar_tensor_tensor(out=junk[:], in0=lgt[:], scalar=m8[:, k:k + 1], in1=gts[:],
                                                                  op0=ALU.is_equal, op1=ALU.mult, accum_out=GK[:, ot, k:k + 1]),
                          [lgt, m8, gts], [junk, GK])
                kb.op("dve", lambda e: e.tensor_copy(out=SLI[:, ot, :], in_=slk[:]), [slk], [SLI])
                for k in range(4):
                    kb.dma("pool", None, None, reads=[h1, SLI], indirect=lambda g_: g_.indirect_dma_start(
                        out=xs_d, out_offset=bass.IndirectOffsetOnAxis(ap=SLI[:, ot, k:k + 1], axis=0), in_=h1[:], in_offset=None,
                        bounds_check=bc_reg, oob_is_err=False))
        fence()
        if upto == "E":
            if "h1" in dbg:
                kb.dma("sp", dbg["h1"], h1_d)
            finish()
            nc.used_inputs = used_inputs
            return nc
        barrier()
        with ExitStack() as es:
            NST = CAP // 128
            WS = [kb.sb(es, [128, 8, 512], F32, "WS%d" % k) for k in range(3)]
            WB = [kb.sb(es, [128, 8, 512], F32R, "WB%d" % k) for k in range(4)]
            xsb = [kb.sb(es, [128, NST, D], F32, "xsb") for _ in range(2)]
            xT = kb.sb(es, [128, 8, CAP], F32R, "xTe")
            actT = kb.sb(es, [128, 8, CAP], F32R, "actT")
            bgu = kb.sb(es, [128, NEXP, 16], F32, "bgu")
            kb.dma("sp", bgu[:], bgu_col, writes=[bgu])
            bd = [kb.sb(es, [128, D], F32, "bd") for _ in range(2)]
            gs = [kb.sb(es, [128, CAP], F32, "gs") for _ in range(2)]
            sg = [kb.sb(es, [128, CAP], F32, "sg") for _ in range(2)]
            us = [kb.sb(es, [128, CAP], F32, "us") for _ in range(2)]
            yt = [kb.sb(es, [128, D], F32, "yt") for _ in range(2)]
            pg_ = [kb.ps(es, [128, 512], F32, "pgF") for _ in range(2)]
            pu_ = [kb.ps(es, [128, 512], F32, "puF") for _ in range(2)]
            py_ = [kb.ps(es, [128, 512], F32, "pyF") for _ in range(2)]
            ptx = [kb.ps(es, [128, 512], F32, "ptxF") for _ in range(2)]
            wcnt = [0]

            def wload(src_ap):
                k = wcnt[0]
                wcnt[0] += 1
                st_, t = WS[k % 3], WB[k % 4]
                kb.dma("sp", st_[:], _f(src_ap).rearrange("(c p) n -> p c n", p=128), writes=[st_])
                if k % 2 == 0:
                    kb.op("act", lambda e: e.activation(out=t[:], in_=st_[:], func=AF.Identity), [st_], [t])
                else:
                    kb.op("dve", lambda e: e.tensor_copy(out=t[:], in_=st_[:]), [st_], [t])
                return t

            for ex_ in range(NEXP):
                xs = xsb[ex_ % 2]
                kb.dma("sp", xs[:], xs_d[ex_ * CAP:(ex_ + 1) * CAP, :].rearrange("(k p) n -> p k n", p=128), writes=[xs])
                bdt = bd[ex_ % 2]
                kb.dma("sp", bdt[:], b_down[ex_, :].partition_broadcast(128), writes=[bdt])
                for dc in range(8):
                    px = ptx[dc % 2]
                    for st in range(NST):
                        kb.op("pe", lambda e: e.transpose(out=px[:, st * 128:(st + 1) * 128], in_=xs[:, st, dc * 128:(dc + 1) * 128],
                                                          identity=c_ident[:]), [xs, c_ident], [px])
                    if dc % 2 == 0:
                        kb.op("act", lambda e: e.activation(out=xT[:, dc, :], in_=px[:, 0:CAP], func=AF.Identity), [px], [xT])
                    else:
                        kb.op("dve", lambda e: e.tensor_copy(out=xT[:, dc, :], in_=px[:, 0:CAP]), [px], [xT])
                for hf in range(2):
                    wg_ = wload(w_gate[ex_, :, hf * 512:(hf + 1) * 512])
                    wu_ = wload(w_up[ex_, :, hf * 512:(hf + 1) * 512])
                    for f4 in range(4):
                        fc = hf * 4 + f4
                        par = fc % 2
                        for dc in range(8):
                            kb.op("pe", lambda e: e.matmul(pg_[par][:, 0:CAP], wg_[:, dc, f4 * 128:(f4 + 1) * 128], xT[:, dc, :],
                                                           start=(dc == 0), stop=(dc == 7)), [wg_, xT], [pg_[par]])
                        for dc in range(8):
                            kb.op("pe", lambda e: e.matmul(pu_[par][:, 0:CAP], wu_[:, dc, f4 * 128:(f4 + 1) * 128], xT[:, dc, :],
                                                           start=(dc == 0), stop=(dc == 7)), [wu_, xT], [pu_[par]])
                        kb.op("dve", lambda e: e.tensor_scalar(out=gs[par][:], in0=pg_[par][:, 0:CAP], scalar1=bgu[:, ex_, fc:fc + 1],
                                                              scalar2=7.0, op0=ALU.add, op1=ALU.min), [pg_[par], bgu], [gs[par]])
                        kb.op("act", lambda e: e.activation(out=sg[par][:], in_=gs[par][:], func=AF.Sigmoid, scale=1.702),
                              [gs[par]], [sg[par]])
                        kb.op("dve", lambda e: e.tensor_scalar(out=us[par][:], in0=pu_[par][:, 0:CAP], scalar1=bgu[:, ex_, 8 + fc:9 + fc],
                                                              scalar2=7.0, op0=ALU.add, op1=ALU.min), [pu_[par], bgu], [us[par]])
                        kb.op("dve", lambda e: e.tensor_scalar(out=us[par][:], in0=us[par][:], scalar1=-7.0, scalar2=1.0,
                                                              op0=ALU.max, op1=ALU.add), [us[par]], [us[par]])
                        kb.op("pool", lambda e: e.tensor_tensor(out=gs[par][:], in0=gs[par][:], in1=sg[par][:], op=ALU.mult),
                              [gs[par], sg[par]], [gs[par]])
                        kb.op("pool", lambda e: e.tensor_tensor(out=actT[:, fc, :], in0=gs[par][:], in1=us[par][:], op=ALU.mult),
                              [gs[par], us[par]], [actT])
                wd = [wload(w_down[ex_, :, hf * 512:(hf + 1) * 512]) for hf in range(2)]
                for st in range(NST):
                    y = yt[st % 2]
                    for hf in range(2):
                        for fc in range(8):
                            kb.op("pe", lambda e: e.matmul(py_[hf][:], actT[:, fc, st * 128:(st + 1) * 128], wd[hf][:, fc, :],
                                                           start=(fc == 0), stop=(fc == 7)), [actT, wd[hf]], [py_[hf]])
                        kb.op("dve", lambda e: e.tensor_tensor(out=y[:, hf * 512:(hf + 1) * 512], in0=py_[hf][:],
                                                               in1=bdt[:, hf * 512:(hf + 1) * 512], op=ALU.add), [py_[hf], bdt], [y])
                    kb.dma("sp", ys_d[ex_ * CAP + st * 128: ex_ * CAP + (st + 1) * 128, :], y[:], reads=[y])
        fence()
        barrier()
        with ExitStack() as es:
            g2, b2 = [bcast_load(es, vecs[k, :], D, "lnv%d" % k) for k in (4, 5)]
            yks = [[kb.sb(es, [128, D], F32, "yk%d" % k) for k in range(4)] for _ in range(2)]
            h1ts = [kb.sb(es, [128, D], F32, "h1t") for _ in range(2)]
            acc = kb.sb(es, [128, D], F32, "accG")
            ot_ = kb.sb(es, [128, D], F32, "outG")
            stp = (kb.sb(es, [128, 2, 6], F32, "stG"), kb.sb(es, [128, 4], F32, "mvG"))
            for ot in range(NOT):
                yk, h1t = yks[ot % 2], h1ts[ot % 2]
                kb.dma("pool", h1t[:], h1_d[ot * 128:(ot + 1) * 128, :], writes=[h1t])
                for k in range(4):
                    kb.op("pool", lambda e: e.memset(yk[k][:], 0.0), [], [yk[k]])
                    kb.dma("pool", None, None, reads=[SLI], writes=[yk[k]], indirect=lambda g_: g_.indirect_dma_start(
                        out=yk[k][:], out_offset=None, in_=ys_d, in_offset=bass.IndirectOffsetOnAxis(ap=SLI[:, ot, k:k + 1], axis=0),
                        bounds_check=bc_reg, oob_is_err=False))
                kb.op("dve", lambda e: e.tensor_scalar(out=acc[:], in0=h1t[:], scalar1=float(ALPHA), scalar2=None, op0=ALU.mult),
                      [h1t], [acc])
                for k in range(4):
                    kb.op("dve", lambda e: e.scalar_tensor_tensor(out=acc[:], in0=yk[k][:], scalar=GK[:, ot, k:k + 1], in1=acc[:],
                                                                  op0=ALU.mult, op1=ALU.add), [yk[k], GK, acc], [acc])
                ln_full(acc, stp, g2, b2, ot_)
                kb.dma("sp", out[ot * 128:(ot + 1) * 128, :], ot_[:], reads=[ot_])
        finish()
    nc.used_inputs = used_inputs
    return nc


def _own_idx(T, j):
    c = np.arange(T // 64)[:, None] * 64 + 16 * j + np.arange(16)[None, :]
    return c.reshape(-1)


def _col(v, n):
    return np.ascontiguousarray(v.reshape(n, 128).T)


def prep(inputs, T):
    f = np.float32
    x = np.asarray(inputs["x"], f)
    B = x.shape[0]
    w_in = np.ascontiguousarray(np.asarray(inputs["w_in"], f)[0])
    b_in = np.asarray(inputs["b_in"], f)[0]
    conv_w = np.asarray(inputs["conv_w"], f)[0]
    conv_b = np.asarray(inputs["conv_b"], f)[0]
    cols = np.zeros((128, 128), f)
    cols[:, 0:8] = _col(np.asarray(inputs["ln_in_g"], f), 8)
    cols[:, 8:16] = _col(np.asarray(inputs["ln_in_b"], f), 8)
    cols[:, 16:32] = _col(b_in[0:2048], 16)
    cols[:, 32:48] = _col(conv_b, 16)
    conv_wc = np.ascontiguousarray(conv_w.reshape(4, 16, 128).transpose(2, 1, 0))
    pos = np.arange(T, dtype=np.float64)

    def rope_tab(half):
        inv = 10000.0 ** (-np.arange(half, dtype=np.float64) / half)
        ang = (pos[:, None].astype(f) * inv[None, :].astype(f)).astype(f)
        return np.stack([np.cos(ang), np.sin(ang)], axis=1).astype(f)

    ra, ri = rope_tab(64), rope_tab(32)
    p = np.arange(128)
    p64 = np.arange(64)
    tri64 = (p64[:, None] <= p64[None, :]).astype(f)
    ident = np.eye(128, dtype=f)
    pw2 = np.tile((2.0 ** -(np.arange(NBIS) + 1.0)).astype(f)[None, :], (128, 1))
    ustri = (p[:, None] < p[None, :]).astype(f)
    eoff = np.tile((np.arange(NEXP) * CAP).astype(f)[None, :], (128, 1))
    vecs = np.stack([np.asarray(inputs[k], f).reshape(-1) for k in
                     ("ln_in_g", "ln_in_b", "ln1_g", "ln1_b", "ln2_g", "ln2_b")])
    bg = np.asarray(inputs["b_gate"], f)[0]
    bu = np.asarray(inputs["b_up"], f)[0]
    bgu = np.zeros((128, NEXP, 16), f)
    for e_ in range(NEXP):
        bgu[:, e_, 0:8] = _col(bg[e_], 8)
        bgu[:, e_, 8:16] = _col(bu[e_], 8)
    m = np.arange(128)[:, None] // 16
    s = np.arange(512)[None, :] // 64
    admb = np.where(s <= m, 0.0, -3.0e38).astype(f)
    shared = dict(w_in=w_in, b_in=b_in[None, :], cols=cols, conv_wc=conv_wc, rope_a=ra, rope_i=ri, tri64=tri64,
                  ident=ident, pw2=pw2, admb=admb, m_norm_g=np.asarray(inputs["m_norm_g"], f).reshape(1, D),
                  w_out=np.ascontiguousarray(np.asarray(inputs["w_out"], f)[0]), vecs=vecs,
                  w_router=np.ascontiguousarray(np.asarray(inputs["w_router"], f)[0]),
                  b_router=np.asarray(inputs["b_router"], f).reshape(1, NEXP),
                  w_gate=np.ascontiguousarray(np.asarray(inputs["w_gate"], f)[0]),
                  w_up=np.ascontiguousarray(np.asarray(inputs["w_up"], f)[0]),
                  w_down=np.ascontiguousarray(np.asarray(inputs["w_down"], f)[0]),
                  bgu_col=bgu, b_down=np.ascontiguousarray(np.asarray(inputs["b_down"], f)[0]),
                  eoff=eoff, ustri=ustri)
    maps = []
    for c in range(8):
        b, j = (c // 4) % B, c % 4
        oi = _own_idx(T, j)
        sel = np.zeros((128, 32), f)
        trisel = np.zeros((8, 64, 128), f)
        for ch in range(2):
            for r in range(16):
                sel[ch * 64 + 16 * j + r, ch * 16 + r] = 1.0 / 16.0
        for cp in range(8):
            for r in range(16):
                trisel[cp, 0:16 * j + r + 1, cp * 16 + r] = 1.0
        cmask = (np.arange(64)[:, None] <= (16 * j + np.arange(16))[None, :]).astype(f)
        d = dict(shared)
        d.update(x_all=np.ascontiguousarray(x[b, :T]), x_own=np.ascontiguousarray(x[b, oi]),
                 rope_ao=np.ascontiguousarray(ra[oi]), rope_io=np.ascontiguousarray(ri[oi]),
                 sel=sel, trisel=trisel, cmask=cmask)
        maps.append(d)
    return maps


def assemble(results, T, B):
    out = np.zeros((B, T, D), np.float32)
    for c in range(8):
        b, j = c // 4, c % 4
        if b < B:
            out[b, _own_idx(T, j)] = results[c]["out"]
    return out


_NC_CACHE = {}


def kernel(**inputs):
    T = inputs["x"].shape[1]
    B = inputs["x"].shape[0]
    if T not in _NC_CACHE:
        _NC_CACHE[T] = build(T)
    nc = _NC_CACHE[T]
    maps = [{k: m[k] for k in nc.used_inputs} for m in prep(inputs, T)]
    res = run_bass_kernel_spmd(nc, maps, core_ids=list(range(8)))
    return assemble(res.results, T, B)
```

```python
import numpy as np
from contextlib import ExitStack
import concourse.bass as bass
import concourse.mybir as mybir
from concourse.bass_utils import run_bass_kernel_spmd

F32 = mybir.dt.float32
F32R = mybir.dt.float32r
BF16 = mybir.dt.bfloat16
I32 = mybir.dt.int32
U32 = mybir.dt.uint32
AF = mybir.ActivationFunctionType
ALU = mybir.AluOpType
AX = mybir.AxisListType

D = 1024
INW = 9808
NEXP = 32
CAP = 384
NBIS = 22
NDMASEM = 40
LN_EPS = 1e-5
ALPHA = 2.0 ** 0.25

O_MQ, O_MK, O_MV, O_MO, O_MI, O_MF = 0, 1024, 2048, 3072, 4096, 4100
O_AQ, O_AK, O_AV, O_XQ, O_XK, O_XW, O_GM, O_GA = 4104, 5128, 6152, 7176, 7688, 7752, 7760, 8784


class Buf:
    __slots__ = ("w", "r")

    def __init__(self):
        self.w = {}
        self.r = {}


class Tl:
    def __init__(self, t):
        self.t = t
        self.b = Buf()

    def __getitem__(self, k):
        return self.t[k]


class KB:
    def __init__(self, nc, es):
        self.nc = nc
        self.engs = {"pe": nc.tensor, "act": nc.scalar, "dve": nc.vector, "pool": nc.gpsimd, "sp": nc.sync}
        self.sem = {k: es.enter_context(nc.semaphore("sem_" + k)) for k in self.engs}
        self.cnt = {k: 0 for k in self.engs}
        self.seen = {}
        self.dsem = [es.enter_context(nc.semaphore("dq%d" % i)) for i in range(NDMASEM)]
        self.dval = [0] * NDMASEM
        self.qpool = {"sp": list(range(0, 20)), "pool": list(range(20, 36)), "act": list(range(36, 40))}
        self.qrr = {"sp": 0, "pool": 0, "act": 0}
        self.uid = 0

    def sb(self, es, shape, dt, name=None):
        self.uid += 1
        return Tl(es.enter_context(self.nc.sbuf_tensor("%s_%d" % (name or "t", self.uid), list(shape), dt)))

    def ps(self, es, shape, dt=F32, name=None):
        self.uid += 1
        return Tl(es.enter_context(self.nc.psum_tensor("%s_%d" % (name or "p", self.uid), list(shape), dt)))

    def _wait(self, e, tok):
        sem, val, key = tok
        if self.seen.get((e, key), 0) >= val:
            return
        self.seen[(e, key)] = val
        self.engs[e].wait_ge(sem, val)

    def _deps(self, e, reads, writes):
        toks = []
        for b in reads:
            toks.extend(b.b.w.values())
        for b in writes:
            toks.extend(b.b.w.values())
            toks.extend(b.b.r.values())
        for t in toks:
            if t[2] == "pe" and e == "pe":
                continue
            self._wait(e, t)

    def _mark(self, tok, reads, writes):
        for b in reads:
            b.b.r[tok[2]] = tok
        for b in writes:
            b.b.w[tok[2]] = tok
            b.b.r = {}

    def op(self, e, fn, reads=(), writes=()):
        self._deps(e, reads, writes)
        ins = fn(self.engs[e])
        self.cnt[e] += 1
        ins.then_inc(self.sem[e], 1)
        self._mark((self.sem[e], self.cnt[e], e), reads, writes)

    def dma(self, q, out, in_, reads=(), writes=(), indirect=None):
        pl = self.qpool[q]
        i = pl[self.qrr[q] % len(pl)]
        self.qrr[q] += 1
        key = "d%d" % i
        if self.dval[i] > 0:
            self._wait(q, (self.dsem[i], self.dval[i], key))
        self._deps(q, reads, writes)
        if indirect is None:
            ins = self.engs[q].dma_start(out=out, in_=in_)
        else:
            ins = indirect(self.engs[q])
        self.dval[i] += 16
        ins.then_inc(self.dsem[i], 16)
        self._mark((self.dsem[i], self.dval[i], key), reads, writes)

    def barrier_all(self, bufs):
        for e in self.engs:
            self._deps(e, bufs, ())


def _r(ap):
    return ap.bitcast(F32R)


def _f(ap):
    return ap.bitcast(F32)


def build(T, debug=None, upto="all"):
    NT = T // 128
    NG = T // 512
    NO = T // 4
    NOT = NO // 128
    NCH = T // 64
    nc = bass.Bass("TRN2", target_bir_lowering=False)

    used_inputs = []

    def din(name, shape, dt=F32):
        used_inputs.append(name)
        return nc.dram_tensor(name, list(shape), dt, kind="ExternalInput").ap()

    def dscr(name, shape, dt=F32):
        return nc.dram_tensor(name, list(shape), dt, kind="Internal").ap()

    x_all = din("x_all", [T, D])
    x_own = din("x_own", [NO, D])
    w_in = din("w_in", [D, INW], F32R)
    b_in = din("b_in", [1, INW])
    cols = din("cols", [128, 128])
    conv_wc = din("conv_wc", [128, 16, 4])
    rope_a = din("rope_a", [T, 2, 64])
    rope_i = din("rope_i", [T, 2, 32])
    rope_ao = din("rope_ao", [NO, 2, 64])
    rope_io = din("rope_io", [NO, 2, 32])
    sel = din("sel", [128, 32], F32R)
    trisel = din("trisel", [8, 64, 128])
    tri64 = din("tri64", [64, 64])
    cmask = din("cmask", [64, 16])
    ident = din("ident", [128, 128])
    pw2 = din("pw2", [128, NBIS])
    admb = din("admb", [128, 512])
    m_norm_g = din("m_norm_g", [1, D])
    w_out = din("w_out", [D, D], F32R)
    vecs = din("vecs", [6, D])
    w_router = din("w_router", [D, NEXP])
    b_router = din("b_router", [1, NEXP])
    if upto == "all":
        w_gate = din("w_gate", [NEXP, D, D], F32R)
        w_up = din("w_up", [NEXP, D, D], F32R)
        w_down = din("w_down", [NEXP, D, D], F32R)
    bgu_col = din("bgu_col", [128, NEXP, 16])
    b_down = din("b_down", [NEXP, D])
    eoff = din("eoff", [128, NEXP])
    ustri = din("ustri", [128, 128])
    out = nc.dram_tensor("out", [NO, D], F32, kind="ExternalOutput").ap()

    xnT_d = dscr("xnT_d", [8, 128, T])
    xnTo_d = dscr("xnTo_d", [8, 128, NO])
    kT_d = dscr("kT_d", [8, 128, T])
    ktok_d = dscr("ktok_d", [T, D])
    qTo_d = dscr("qTo_d", [8, 128, NO])
    v_d = dscr("v_d", [T, D])
    ikT_d = dscr("ikT_d", [64, T])
    g_d = dscr("g_d", [T, 8])
    mg_d = dscr("mg_d", [NO, D])
    akT_d = dscr("akT_d", [8, 128, T], BF16)
    av_d = dscr("av_d", [8, 128, T // 128, 130], BF16)
    aqT_d = dscr("aqT_d", [NOT, 128, 8, 128], BF16)
    iqT_d = dscr("iqT_d", [NOT, 64, 8, 128])
    og_d = dscr("og_d", [NO, D])
    ga_d = dscr("ga_d", [NO, D])
    ym_d = dscr("ym_d", [NO, D])
    h1_d = dscr("h1_d", [NO, D])
    xs_d = dscr("xs_d", [NEXP * CAP, D])
    ys_d = dscr("ys_d", [NEXP * CAP, D])
    dbg = {}
    if debug:
        for nm, shp in debug.items():
            dbg[nm] = nc.dram_tensor("dbg_" + nm, list(shp), F32, kind="ExternalOutput").ap()

    with ExitStack() as es0:
        kb = KB(nc, es0)
        V = nc.vector
        c_cols = kb.sb(es0, [128, 128], F32, "cols")
        c_ident = kb.sb(es0, [128, 128], F32, "ident")
        c_identb = kb.sb(es0, [128, 128], BF16, "identb")
        kb.dma("sp", c_cols[:], cols, writes=[c_cols])
        kb.dma("sp", c_ident[:], ident, writes=[c_ident])
        kb.op("dve", lambda e: e.tensor_copy(out=c_identb[:], in_=c_ident[:]), [c_ident], [c_identb])
        GCOL, BCOL, BQK, CBC = 0, 8, 16, 32

        def layernorm_stats(es, xt, tmp_pool):
            st, mv = tmp_pool
            kb.op("dve", lambda e: e.bn_stats(out=st[:, 0, :], in_=xt[:, 0:512]), [xt], [st])
            kb.op("dve", lambda e: e.bn_stats(out=st[:, 1, :], in_=xt[:, 512:1024]), [xt], [st])
            kb.op("dve", lambda e: e.bn_aggr(out=mv[:, 0:2], in_=st[:].rearrange("p a b -> p (a b)")), [st], [mv])
            kb.op("dve", lambda e: e.tensor_scalar(out=mv[:, 2:3], in0=mv[:, 1:2], scalar1=LN_EPS, scalar2=None,
                                                  op0=ALU.add), [mv], [mv])
            kb.op("act", lambda e: e.activation(out=mv[:, 2:3], in_=mv[:, 2:3], func=AF.Sqrt), [mv], [mv])
            kb.op("dve", lambda e: e.reciprocal(out=mv[:, 3:4], in_=mv[:, 2:3]), [mv], [mv])
            return mv[:, 0:1], mv[:, 3:4]

        def phase_ln_T(xsrc, ntiles, dst):
            with ExitStack() as es:
                xts = [kb.sb(es, [128, D], F32, "xt") for _ in range(3)]
                xhs = [kb.sb(es, [128, D], F32, "xh") for _ in range(2)]
                sts = [(kb.sb(es, [128, 2, 6], F32, "st"), kb.sb(es, [128, 4], F32, "mv")) for _ in range(2)]
                xns = [kb.sb(es, [128, 8, 512], F32, "xn") for _ in range(2)]
                pts = [kb.ps(es, [128, 512], F32, "pt") for _ in range(4)]
                for tt in range(ntiles):
                    xt, xh, stp, xn = xts[tt % 3], xhs[tt % 2], sts[tt % 2], xns[(tt // 4) % 2]
                    t4 = tt % 4
                    kb.dma("pool", xt[:], xsrc[tt * 128:(tt + 1) * 128, :], writes=[xt])
                    mean, rstd = layernorm_stats(es, xt, stp)
                    kb.op("dve", lambda e: e.tensor_scalar(out=xh[:], in0=xt[:], scalar1=mean, scalar2=rstd,
                                                          op0=ALU.subtract, op1=ALU.mult), [xt, stp[1]], [xh])
                    for half in range(2):
                        pt = pts[(tt % 2) * 2 + half]
                        for k in range(4):
                            dc = half * 4 + k
                            kb.op("pe", lambda e: e.transpose(out=pt[:, k * 128:(k + 1) * 128],
                                                              in_=xh[:, dc * 128:(dc + 1) * 128], identity=c_ident[:]),
                                  [xh, c_ident], [pt])
                        for k in range(4):
                            dc = half * 4 + k
                            kb.op("act", lambda e: e.activation(out=xn[:, dc, t4 * 128:(t4 + 1) * 128], in_=pt[:, k * 128:(k + 1) * 128],
                                                                func=AF.Identity, scale=c_cols[:, GCOL + dc:GCOL + dc + 1],
                                                                bias=c_cols[:, BCOL + dc:BCOL + dc + 1]),
                                  [pt, c_cols], [xn])
                    if t4 == 3:
                        g4 = tt // 4
                        kb.dma("sp", dst[:, :, g4 * 512:(g4 + 1) * 512].rearrange("c p t -> p c t"), xn[:], reads=[xn])

        def fence():
            for e in kb.engs:
                for i in range(NDMASEM):
                    if kb.dval[i] > 0:
                        kb._wait(e, (kb.dsem[i], kb.dval[i], "d%d" % i))

        def barrier():
            fence()
            for e in kb.engs:
                for e2 in kb.engs:
                    if kb.cnt[e2] > 0 and e2 != e:
                        kb._wait(e, (kb.sem[e2], kb.cnt[e2], e2))

        phase_ln_T(x_all, NT, xnT_d)
        barrier()
        phase_ln_T(x_own, NOT, xnTo_d)

        barrier()

        def w_blocks(W, ncols, specs):
            nb = (ncols + 511) // 512
            Wb = [Tl(None) for _ in range(nb)]
            pieces = []
            for (src0, n, dst0) in specs:
                o = 0
                while o < n:
                    d = dst0 + o
                    m = min(512 - (d % 512), n - o)
                    pieces.append((src0 + o, m, d))
                    o += m
            pieces.sort(key=lambda t: t[2])

            def emit(sel_):
                for (s0, m, d0) in pieces:
                    if sel_(d0 // 512):
                        kb.dma("pool", W[:, :, d0:d0 + m], w_in[:, s0:s0 + m].rearrange("(c p) n -> p c n", p=128),
                               writes=[Wb[d0 // 512]])
            return Wb, (lambda: emit(lambda b_: b_ == 0)), (lambda: emit(lambda b_: b_ > 0))

        with ExitStack() as es:
            W = kb.sb(es, [128, 8, 2048], F32R, "Wqk")
            Wb, w_first, w_rest = w_blocks(W, 2048, [(0, 2048, 0)])
            w_first()
            c_cw = kb.sb(es, [128, 16, 4], F32, "cw")
            kb.dma("sp", c_cw[:], conv_wc, writes=[c_cw])
            c_sel = kb.sb(es, [128, 32], F32R, "sel")
            kb.dma("pool", c_sel[:], sel, writes=[c_sel])
            xgs = [kb.sb(es, [128, 8, 512], F32R, "xg") for _ in range(2)]
            pre = kb.sb(es, [128, 16, 516], F32, "pre")
            preb = [Tl(None) for _ in range(16)]
            kb.op("dve", lambda e: e.memset(pre[:], 0.0), [], [pre] + preb)
            accs = [kb.sb(es, [128, 512], F32, "acc") for _ in range(2)]
            sqs = [kb.sb(es, [128, 512], F32, "sq") for _ in range(2)]
            qtok = kb.sb(es, [128, 4, D], F32R, "qtok")
            ktok = kb.sb(es, [128, 4, D], F32, "ktok")
            qTo = kb.sb(es, [128, 8, 128], F32, "qTo")
            pms = [kb.ps(es, [128, 512], F32, "pm") for _ in range(3)]
            ptr = [kb.ps(es, [128, 512], F32, "ptr") for _ in range(2)]
            psels = [kb.ps(es, [128, 512], F32, "psel") for _ in range(2)]
            pend_a1 = []
            for g in range(NG):
                xg = xgs[g % 2]
                kb.dma("pool", xg[:], _r(xnT_d[:, :, g * 512:(g + 1) * 512].rearrange("c p t -> p c t")), writes=[xg])
                if g == 0:
                    w_rest()
                pend_silu = []

                def emit_silu(cc):
                    acc, sq = accs[cc % 2], sqs[cc % 2]
                    kb.op("act", lambda e: e.activation(out=sq[:], in_=acc[:], func=AF.Silu), [acc], [sq])
                    if cc >= 8:
                        kb.dma("sp", kT_d[cc - 8, :, g * 512:(g + 1) * 512], sq[:], reads=[sq])

                    def tail(cc=cc, sq=sq):
                        pt = ptr[cc % 2]
                        for tk in range(4):
                            kb.op("pe", lambda e: e.transpose(out=pt[:, tk * 128:(tk + 1) * 128],
                                                              in_=sq[:, tk * 128:(tk + 1) * 128], identity=c_ident[:]),
                                  [sq, c_ident], [pt])
                        dst = qtok if cc < 8 else ktok
                        c8 = cc % 8
                        kb.op("dve", lambda e: e.tensor_copy(out=dst[:, :, c8 * 128:(c8 + 1) * 128],
                                                             in_=pt[:].rearrange("p (k c) -> p k c", k=4)), [pt], [dst])
                    pend_a1.append(tail)

                for cc in range(16):
                    pm = pms[cc % 3]
                    acc = accs[cc % 2]
                    pb = preb[cc]
                    for dc in range(8):
                        kb.op("pe", lambda e: e.matmul(pm[:], W[:, dc, cc * 128:(cc + 1) * 128], xg[:, dc, :],
                                                       start=(dc == 0), stop=(dc == 7)), [Wb[cc // 4], xg], [pm])
                    while pend_a1:
                        pend_a1.pop(0)()
                    kb.op("act", lambda e: e.activation(out=pre[:, cc, 0:4], in_=pre[:, cc, 512:516], func=AF.Identity),
                          [pb], [pb])
                    kb.op("act", lambda e: e.activation(out=pre[:, cc, 4:516], in_=pm[:], func=AF.Identity,
                                                        bias=c_cols[:, BQK + cc:BQK + cc + 1]), [pm, c_cols], [pb])
                    kb.op("act", lambda e: e.activation(out=acc[:], in_=pre[:, cc, 4:516], func=AF.Identity,
                                                        scale=c_cw[:, cc, 3:4], bias=c_cols[:, CBC + cc:CBC + cc + 1]),
                          [pb, c_cw, c_cols], [acc])
                    for j in range(3):
                        kb.op("dve", lambda e: e.scalar_tensor_tensor(out=acc[:], in0=pre[:, cc, 1 + j:513 + j],
                                                                      scalar=c_cw[:, cc, j:j + 1], in1=acc[:],
                                                                      op0=ALU.mult, op1=ALU.add), [pb, c_cw, acc], [acc])
                    while pend_silu:
                        emit_silu(pend_silu.pop(0))
                    pend_silu.append(cc)
                while pend_silu:
                    emit_silu(pend_silu.pop(0))
                while pend_a1:
                    pend_a1.pop(0)()
                kb.dma("sp", ktok_d[g * 512:(g + 1) * 512, :].rearrange("(k p) n -> p k n", p=128), ktok[:], reads=[ktok])
                for tk in range(4):
                    psl = psels[tk % 2]
                    for c8 in range(8):
                        kb.op("pe", lambda e: e.matmul(psl[:, c8 * 32:(c8 + 1) * 32], qtok[:, tk, c8 * 128:(c8 + 1) * 128],
                                                       c_sel[:], start=True, stop=True), [qtok, c_sel], [psl])
                    kb.op("act", lambda e: e.activation(out=qTo[:, :, tk * 32:(tk + 1) * 32],
                                                        in_=psl[:, 0:256].rearrange("p (c m) -> p c m", c=8),
                                                        func=AF.Identity), [psl], [qTo])
                kb.dma("sp", qTo_d[:, :, g * 128:(g + 1) * 128].rearrange("c p t -> p c t"), qTo[:], reads=[qTo])
        fence()
        def finish():
            fence()
            for e in kb.engs:
                for e2 in kb.engs:
                    if kb.cnt[e2] > 0:
                        kb._wait(e, (kb.sem[e2], kb.cnt[e2], e2))

        if upto == "A":
            for nm, src in (("kT", kT_d[:, :, :].rearrange("c p t -> (c p) t")), ("qTo", qTo_d[:, :, :].rearrange("c p t -> (c p) t")),
                            ("ktok", ktok_d)):
                if nm in dbg:
                    kb.dma("sp", dbg[nm], src)
            finish()
            nc.used_inputs = used_inputs
            return nc
        def bcast_load(es, src_row_ap, n, name):
            t = kb.sb(es, [128, n], F32, name)
            kb.dma("sp", t[:], src_row_ap.partition_broadcast(128), writes=[t])
            return t

        def rope(es, src, dst, cos, sin, nh, half, tmp):
            shp = [128, nh, half]
            cb = cos.unsqueeze(1).to_broadcast(shp)
            sb_ = sin.unsqueeze(1).to_broadcast(shp)
            x1, x2 = src[0], src[1]
            t = tmp
            kb.op("dve", lambda e: e.tensor_tensor(out=t[:, :, 0:half], in0=x1, in1=cb, op=ALU.mult), src[2], [t])
            kb.op("dve", lambda e: e.tensor_tensor(out=t[:, :, half:2 * half], in0=x2, in1=sb_, op=ALU.mult), src[2], [t])
            kb.op("dve", lambda e: e.tensor_tensor(out=dst[0], in0=t[:, :, 0:half], in1=t[:, :, half:2 * half],
                                                   op=ALU.subtract), [t], dst[2])
            kb.op("dve", lambda e: e.tensor_tensor(out=t[:, :, 0:half], in0=x2, in1=cb, op=ALU.mult), src[2], [t])
            kb.op("dve", lambda e: e.tensor_tensor(out=t[:, :, half:2 * half], in0=x1, in1=sb_, op=ALU.mult), src[2], [t])
            kb.op("dve", lambda e: e.tensor_tensor(out=dst[1], in0=t[:, :, 0:half], in1=t[:, :, half:2 * half],
                                                   op=ALU.add), [t], dst[2])

        WQ = kb.sb(es0, [128, NOT, 8], F32, "WQ")
        barrier()
        with ExitStack() as es:
            NC2 = 1096
            W = kb.sb(es, [128, 8, NC2], F32R, "W2")
            Wb, w_first, w_rest = w_blocks(W, NC2, [(O_MV, 1024, 0), (O_MI, 8, 1024), (O_XK, 64, 1032)])
            w_first()
            bias = kb.sb(es, [128, NC2], F32, "bias2")
            kb.dma("sp", bias[:, 0:1024], b_in[0, O_MV:O_MV + 1024].partition_broadcast(128), writes=[bias])
            kb.dma("sp", bias[:, 1024:1032], b_in[0, O_MI:O_MI + 8].partition_broadcast(128), writes=[bias])
            kb.dma("sp", bias[:, 1032:1096], b_in[0, O_XK:O_XK + 64].partition_broadcast(128), writes=[bias])
            xgs = [kb.sb(es, [128, 8, 512], F32R, "xg") for _ in range(2)]
            vts = [kb.sb(es, [128, 4, D], F32, "vt") for _ in range(2)]
            gxs = [kb.sb(es, [128, 4, 72], F32, "gx") for _ in range(2)]
            rps = [kb.sb(es, [128, 4, 2, 32], F32, "rp") for _ in range(2)]
            xkr = kb.sb(es, [128, 4, 64], F32, "xkr")
            rtmp = kb.sb(es, [128, 4, 64], F32, "rtmp")
            ikg = kb.sb(es, [64, 512], F32, "ikg")
            pA = [kb.ps(es, [128, 512], F32, "pA") for _ in range(6)]
            pT = kb.ps(es, [128, 512], F32, "pT")
            for g in range(NG):
                xg, vt, gx, rp = xgs[g % 2], vts[g % 2], gxs[g % 2], rps[g % 2]
                kb.dma("pool", xg[:], _r(xnT_d[:, :, g * 512:(g + 1) * 512].rearrange("c p t -> p c t")), writes=[xg])
                if g == 0:
                    w_rest()
                kb.dma("sp", rp[:], rope_i[g * 512:(g + 1) * 512].rearrange("(k p) c d -> p k c d", p=128), writes=[rp])
                for tk in range(4):
                    ps3 = [pA[(tk % 2) * 3 + q] for q in range(3)]
                    for q, (c0, c1) in enumerate(((0, 512), (512, 1024), (1024, 1096))):
                        for dc in range(8):
                            kb.op("pe", lambda e: e.matmul(ps3[q][:, 0:c1 - c0], xg[:, dc, tk * 128:(tk + 1) * 128],
                                                           W[:, dc, c0:c1], start=(dc == 0), stop=(dc == 7)),
                                  [xg, Wb[q]], [ps3[q]])
                    kb.op("dve", lambda e: e.tensor_tensor(out=vt[:, tk, 0:512], in0=ps3[0][:], in1=bias[:, 0:512],
                                                           op=ALU.add), [ps3[0], bias], [vt])
                    kb.op("dve", lambda e: e.tensor_tensor(out=vt[:, tk, 512:1024], in0=ps3[1][:], in1=bias[:, 512:1024],
                                                           op=ALU.add), [ps3[1], bias], [vt])
                    kb.op("dve", lambda e: e.tensor_tensor(out=gx[:, tk, :], in0=ps3[2][:, 0:72], in1=bias[:, 1024:1096],
                                                           op=ALU.add), [ps3[2], bias], [gx])
                kb.dma("sp", v_d[g * 512:(g + 1) * 512, :].rearrange("(k p) n -> p k n", p=128), vt[:], reads=[vt])
                kb.dma("sp", g_d[g * 512:(g + 1) * 512, :].rearrange("(k p) n -> p k n", p=128), gx[:, :, 0:8], reads=[gx])
                for tk in range(4):
                    cosv, sinv = rp[:, tk, 0, :], rp[:, tk, 1, :]
                    x1, x2 = gx[:, tk, 8:40], gx[:, tk, 40:72]
                    kb.op("dve", lambda e: e.tensor_tensor(out=rtmp[:, tk, 0:32], in0=x1, in1=cosv, op=ALU.mult), [gx, rp], [rtmp])
                    kb.op("dve", lambda e: e.tensor_tensor(out=rtmp[:, tk, 32:64], in0=x2, in1=sinv, op=ALU.mult), [gx, rp], [rtmp])
                    kb.op("dve", lambda e: e.tensor_tensor(out=xkr[:, tk, 0:32], in0=rtmp[:, tk, 0:32], in1=rtmp[:, tk, 32:64],
                                                           op=ALU.subtract), [rtmp], [xkr])
                    kb.op("dve", lambda e: e.tensor_tensor(out=rtmp[:, tk, 0:32], in0=x2, in1=cosv, op=ALU.mult), [gx, rp], [rtmp])
                    kb.op("dve", lambda e: e.tensor_tensor(out=rtmp[:, tk, 32:64], in0=x1, in1=sinv, op=ALU.mult), [gx, rp], [rtmp])
                    kb.op("dve", lambda e: e.tensor_tensor(out=xkr[:, tk, 32:64], in0=rtmp[:, tk, 0:32], in1=rtmp[:, tk, 32:64],
                                                           op=ALU.add), [rtmp], [xkr])
                for tk in range(4):
                    kb.op("pe", lambda e: e.transpose(out=pT[0:64, tk * 128:(tk + 1) * 128], in_=xkr[:, tk, :],
                                                      identity=c_ident[:]), [xkr, c_ident], [pT])
                kb.op("act", lambda e: e.activation(out=ikg[:], in_=pT[0:64, :], func=AF.Identity), [pT], [ikg])
                kb.dma("sp", ikT_d[:, g * 512:(g + 1) * 512], ikg[:], reads=[ikg])
        barrier()
        with ExitStack() as es:
            W = kb.sb(es, [128, 8, 2048], F32R, "W3")
            Wb, w_first, w_rest = w_blocks(W, 2048, [(O_AK, 2048, 0)])
            w_first()
            bias = bcast_load(es, b_in[0, O_AK:O_AK + 2048], 2048, "bias3")
            xgs = [kb.sb(es, [128, 8, 512], F32R, "xg") for _ in range(2)]
            rps = [kb.sb(es, [128, 4, 2, 64], F32, "rpa") for _ in range(2)]
            ak = kb.sb(es, [128, 8, 128], F32, "ak")
            akrs = [kb.sb(es, [128, 8, 128], BF16, "akr") for _ in range(2)]
            pend_a3 = []
            rtmp = kb.sb(es, [128, 8, 128], F32, "rtmp3")
            akTs = [kb.sb(es, [128, 8, 512], BF16, "akT") for _ in range(2)]
            vaugs = [kb.sb(es, [128, 8, 4, 130], BF16, "vaug") for _ in range(2)]
            for va in vaugs:
                kb.op("dve", lambda e: e.memset(va[:], 1.0), [], [va])
            pA = [kb.ps(es, [128, 512], F32, "pA3") for _ in range(4)]
            pTb = [kb.ps(es, [128, 1024], BF16, "pTb") for _ in range(2)]
            for g in range(NG):
                xg, rp, akT, va = xgs[g % 2], rps[g % 2], akTs[g % 2], vaugs[g % 2]
                kb.dma("pool", xg[:], _r(xnT_d[:, :, g * 512:(g + 1) * 512].rearrange("c p t -> p c t")), writes=[xg])
                if g == 0:
                    w_rest()
                kb.dma("sp", rp[:], rope_a[g * 512:(g + 1) * 512].rearrange("(k p) c d -> p k c d", p=128), writes=[rp])
                for tk in range(4):
                    akr = akrs[tk % 2]
                    for q in range(4):
                        for dc in range(8):
                            kb.op("pe", lambda e: e.matmul(pA[q][:], xg[:, dc, tk * 128:(tk + 1) * 128],
                                                           W[:, dc, q * 512:(q + 1) * 512], start=(dc == 0), stop=(dc == 7)),
                                  [xg, Wb[q]], [pA[q]])
                    while pend_a3:
                        pend_a3.pop(0)()
                    akf = ak[:].rearrange("p h d -> p (h d)")
                    for q in range(2):
                        kb.op("dve", lambda e: e.tensor_tensor(out=akf[:, q * 512:(q + 1) * 512], in0=pA[q][:],
                                                               in1=bias[:, q * 512:(q + 1) * 512], op=ALU.add),
                              [pA[q], bias], [ak])
                    for q in range(2):
                        kb.op("dve", lambda e: e.tensor_tensor(out=va[:, q * 4:(q + 1) * 4, tk, 0:128],
                                                               in0=pA[2 + q][:].rearrange("p (h d) -> p h d", h=4),
                                                               in1=bias[:, 1024 + q * 512:1024 + (q + 1) * 512].rearrange("p (h d) -> p h d", h=4),
                                                               op=ALU.add), [pA[2 + q], bias], [va])
                    rope(es, (ak[:, :, 0:64], ak[:, :, 64:128], [ak, rp]), (akr[:, :, 0:64], akr[:, :, 64:128], [akr]),
                         rp[:, tk, 0, :], rp[:, tk, 1, :], 8, 64, rtmp)
                    def tail(g=g, tk=tk, akr=akr, akT=akT, va=va):
                        ptb = pTb[tk % 2]
                        for h in range(8):
                            kb.op("pe", lambda e: e.transpose(out=ptb[:, h * 128:(h + 1) * 128], in_=akr[:, h, :],
                                                              identity=c_identb[:]), [akr, c_identb], [ptb])
                        kb.op("act", lambda e: e.activation(out=akT[:, :, tk * 128:(tk + 1) * 128],
                                                            in_=ptb[:].rearrange("p (h t) -> p h t", h=8), func=AF.Identity),
                              [ptb], [akT])
                        if tk == 3:
                            kb.dma("sp", akT_d[:, :, g * 512:(g + 1) * 512].rearrange("h p t -> p h t"), akT[:], reads=[akT])
                            kb.dma("sp", av_d[:, :, g * 4:(g + 1) * 4, :].rearrange("h p k c -> p h k c"), va[:], reads=[va])
                    pend_a3.append(tail)
            while pend_a3:
                pend_a3.pop(0)()
        def q_phase(col_specs, ncols, consume):
            barrier()
            with ExitStack() as es:
                W = kb.sb(es, [128, 8, ncols], F32R, "WQp")
                bias = kb.sb(es, [128, ncols], F32, "biasq")
                Wb, w_first, w_rest = w_blocks(W, ncols, col_specs)
                w_first()
                for (src0, n, dst0) in col_specs:
                    kb.dma("sp", bias[:, dst0:dst0 + n], b_in[0, src0:src0 + n].partition_broadcast(128), writes=[bias])
                xos = [kb.sb(es, [128, 8, 128], F32R, "xo") for _ in range(2)]
                nb = (ncols + 511) // 512
                pQ = [kb.ps(es, [128, 512], F32, "pQ") for _ in range(4)]
                pend_q = []
                for ot in range(NOT):
                    xo = xos[ot % 2]
                    kb.dma("pool", xo[:], _r(xnTo_d[:, :, ot * 128:(ot + 1) * 128].rearrange("c p t -> p c t")), writes=[xo])
                    if ot == 0:
                        w_rest()
                    for q in range(nb):
                        c0, c1 = q * 512, min(ncols, (q + 1) * 512)
                        for dc in range(8):
                            kb.op("pe", lambda e: e.matmul(pQ[q][:, 0:c1 - c0], xo[:, dc, :], W[:, dc, c0:c1],
                                                           start=(dc == 0), stop=(dc == 7)), [xo, Wb[q]], [pQ[q]])
                    while pend_q:
                        pend_q.pop(0)()
                    tl = consume(es, ot, pQ, bias)
                    if tl is not None:
                        pend_q.append(tl)
                while pend_q:
                    pend_q.pop(0)()

        barrier()
        with ExitStack() as esq:
            aq = kb.sb(esq, [128, 8, 128], F32, "aq")
            aqrs = [kb.sb(esq, [128, 8, 128], BF16, "aqr") for _ in range(2)]
            xq = kb.sb(esq, [128, 8, 64], F32, "xq")
            xqrs = [kb.sb(esq, [128, 8, 64], F32, "xqr") for _ in range(2)]
            rtq = kb.sb(esq, [128, 8, 128], F32, "rtq")
            rpa = kb.sb(esq, [128, 2, 64], F32, "rpao")
            rpi = kb.sb(esq, [128, 2, 32], F32, "rpio")
            aqT = kb.sb(esq, [128, 8, 128], BF16, "aqT")
            iqT = kb.sb(esq, [64, 8, 128], F32, "iqT")
            ptb = kb.ps(esq, [128, 1024], BF16, "ptbq")
            pti = [kb.ps(esq, [128, 512], F32, "ptiq") for _ in range(2)]

            def consume_q1(es, ot, pQ, bias):
                aqr, xqr = aqrs[ot % 2], xqrs[ot % 2]
                kb.dma("sp", rpa[:], rope_ao[ot * 128:(ot + 1) * 128], writes=[rpa])
                kb.dma("sp", rpi[:], rope_io[ot * 128:(ot + 1) * 128], writes=[rpi])
                aqf = aq[:].rearrange("p h d -> p (h d)")
                for q in range(2):
                    kb.op("dve", lambda e: e.tensor_tensor(out=aqf[:, q * 512:(q + 1) * 512], in0=pQ[q][:],
                                                           in1=bias[:, q * 512:(q + 1) * 512], op=ALU.add), [pQ[q], bias], [aq])
                kb.op("dve", lambda e: e.tensor_tensor(out=xq[:].rearrange("p h d -> p (h d)"), in0=pQ[2][:],
                                                       in1=bias[:, 1024:1536], op=ALU.add), [pQ[2], bias], [xq])
                kb.op("dve", lambda e: e.tensor_tensor(out=WQ[:, ot, :], in0=pQ[3][:, 0:8], in1=bias[:, 1536:1544],
                                                       op=ALU.add), [pQ[3], bias], [WQ])
                kb.op("dve", lambda e: e.tensor_scalar(out=WQ[:, ot, :], in0=WQ[:, ot, :], scalar1=float(8 ** -0.5 * 64 ** -0.5),
                                                      scalar2=None, op0=ALU.mult), [WQ], [WQ])
                rope(es, (aq[:, :, 0:64], aq[:, :, 64:128], [aq, rpa]), (aqr[:, :, 0:64], aqr[:, :, 64:128], [aqr]),
                     rpa[:, 0, :], rpa[:, 1, :], 8, 64, rtq)
                rope(es, (xq[:, :, 0:32], xq[:, :, 32:64], [xq, rpi]), (xqr[:, :, 0:32], xqr[:, :, 32:64], [xqr]),
                     rpi[:, 0, :], rpi[:, 1, :], 8, 32, rtq)
                def tail(ot=ot, aqr=aqr, xqr=xqr):
                    for h in range(8):
                        kb.op("pe", lambda e: e.transpose(out=ptb[:, h * 128:(h + 1) * 128], in_=aqr[:, h, :],
                                                          identity=c_identb[:]), [aqr, c_identb], [ptb])
                    kb.op("act", lambda e: e.activation(out=aqT[:], in_=ptb[:].rearrange("p (h t) -> p h t", h=8),
                                                        func=AF.Identity), [ptb], [aqT])
                    kb.dma("sp", aqT_d[ot], aqT[:], reads=[aqT])
                    for h in range(8):
                        kb.op("pe", lambda e: e.transpose(out=pti[h // 4][0:64, (h % 4) * 128:(h % 4 + 1) * 128],
                                                          in_=xqr[:, h, :], identity=c_ident[:]), [xqr, c_ident], [pti[h // 4]])
                    for q in range(2):
                        kb.op("act", lambda e: e.activation(out=iqT[:, q * 4:(q + 1) * 4, :],
                                                            in_=pti[q][0:64, :].rearrange("p (h t) -> p h t", h=4),
                                                            func=AF.Identity), [pti[q]], [iqT])
                    kb.dma("sp", iqT_d[ot], iqT[:], reads=[iqT])
                return tail

            q_phase([(O_AQ, 1024, 0), (O_XQ, 512, 1024), (O_XW, 8, 1536)], 1544, consume_q1)

        barrier()
        with ExitStack() as esq:
            t1 = kb.sb(esq, [128, 2048], F32, "t1")
            ogt = kb.sb(esq, [128, D], F32, "ogt")

            def consume_q2(es, ot, pQ, bias):
                for q in range(4):
                    kb.op("dve", lambda e: e.tensor_tensor(out=t1[:, q * 512:(q + 1) * 512], in0=pQ[q][:],
                                                           in1=bias[:, q * 512:(q + 1) * 512], op=ALU.add), [pQ[q], bias], [t1])
                kb.op("act", lambda e: e.activation(out=t1[:], in_=t1[:], func=AF.Sigmoid), [t1], [t1])
                kb.op("dve", lambda e: e.tensor_tensor(out=ogt[:], in0=t1[:, 0:1024], in1=t1[:, 1024:2048], op=ALU.mult),
                      [t1], [ogt])
                kb.dma("sp", og_d[ot * 128:(ot + 1) * 128, :], ogt[:], reads=[ogt])

            q_phase([(O_MO, 1024, 0), (O_GM, 1024, 1024)], 2048, consume_q2)

            def consume_q3(es, ot, pQ, bias):
                for q in range(2):
                    kb.op("dve", lambda e: e.tensor_tensor(out=t1[:, q * 512:(q + 1) * 512], in0=pQ[q][:],
                                                           in1=bias[:, q * 512:(q + 1) * 512], op=ALU.add), [pQ[q], bias], [t1])
                kb.op("act", lambda e: e.activation(out=ogt[:], in_=t1[:, 0:1024], func=AF.Sigmoid), [t1], [ogt])
                kb.dma("sp", ga_d[ot * 128:(ot + 1) * 128, :], ogt[:], reads=[ogt])

            q_phase([(O_GA, 1024, 0)], 1024, consume_q3)
        fence()
        if upto == "Q":
            for nm, src in (("v", v_d), ("ikT", ikT_d), ("og", og_d), ("ga", ga_d)):
                if nm in dbg:
                    kb.dma("sp", dbg[nm], src)
            finish()
            nc.used_inputs = used_inputs
            return nc
        barrier()
        with ExitStack() as es:
            G = kb.sb(es, [64, NCH, 8], F32, "G64")
            for c0 in range(0, NCH, 16):
                c1 = min(NCH, c0 + 16)
                kb.dma("sp", G[:, c0:c1, :], g_d[c0 * 64:c1 * 64, :].rearrange("(c s) n -> s c n", s=64), writes=[G])
            c_tri = kb.sb(es, [64, 64], F32, "tri64")
            kb.dma("sp", c_tri[:], tri64, writes=[c_tri])
            c_trs = kb.sb(es, [64, 8, 128], F32, "trisel")
            kb.dma("sp", c_trs[:], trisel.rearrange("c s m -> s c m"), writes=[c_trs])
            c_cm = kb.sb(es, [64, 16], F32, "cmask")
            kb.dma("sp", c_cm[:], cmask, writes=[c_cm])
            c_ones = kb.sb(es, [64, 128], F32, "ones64")
            kb.op("dve", lambda e: e.memset(c_ones[:], 1.0), [], [c_ones])
            mng = bcast_load(es, m_norm_g[0, :], D, "mng")
            NL = kb.sb(es, [64, NCH, 4], F32, "NL")
            NB = kb.sb(es, [64, NCH, 4], F32, "NB")
            NBL = kb.sb(es, [128, NCH, 4], F32, "NBL")
            DEC = kb.sb(es, [128, NCH, 4], F32, "DEC")
            WG = kb.sb(es, [64, NCH, 4], F32, "WG")
            CF = kb.sb(es, [64, NCH, 4], F32, "CF")
            RF = kb.sb(es, [128, NG, 4], F32, "RF")
            pg = [kb.ps(es, [128, 512], F32, "pgate") for _ in range(2)]
            kb.op("act", lambda e: e.activation(out=NL[:], in_=G[:, :, 4:8], func=AF.Exp, scale=-1.0), [G], [NL])
            kb.op("act", lambda e: e.activation(out=NL[:], in_=NL[:], func=AF.Ln, bias=1.0), [NL], [NL])
            NLf = NL[:].rearrange("s c h -> s (c h)")
            for c0 in range(0, NCH * 4, 512):
                c1 = min(NCH * 4, c0 + 512)
                kb.op("pe", lambda e: e.matmul(pg[0][0:64, 0:c1 - c0], c_tri[:], NLf[:, c0:c1], start=True, stop=True),
                      [c_tri, NL], [pg[0]])
                kb.op("dve", lambda e: e.tensor_copy(out=NB[:].rearrange("s c h -> s (c h)")[:, c0:c1], in_=pg[0][0:64, 0:c1 - c0]),
                      [pg[0]], [NB])
                kb.op("pe", lambda e: e.matmul(pg[1][:, 0:c1 - c0], c_ones[:], NLf[:, c0:c1], start=True, stop=True),
                      [c_ones, NL], [pg[1]])
                kb.op("dve", lambda e: e.tensor_copy(out=NBL[:].rearrange("s c h -> s (c h)")[:, c0:c1], in_=pg[1][:, 0:c1 - c0]),
                      [pg[1]], [NBL])
            kb.op("act", lambda e: e.activation(out=DEC[:], in_=NBL[:], func=AF.Exp, scale=-1.0), [NBL], [DEC])
            kb.op("dve", lambda e: e.tensor_tensor(out=CF[:], in0=G[:, :, 0:4], in1=NB[:], op=ALU.add), [G, NB], [CF])
            kb.op("dve", lambda e: e.tensor_tensor(out=WG[:], in0=CF[:], in1=NBL[0:64], op=ALU.subtract), [CF, NBL], [WG])
            kb.op("act", lambda e: e.activation(out=CF[:], in_=CF[:], func=AF.Exp), [CF], [CF])
            kb.op("act", lambda e: e.activation(out=WG[:], in_=WG[:], func=AF.Exp), [WG], [WG])
            for g in range(NG):
                for cp in range(8):
                    kb.op("pe", lambda e: e.matmul(pg[0][:, 0:4], c_trs[:, cp, :], NL[:, g * 8 + cp, :],
                                                   start=(cp == 0), stop=(cp == 7)), [c_trs, NL], [pg[0]])
                kb.op("act", lambda e: e.activation(out=RF[:, g, :], in_=pg[0][:, 0:4], func=AF.Exp, scale=-1.0), [pg[0]], [RF])
            kTs = [kb.sb(es, [128, 8, 512], F32R, "kTg") for _ in range(2)]
            kts = [kb.sb(es, [64, D], F32, "ktg") for _ in range(2)]
            kws = [kb.sb(es, [64, 4, 256], F32R, "kwg") for _ in range(3)]
            vgs = [kb.sb(es, [64, 4, 258], F32R, "vg") for _ in range(3)]
            qgs = [kb.sb(es, [128, 8, 128], F32R, "qg") for _ in range(2)]
            for vg in vgs:
                kb.op("dve", lambda e: e.memset(_f(vg[:]), 1.0), [], [vg])
            qpad = kb.sb(es, [128, 8, 8, 128], F32R, "qpad")
            spad = kb.sb(es, [64, 4, 8, 128], F32R, "spad")
            kb.op("dve", lambda e: e.memset(_f(qpad[:]), 0.0), [], [qpad])
            kb.op("dve", lambda e: e.memset(_f(spad[:]), 0.0), [], [spad])
            C32 = [kb.sb(es, [128, 2, 258], F32, "C32_%d" % h) for h in range(4)]
            Cr = [kb.sb(es, [128, 2, 258], F32R, "Cr_%d" % h) for h in range(4)]
            for h in range(4):
                kb.op("dve", lambda e: e.memset(C32[h][:], 0.0), [], [C32[h]])
                kb.op("dve", lambda e: e.memset(_f(Cr[h][:]), 0.0), [], [Cr[h]])
            spb = [[Tl(None) for _ in range(8)] for _ in range(4)]
            hm = kb.sb(es, [128, 4, 256], F32, "hm")
            ogts = [kb.sb(es, [128, D], F32, "ogtB") for _ in range(2)]
            zt = kb.sb(es, [128, D], F32, "zt")
            kb.op("dve", lambda e: e.memset(zt[:], 0.0), [], [zt])
            zrows = list(range(0, NEXP * CAP, 128))
            ymt = kb.sb(es, [128, D], F32, "ymt")
            sc = kb.sb(es, [128, 16], F32, "scB")
            stB = (kb.sb(es, [128, 1, 6], F32, "stB"), kb.sb(es, [128, 4], F32, "mvB"))
            pacc = [kb.ps(es, [128, 512], F32, "pacc") for _ in range(4)]
            pU = pg
            pS = kb.ps(es, [128, 512], F32, "pS")
            pS_s = [Tl(None), Tl(None)]
            pS_n = [Tl(None), Tl(None)]
            c_one2 = kb.sb(es, [64, 2], F32R, "one2")
            kb.op("dve", lambda e: e.memset(_f(c_one2[:]), 1.0), [], [c_one2])
            def group_loads(g):
                kT, qg = kTs[g % 2], qgs[g % 2]
                kb.dma("pool", kT[:], _r(kT_d[:, :, g * 512:(g + 1) * 512].rearrange("c p t -> p c t")), writes=[kT])
                kb.dma("pool", qg[:], _r(qTo_d[:, :, g * 128:(g + 1) * 128].rearrange("c p t -> p c t")), writes=[qg])
                kb.dma("sp", ogts[g % 2][:], og_d[g * 128:(g + 1) * 128, :], writes=[ogts[g % 2]])

            for g in range(NG):
                kT, qg, ogt = kTs[g % 2], qgs[g % 2], ogts[g % 2]
                if g == 0:
                    group_loads(0)
                for cp in range(8):
                    kb.op("pool", lambda e: e.tensor_copy(out=qpad[:, :, cp, cp * 16:(cp + 1) * 16], in_=qg[:, :, cp * 16:(cp + 1) * 16]),
                          [qg], [qpad])
                if g + 1 < NG:
                    group_loads(g + 1)
                nz = (len(zrows) + NG - 1) // NG
                for r0 in zrows[g * nz:(g + 1) * nz]:
                    kb.dma("sp", xs_d[r0:r0 + 128, :], zt[:], reads=[zt])
                def chunk_loads(c):
                    kt, kw, vg = kts[c % 2], kws[c % 3], vgs[c % 3]
                    kb.dma("sp", kt[:], ktok_d[c * 64:(c + 1) * 64, :], writes=[kt])
                    kb.dma("pool", vg[:, :, 0:256], _r(v_d[c * 64:(c + 1) * 64, :].rearrange("s (h d) -> s h d", h=4)), writes=[vg])
                    kb.op("pool", lambda e: e.tensor_tensor(out=kw[:], in0=kt[:].rearrange("s (h d) -> s h d", h=4),
                                                            in1=WG[:, c, :].unsqueeze(2).to_broadcast([64, 4, 256]),
                                                            op=ALU.mult), [kt, WG], [kw])

                def emit_S(cp, h):
                    c = g * 8 + cp
                    par = h % 2
                    for half in range(2):
                        kb.op("pe", lambda e: e.matmul(pS[0:64, par * 16:(par + 1) * 16], kT[:, h * 2 + half, cp * 64:(cp + 1) * 64],
                                                       qg[:, h * 2 + half, cp * 16:(cp + 1) * 16], start=(half == 0), stop=(half == 1)),
                              [kT, qg], [pS_s[par]])
                    kb.op("dve", lambda e: e.scalar_tensor_tensor(out=spad[:, h, cp, cp * 16:(cp + 1) * 16],
                                                                  in0=pS[0:64, par * 16:(par + 1) * 16],
                                                                  scalar=CF[:, c, h:h + 1], in1=c_cm[:], op0=ALU.mult, op1=ALU.mult),
                          [pS_s[par], CF, c_cm], [spb[h][cp]])

                steps = [(cp, h) for cp in range(8) for h in range(4)]
                emit_S(*steps[0])
                for si, (cp, h) in enumerate(steps):
                    c = g * 8 + cp
                    kt, kw, vg = kts[c % 2], kws[c % 3], vgs[c % 3]
                    if h == 0:
                        if c == 0:
                            chunk_loads(0)
                        if c + 1 < NCH:
                            chunk_loads(c + 1)
                    if si + 1 < len(steps):
                        emit_S(*steps[si + 1])
                    par = h % 2
                    kb.op("pe", lambda e: e.matmul(pacc[h][:, 0:258], spad[:, h, cp, :], vg[:, h, :],
                                                   start=(cp == 0), stop=False), [spb[h][cp], vg], [pacc[h]])
                    for half in range(2):
                        kb.op("pe", lambda e: e.matmul(pacc[h][:, 0:258], qpad[:, h * 2 + half, cp, :], Cr[h][:, half, :],
                                                       start=False, stop=(cp == 7 and half == 1)), [qpad, Cr[h]], [pacc[h]])
                    for half in range(2):
                        kb.op("pe", lambda e: e.matmul(pU[par][:, half * 256:(half + 1) * 256], kw[:, h, half * 128:(half + 1) * 128],
                                                       vg[:, h, 0:256], start=True, stop=True), [kw, vg], [pU[par]])
                        kb.op("pe", lambda e: e.matmul(pS[:, 64 + par * 4 + half * 2:64 + par * 4 + half * 2 + 2],
                                                       kw[:, h, half * 128:(half + 1) * 128], c_one2[:], start=True, stop=True),
                              [kw, c_one2], [pS_n[par]])
                    kb.op("dve", lambda e: e.scalar_tensor_tensor(out=C32[h][:, :, 0:256], in0=C32[h][:, :, 0:256],
                                                                  scalar=DEC[:, c, h:h + 1],
                                                                  in1=pU[par][:].rearrange("p (a d) -> p a d", a=2),
                                                                  op0=ALU.mult, op1=ALU.add), [C32[h], DEC, pU[par]], [C32[h]])
                    kb.op("dve", lambda e: e.scalar_tensor_tensor(out=C32[h][:, :, 256:258], in0=C32[h][:, :, 256:258],
                                                                  scalar=DEC[:, c, h:h + 1],
                                                                  in1=pS[:, 64 + par * 4:64 + par * 4 + 4].rearrange("p (a d) -> p a d", a=2),
                                                                  op0=ALU.mult, op1=ALU.add), [C32[h], DEC, pS_n[par]], [C32[h]])
                    kb.op("act", lambda e: e.activation(out=Cr[h][:], in_=C32[h][:], func=AF.Identity), [C32[h]], [Cr[h]])
                for h in range(4):
                    kb.op("dve", lambda e: e.tensor_scalar(out=sc[:, 0:1], in0=pacc[h][:, 256:257], scalar1=RF[:, g, h:h + 1],
                                                          scalar2=None, op0=ALU.mult), [pacc[h], RF], [sc])
                    kb.op("dve", lambda e: e.tensor_scalar(out=sc[:, 3:4], in0=sc[:, 0:1], scalar1=-1.0, scalar2=None,
                                                          op0=ALU.mult), [sc], [sc])
                    kb.op("dve", lambda e: e.tensor_tensor(out=sc[:, 0:1], in0=sc[:, 0:1], in1=sc[:, 3:4], op=ALU.max), [sc], [sc])
                    kb.op("dve", lambda e: e.tensor_scalar(out=sc[:, 0:1], in0=sc[:, 0:1], scalar1=1.0, scalar2=None,
                                                          op0=ALU.max), [sc], [sc])
                    kb.op("dve", lambda e: e.reciprocal(out=sc[:, 1:2], in_=sc[:, 0:1]), [sc], [sc])
                    kb.op("dve", lambda e: e.tensor_tensor(out=sc[:, 2:3], in0=sc[:, 1:2], in1=RF[:, g, h:h + 1], op=ALU.mult),
                          [sc, RF], [sc])
                    kb.op("act", lambda e: e.activation(out=hm[:, h, :], in_=pacc[h][:, 0:256], func=AF.Identity, scale=sc[:, 2:3]),
                          [pacc[h], sc], [hm])
                    kb.op("dve", lambda e: e.bn_stats(out=stB[0][:, 0, :], in_=hm[:, h, :]), [hm], [stB[0]])
                    kb.op("dve", lambda e: e.bn_aggr(out=stB[1][:, 0:2], in_=stB[0][:, 0, :]), [stB[0]], [stB[1]])
                    kb.op("dve", lambda e: e.tensor_scalar(out=stB[1][:, 2:3], in0=stB[1][:, 1:2], scalar1=LN_EPS, scalar2=None,
                                                          op0=ALU.add), [stB[1]], [stB[1]])
                    kb.op("act", lambda e: e.activation(out=stB[1][:, 2:3], in_=stB[1][:, 2:3], func=AF.Sqrt), [stB[1]], [stB[1]])
                    kb.op("dve", lambda e: e.reciprocal(out=stB[1][:, 3:4], in_=stB[1][:, 2:3]), [stB[1]], [stB[1]])
                    kb.op("dve", lambda e: e.tensor_scalar(out=ymt[:, h * 256:(h + 1) * 256], in0=hm[:, h, :], scalar1=stB[1][:, 0:1],
                                                          scalar2=stB[1][:, 3:4], op0=ALU.subtract, op1=ALU.mult), [hm, stB[1]], [ymt])
                kb.op("dve", lambda e: e.tensor_tensor(out=ymt[:], in0=ymt[:], in1=mng[:], op=ALU.mult), [ymt, mng], [ymt])
                kb.op("dve", lambda e: e.tensor_tensor(out=ymt[:], in0=ymt[:], in1=ogt[:], op=ALU.mult), [ymt, ogt], [ymt])
                kb.dma("sp", ym_d[g * 128:(g + 1) * 128, :], ymt[:], reads=[ymt])
        fence()
        if upto == "B":
            if "ym" in dbg:
                kb.dma("sp", dbg["ym"], ym_d)
            finish()
            nc.used_inputs = used_inputs
            return nc
        barrier()
        with ExitStack() as es:
            c_adm = kb.sb(es, [128, 512], F32, "admb")
            kb.dma("sp", c_adm[:], admb, writes=[c_adm])
            c_pw = kb.sb(es, [128, NBIS], F32, "pw2")
            kb.dma("sp", c_pw[:], pw2, writes=[c_pw])
            NMAX = T
            score = kb.sb(es, [128, NMAX], F32, "score")
            msk = kb.sb(es, [128, NMAX], BF16, "msk")
            mbT = kb.sb(es, [128, NMAX // 128, 128], BF16, "mbT")
            Rb = [kb.sb(es, [128, 2, 512], F32R, "Rb") for _ in range(4)]
            Dw = kb.sb(es, [128, 8, 128], F32R, "Dw")
            iqs = [kb.sb(es, [64, 8, 128], F32R, "iq") for _ in range(2)]
            aqs = [kb.sb(es, [128, 8, 128], BF16, "aqs") for _ in range(2)]
            iks = [kb.sb(es, [64, 512], F32R, "ik") for _ in range(2)]
            KhT = [kb.sb(es, [128, NMAX], BF16, "KhT") for _ in range(2)]
            Vh = [kb.sb(es, [128, NMAX // 128, 130], BF16, "Vh") for _ in range(2)]
            PT = [kb.sb(es, [128, 4, 128], BF16, "PT") for _ in range(2)]
            bs = kb.sb(es, [128, 8 + NBIS], F32, "bs")
            rs8 = kb.sb(es, [128, 8], F32, "rs8")
            ya = kb.sb(es, [128, 8, 128], F32, "ya")
            gat = kb.sb(es, [128, D], F32, "gat")
            ymt = kb.sb(es, [128, D], F32, "ymtD")
            PS = [kb.ps(es, [128, 512], F32, "PDs") for _ in range(2)]
            PSC = [kb.ps(es, [128, 512], F32, "PDsc") for _ in range(2)]
            LG = [kb.ps(es, [128, 512], F32, "PDlg") for _ in range(2)]
            PO = kb.ps(es, [128, 512], F32, "PDo")
            PO_t = [Tl(None), Tl(None)]
            Pb = kb.ps(es, [128, 1024], BF16, "PDb")
            qscale = float(128 ** -0.5)

            def kv_load(i, h):
                N = 512 * (i + 1)
                Kt, Vt = KhT[h % 2], Vh[h % 2]
                kb.dma("sp", Kt[:, 0:N], akT_d[h, :, 0:N], writes=[Kt])
                kb.dma("sp", Vt[:, 0:N // 128, :], av_d[h, :, 0:N // 128, :], writes=[Vt])

            def stage1(i):
                N = 512 * (i + 1)
                iq = iqs[i % 2]
                th = []

                def t_load():
                    kb.dma("pool", iq[:], _r(iqT_d[i]), writes=[iq])
                    for h in range(8):
                        kb.op("pool", lambda e: e.tensor_scalar(out=Dw[:, h, :], in0=c_ident[:], scalar1=WQ[:, i, h:h + 1], scalar2=None,
                                                               op0=ALU.mult), [c_ident, WQ], [Dw])
                th.append((1.0, t_load))
                units = [(kt, hp) for kt in range(i + 1) for hp in range(4)]

                def emit_S(u):
                    kt, hp = units[u]
                    ik = iks[kt % 2]
                    if hp == 0:
                        kb.dma("pool", ik[:], _r(ikT_d[:, kt * 512:(kt + 1) * 512]), writes=[ik])
                    R_ = Rb[u % 4]
                    for j in range(2):
                        h = hp * 2 + j
                        pS_ = PS[j]
                        kb.op("pe", lambda e: e.matmul(pS_[:], iq[:, h, :], ik[:], start=True, stop=True), [iq, ik], [pS_])
                        if j == 0:
                            kb.op("act", lambda e: e.activation(out=R_[:, j, :], in_=pS_[:], func=AF.Relu), [pS_], [R_])
                        else:
                            kb.op("dve", lambda e: e.tensor_scalar(out=R_[:, j, :], in0=pS_[:], scalar1=0.0, scalar2=None,
                                                                  op0=ALU.max), [pS_], [R_])

                def emit_Sc(u):
                    kt, hp = units[u]
                    psc = PSC[kt % 2]
                    R_ = Rb[u % 4]
                    for j in range(2):
                        h = hp * 2 + j
                        kb.op("pe", lambda e: e.matmul(psc[:], Dw[:, h, :], R_[:, j, :], start=(h == 0), stop=(h == 7)),
                              [Dw, R_], [psc])
                    if hp == 3:
                        kb.op("act", lambda e: e.activation(out=score[:, kt * 512:(kt + 1) * 512], in_=psc[:], func=AF.Identity),
                              [psc], [score])

                th.append((1.0, lambda: emit_S(0)))
                for u in range(len(units)):
                    def t_unit(u=u):
                        if u + 1 < len(units):
                            emit_S(u + 1)
                        emit_Sc(u)
                    th.append((1.3, t_unit))

                def t_prep():
                    kb.op("dve", lambda e: e.tensor_reduce(out=bs[:, 0:1], in_=score[:, 0:N], axis=AX.X, op=ALU.max), [score], [bs])
                    kb.op("dve", lambda e: e.tensor_reduce(out=bs[:, 1:2], in_=score[:, 0:N], axis=AX.X, op=ALU.min), [score], [bs])
                    kb.op("dve", lambda e: e.tensor_scalar(out=bs[:, 2:3], in0=bs[:, 1:2], scalar1=-1.0, scalar2=None, op0=ALU.add), [bs], [bs])
                    kb.op("dve", lambda e: e.tensor_tensor(out=bs[:, 3:4], in0=bs[:, 0:1], in1=bs[:, 2:3], op=ALU.subtract), [bs], [bs])
                    kb.op("dve", lambda e: e.tensor_scalar(out=bs[:, 8:8 + NBIS], in0=c_pw[:], scalar1=bs[:, 3:4], scalar2=None,
                                                          op0=ALU.mult), [bs, c_pw], [bs])
                    kb.op("dve", lambda e: e.tensor_tensor(out=score[:, N - 512:N], in0=score[:, N - 512:N], in1=c_adm[:], op=ALU.add),
                          [score, c_adm], [score])
                th.append((2.0 * N / 960.0 + 1.0, t_prep))

                def t_bis(n):
                    kb.op("dve", lambda e: e.tensor_tensor(out=bs[:, 4:5], in0=bs[:, 2:3], in1=bs[:, 8 + n:9 + n], op=ALU.add), [bs], [bs])
                    kb.op("dve", lambda e: e.tensor_scalar(out=msk[:, 0:N], in0=score[:, 0:N], scalar1=bs[:, 4:5], scalar2=0.0,
                                                          op0=ALU.is_gt, op1=ALU.add, accum_out=bs[:, 5:6]), [score, bs], [msk, bs])
                    kb.op("dve", lambda e: e.tensor_scalar(out=bs[:, 6:7], in0=bs[:, 5:6], scalar1=255.5, scalar2=None, op0=ALU.is_gt),
                          [bs], [bs])
                    kb.op("dve", lambda e: e.scalar_tensor_tensor(out=bs[:, 2:3], in0=bs[:, 8 + n:9 + n], scalar=bs[:, 6:7],
                                                                  in1=bs[:, 2:3], op0=ALU.mult, op1=ALU.add), [bs], [bs])
                for n in range(NBIS):
                    th.append((N / 960.0 + 0.5, lambda n=n: t_bis(n)))

                def t_final():
                    kb.op("dve", lambda e: e.tensor_scalar(out=msk[:, 0:N], in0=score[:, 0:N], scalar1=bs[:, 2:3], scalar2=None,
                                                          op0=ALU.is_gt), [score, bs], [msk])
                th.append((N / 960.0 + 0.2, t_final))
                return th

            def mask_T(i):
                N = 512 * (i + 1)
                for k8 in range(0, N // 128, 8):
                    for kb_ in range(8):
                        kk = k8 + kb_
                        if kk >= N // 128:
                            break
                        kb.op("pe", lambda e: e.transpose(out=Pb[:, kb_ * 128:(kb_ + 1) * 128], in_=msk[:, kk * 128:(kk + 1) * 128],
                                                          identity=c_identb[:]), [msk, c_identb], [Pb])
                    nn = min(8, N // 128 - k8)
                    kb.op("act", lambda e: e.activation(out=mbT[:, k8:k8 + nn, :],
                                                        in_=Pb[:, 0:nn * 128].rearrange("p (k q) -> p k q", k=nn),
                                                        func=AF.Identity, scale=30000.0, bias=-30000.0), [Pb], [mbT])

            def stage2(i):
                N = 512 * (i + 1)
                aq_ = aqs[i % 2]
                th = []

                def t_loads():
                    if i == 0:
                        kb.dma("sp", aqs[0][:], aqT_d[0], writes=[aqs[0]])
                        kv_load(0, 0)
                    if i + 1 < NG:
                        kb.dma("sp", aqs[(i + 1) % 2][:], aqT_d[i + 1], writes=[aqs[(i + 1) % 2]])
                    kb.dma("sp", gat[:], ga_d[i * 128:(i + 1) * 128, :], writes=[gat])
                    kb.dma("sp", ymt[:], ym_d[i * 128:(i + 1) * 128, :], writes=[ymt])
                th.append((0.2, t_loads))

                def emit_lg(h, k4):
                    Kt = KhT[h % 2]
                    lg = LG[k4 % 2]
                    pt = PT[k4 % 2]
                    for j4 in range(4):
                        kk = k4 * 4 + j4
                        kb.op("pe", lambda e: e.matmul(lg[:, j4 * 128:(j4 + 1) * 128], Kt[:, kk * 128:(kk + 1) * 128], aq_[:, h, :],
                                                       start=True, stop=False), [Kt, aq_], [lg])
                        kb.op("pe", lambda e: e.matmul(lg[:, j4 * 128:(j4 + 1) * 128], c_identb[:], mbT[:, kk, :],
                                                       start=False, stop=True), [c_identb, mbT], [lg])
                    kb.op("act", lambda e: e.activation(out=pt[:].rearrange("p k q -> p (k q)"), in_=lg[:], func=AF.Exp, scale=qscale),
                          [lg], [pt])

                def emit_pv(h, k4):
                    Vt = Vh[h % 2]
                    pt = PT[k4 % 2]
                    par = h % 2
                    O = PO[:, par * 256:par * 256 + 130]
                    for j4 in range(4):
                        kk = k4 * 4 + j4
                        kb.op("pe", lambda e: e.matmul(O, pt[:, j4, :], Vt[:, kk, :], start=(kk == 0),
                                                       stop=(kk == N // 128 - 1)), [pt, Vt], [PO_t[par]])
                    if k4 == N // 512 - 1:
                        kb.op("dve", lambda e: e.reciprocal(out=rs8[:, h:h + 1], in_=PO[:, par * 256 + 128:par * 256 + 129]),
                              [PO_t[par]], [rs8])
                        kb.op("dve", lambda e: e.tensor_scalar(out=ya[:, h, :], in0=PO[:, par * 256:par * 256 + 128],
                                                              scalar1=rs8[:, h:h + 1], scalar2=None, op0=ALU.mult),
                              [PO_t[par], rs8], [ya])

                for h in range(8):
                    def t_first(h=h):
                        if h < 7:
                            kv_load(i, h + 1)
                        elif i + 1 < NG:
                            kv_load(i + 1, 0)
                        emit_lg(h, 0)
                    th.append((0.9, t_first))
                    for k4 in range(N // 512):
                        def t_blk(h=h, k4=k4):
                            if k4 + 1 < N // 512:
                                emit_lg(h, k4 + 1)
                            emit_pv(h, k4)
                        th.append((0.75, t_blk))

                def t_merge():
                    kb.op("dve", lambda e: e.tensor_tensor(out=gat[:], in0=gat[:], in1=ya[:].rearrange("p h d -> p (h d)"), op=ALU.mult),
                          [gat, ya], [gat])
                    kb.op("dve", lambda e: e.tensor_tensor(out=gat[:], in0=gat[:], in1=ymt[:], op=ALU.add), [gat, ymt], [gat])
                    kb.dma("sp", mg_d[i * 128:(i + 1) * 128, :], gat[:], reads=[gat])
                th.append((2.0, t_merge))
                return th

            def run_merged(A, Bl):
                ta = sum(w for w, _ in A) or 1.0
                tb = sum(w for w, _ in Bl) or 1.0
                ia = ib = 0
                da = db = 0.0
                while ia < len(A) or ib < len(Bl):
                    if ib >= len(Bl) or (ia < len(A) and da / ta <= db / tb):
                        w, f = A[ia]
                        ia += 1
                        da += w
                    else:
                        w, f = Bl[ib]
                        ib += 1
                        db += w
                    f()

            for _, f in stage1(0):
                f()
            mask_T(0)
            for i in range(NG):
                run_merged(stage2(i), stage1(i + 1) if i + 1 < NG else [])
                if i + 1 < NG:
                    mask_T(i + 1)
        fence()
        if upto == "D":
            if "mg" in dbg:
                kb.dma("sp", dbg["mg"], mg_d)
            finish()
            nc.used_inputs = used_inputs
            return nc
        barrier()
        NSL = NEXP * CAP
        GK = kb.sb(es0, [128, NOT, 4], F32, "GK")
        bc_reg = nc.gpsimd.to_reg(NSL - 1)
        SLI = kb.sb(es0, [128, NOT, 4], I32, "SLI")

        def ln_full(xt, stp, gt, bt, outt):
            mean, rstd = layernorm_stats(None, xt, stp)
            kb.op("dve", lambda e: e.tensor_scalar(out=outt[:], in0=xt[:], scalar1=mean, scalar2=rstd,
                                                  op0=ALU.subtract, op1=ALU.mult), [xt, stp[1]], [outt])
            kb.op("dve", lambda e: e.tensor_tensor(out=outt[:], in0=outt[:], in1=gt[:], op=ALU.mult), [outt, gt], [outt])
            kb.op("dve", lambda e: e.tensor_tensor(out=outt[:], in0=outt[:], in1=bt[:], op=ALU.add), [outt, bt], [outt])

        with ExitStack() as es:
            Wo = kb.sb(es, [128, 8, D], F32R, "Wo")
            for dc in range(8):
                kb.dma("pool", Wo[:, dc, :], w_out[dc * 128:(dc + 1) * 128, :], writes=[Wo])
            g0, b0, g1, b1 = [bcast_load(es, vecs[k, :], D, "lnv%d" % k) for k in range(4)]
            Wr = kb.sb(es, [128, 8, NEXP], F32, "Wr")
            kb.dma("sp", Wr[:], w_router.rearrange("(c p) n -> p c n", p=128), writes=[Wr])
            br = bcast_load(es, b_router[0, :], NEXP, "br")
            c_us = kb.sb(es, [128, 128], F32, "ustri")
            kb.dma("sp", c_us[:], ustri, writes=[c_us])
            c_on = kb.sb(es, [128, 128], F32, "ones128")
            kb.op("dve", lambda e: e.memset(c_on[:], 1.0), [], [c_on])
            c_eo = kb.sb(es, [128, NEXP], F32, "eoff")
            kb.dma("sp", c_eo[:], eoff, writes=[c_eo])
            MK = kb.sb(es, [128, NOT, NEXP], F32, "MK")
            mgt = kb.sb(es, [128, D], F32, "mgt")
            mT = kb.sb(es, [128, 8, 128], F32R, "mT")
            xt = kb.sb(es, [128, D], F32, "xtE")
            h0 = kb.sb(es, [128, D], F32, "h0")
            rs = kb.sb(es, [128, D], F32, "rs")
            h1 = kb.sb(es, [128, D], F32, "h1")
            h1T = kb.sb(es, [128, 8, 128], F32, "h1T")
            stp = (kb.sb(es, [128, 2, 6], F32, "stE"), kb.sb(es, [128, 4], F32, "mvE"))
            lgt = kb.sb(es, [128, NEXP], F32, "lgt")
            ex = kb.sb(es, [128, NEXP], F32, "ex")
            gts = kb.sb(es, [128, NEXP], F32, "gts")
            slf = kb.sb(es, [128, NEXP], F32, "slf")
            m8 = kb.sb(es, [128, 8], F32, "m8")
            sm = kb.sb(es, [128, 8], F32, "sm")
            slk = kb.sb(es, [128, 4], F32, "slk")
            junk = kb.sb(es, [128, NEXP], F32, "junk")
            pt = [kb.ps(es, [128, 512], F32, "ptE") for _ in range(2)]
            po = [kb.ps(es, [128, 512], F32, "poE") for _ in range(2)]
            pr = kb.ps(es, [128, 512], F32, "prE")
            pp = kb.ps(es, [128, 512], F32, "ppE")
            fence()
            for ot in range(NOT):
                kb.dma("sp", mgt[:], mg_d[ot * 128:(ot + 1) * 128, :], writes=[mgt])
                kb.dma("sp", xt[:], x_own[ot * 128:(ot + 1) * 128, :], writes=[xt])
                for half in range(2):
                    for k in range(4):
                        dc = half * 4 + k
                        kb.op("pe", lambda e: e.transpose(out=pt[half][:, k * 128:(k + 1) * 128], in_=mgt[:, dc * 128:(dc + 1) * 128],
                                                          identity=c_ident[:]), [mgt, c_ident], [pt[half]])
                    kb.op("act", lambda e: e.activation(out=mT[:, half * 4:(half + 1) * 4, :],
                                                        in_=pt[half][:].rearrange("p (k t) -> p k t", k=4), func=AF.Identity),
                          [pt[half]], [mT])
                for q in range(2):
                    for dc in range(8):
                        kb.op("pe", lambda e: e.matmul(po[q][:], mT[:, dc, :], Wo[:, dc, q * 512:(q + 1) * 512],
                                                       start=(dc == 0), stop=(dc == 7)), [mT, Wo], [po[q]])
                ln_full(xt, stp, g0, b0, h0)
                for q in range(2):
                    kb.op("dve", lambda e: e.scalar_tensor_tensor(out=rs[:, q * 512:(q + 1) * 512], in0=h0[:, q * 512:(q + 1) * 512],
                                                                  scalar=float(ALPHA), in1=po[q][:], op0=ALU.mult, op1=ALU.add),
                          [h0, po[q]], [rs])
                ln_full(rs, stp, g1, b1, h1)
                kb.dma("sp", h1_d[ot * 128:(ot + 1) * 128, :], h1[:], reads=[h1])
                for half in range(2):
                    for k in range(4):
                        dc = half * 4 + k
                        kb.op("pe", lambda e: e.transpose(out=pt[half][:, k * 128:(k + 1) * 128], in_=h1[:, dc * 128:(dc + 1) * 128],
                                                          identity=c_ident[:]), [h1, c_ident], [pt[half]])
                    kb.op("act", lambda e: e.activation(out=h1T[:, half * 4:(half + 1) * 4, :],
                                                        in_=pt[half][:].rearrange("p (k t) -> p k t", k=4), func=AF.Identity),
                          [pt[half]], [h1T])
                for dc in range(8):
                    kb.op("pe", lambda e: e.matmul(pr[:, 0:NEXP], h1T[:, dc, :], Wr[:, dc, :], start=(dc == 0), stop=(dc == 7)),
                          [h1T, Wr], [pr])
                kb.op("dve", lambda e: e.tensor_tensor(out=lgt[:], in0=pr[:, 0:NEXP], in1=br[:], op=ALU.add), [pr, br], [lgt])
                kb.op("dve", lambda e: e.max(out=m8[:], in_=lgt[:]), [lgt], [m8])
                kb.op("dve", lambda e: e.tensor_scalar(out=MK[:, ot, :], in0=lgt[:], scalar1=m8[:, 3:4], scalar2=None, op0=ALU.is_ge),
                      [lgt, m8], [MK])
                kb.op("dve", lambda e: e.tensor_scalar(out=sm[:, 0:1], in0=m8[:, 0:1], scalar1=-1.0, scalar2=None, op0=ALU.mult), [m8], [sm])
                kb.op("act", lambda e: e.activation(out=ex[:], in_=lgt[:], func=AF.Exp, bias=sm[:, 0:1]), [lgt, sm], [ex])
                kb.op("dve", lambda e: e.scalar_tensor_tensor(out=ex[:], in0=ex[:], scalar=1.0, in1=MK[:, ot, :], op0=ALU.mult, op1=ALU.mult,
                                                              accum_out=sm[:, 1:2]), [ex, MK], [ex, sm])
                kb.op("dve", lambda e: e.reciprocal(out=sm[:, 2:3], in_=sm[:, 1:2]), [sm], [sm])
                kb.op("dve", lambda e: e.tensor_scalar(out=gts[:], in0=ex[:], scalar1=sm[:, 2:3], scalar2=None, op0=ALU.mult), [ex, sm], [gts])
                kb.op("pe", lambda e: e.matmul(pp[:, 0:NEXP], c_us[:], MK[:, ot, :], start=True, stop=(ot == 0)), [c_us, MK], [pp])
                for o2 in range(ot):
                    kb.op("pe", lambda e: e.matmul(pp[:, 0:NEXP], c_on[:], MK[:, o2, :], start=False, stop=(o2 == ot - 1)), [c_on, MK], [pp])
                kb.op("dve", lambda e: e.tensor_scalar(out=junk[:], in0=pp[:, 0:NEXP], scalar1=float(CAP) - 0.5, scalar2=None, op0=ALU.is_lt),
                      [pp], [junk])
                kb.op("dve", lambda e: e.tensor_tensor(out=junk[:], in0=junk[:], in1=MK[:, ot, :], op=ALU.mult), [junk, MK], [junk])
                kb.op("dve", lambda e: e.tensor_tensor(out=slf[:], in0=pp[:, 0:NEXP], in1=c_eo[:], op=ALU.add), [pp, c_eo], [slf])
                kb.op("dve", lambda e: e.tensor_scalar(out=slf[:], in0=slf[:], scalar1=-1.0e6, scalar2=None, op0=ALU.add), [slf], [slf])
                kb.op("dve", lambda e: e.tensor_tensor(out=slf[:], in0=slf[:], in1=junk[:], op=ALU.mult), [slf, junk], [slf])
                kb.op("dve", lambda e: e.tensor_scalar(out=slf[:], in0=slf[:], scalar1=1.0e6, scalar2=None, op0=ALU.add), [slf], [slf])
                for k in range(4):
                    kb.op("dve", lambda e: e.scalar_tensor_tensor(out=junk[:], in0=lgt[:], scalar=m8[:, k:k + 1], in1=slf[:],
                                                                  op0=ALU.is_equal, op1=ALU.mult, accum_out=slk[:, k:k + 1]),
                          [lgt, m8, slf], [junk, slk])
                    kb.op("dve", lambda e: e.scalar_tensor_tensor(out=junk[:], in0=lgt[:], scalar=m8[:, k:k + 1], in1=gts[:],
                                                                  op0=ALU.is_equal, op1=ALU.mult, accum_out=GK[:, ot, k:k + 1]),
                          [lgt, m8, gts], [junk, GK])
                kb.op("dve", lambda e: e.tensor_copy(out=SLI[:, ot, :], in_=slk[:]), [slk], [SLI])
                for k in range(4):
                    kb.dma("pool", None, None, reads=[h1, SLI], indirect=lambda g_: g_.indirect_dma_start(
                        out=xs_d, out_offset=bass.IndirectOffsetOnAxis(ap=SLI[:, ot, k:k + 1], axis=0), in_=h1[:], in_offset=None,
                        bounds_check=bc_reg, oob_is_err=False))
        fence()
        if upto == "E":
            if "h1" in dbg:
                kb.dma("sp", dbg["h1"], h1_d)
            finish()
            nc.used_inputs = used_inputs
            return nc
        barrier()
        with ExitStack() as es:
            NST = CAP // 128
            WS = [kb.sb(es, [128, 8, 512], F32, "WS%d" % k) for k in range(3)]
            WB = [kb.sb(es, [128, 8, 512], F32R, "WB%d" % k) for k in range(4)]
            xsb = [kb.sb(es, [128, NST, D], F32, "xsb") for _ in range(2)]
            xT = kb.sb(es, [128, 8, CAP], F32R, "xTe")
            actT = kb.sb(es, [128, 8, CAP], F32R, "actT")
            bgu = kb.sb(es, [128, NEXP, 16], F32, "bgu")
            kb.dma("sp", bgu[:], bgu_col, writes=[bgu])
            bd = [kb.sb(es, [128, D], F32, "bd") for _ in range(2)]
            gs = [kb.sb(es, [128, CAP], F32, "gs") for _ in range(2)]
            sg = [kb.sb(es, [128, CAP], F32, "sg") for _ in range(2)]
            us = [kb.sb(es, [128, CAP], F32, "us") for _ in range(2)]
            yt = [kb.sb(es, [128, D], F32, "yt") for _ in range(2)]
            pg_ = [kb.ps(es, [128, 512], F32, "pgF") for _ in range(2)]
            pu_ = [kb.ps(es, [128, 512], F32, "puF") for _ in range(2)]
            py_ = [kb.ps(es, [128, 512], F32, "pyF") for _ in range(2)]
            ptx = [kb.ps(es, [128, 512], F32, "ptxF") for _ in range(2)]
            wcnt = [0]

            def wload(src_ap):
                k = wcnt[0]
                wcnt[0] += 1
                st_, t = WS[k % 3], WB[k % 4]
                kb.dma("sp", st_[:], _f(src_ap).rearrange("(c p) n -> p c n", p=128), writes=[st_])
                if k % 2 == 0:
                    kb.op("act", lambda e: e.activation(out=t[:], in_=st_[:], func=AF.Identity), [st_], [t])
                else:
                    kb.op("dve", lambda e: e.tensor_copy(out=t[:], in_=st_[:]), [st_], [t])
                return t

            for ex_ in range(NEXP):
                xs = xsb[ex_ % 2]
                kb.dma("sp", xs[:], xs_d[ex_ * CAP:(ex_ + 1) * CAP, :].rearrange("(k p) n -> p k n", p=128), writes=[xs])
                bdt = bd[ex_ % 2]
                kb.dma("sp", bdt[:], b_down[ex_, :].partition_broadcast(128), writes=[bdt])
                for dc in range(8):
                    px = ptx[dc % 2]
                    for st in range(NST):
                        kb.op("pe", lambda e: e.transpose(out=px[:, st * 128:(st + 1) * 128], in_=xs[:, st, dc * 128:(dc + 1) * 128],
                                                          identity=c_ident[:]), [xs, c_ident], [px])
                    if dc % 2 == 0:
                        kb.op("act", lambda e: e.activation(out=xT[:, dc, :], in_=px[:, 0:CAP], func=AF.Identity), [px], [xT])
                    else:
                        kb.op("dve", lambda e: e.tensor_copy(out=xT[:, dc, :], in_=px[:, 0:CAP]), [px], [xT])
                for hf in range(2):
                    wg_ = wload(w_gate[ex_, :, hf * 512:(hf + 1) * 512])
                    wu_ = wload(w_up[ex_, :, hf * 512:(hf + 1) * 512])
                    for f4 in range(4):
                        fc = hf * 4 + f4
                        par = fc % 2
                        for dc in range(8):
                            kb.op("pe", lambda e: e.matmul(pg_[par][:, 0:CAP], wg_[:, dc, f4 * 128:(f4 + 1) * 128], xT[:, dc, :],
                                                           start=(dc == 0), stop=(dc == 7)), [wg_, xT], [pg_[par]])
                        for dc in range(8):
                            kb.op("pe", lambda e: e.matmul(pu_[par][:, 0:CAP], wu_[:, dc, f4 * 128:(f4 + 1) * 128], xT[:, dc, :],
                                                           start=(dc == 0), stop=(dc == 7)), [wu_, xT], [pu_[par]])
                        kb.op("dve", lambda e: e.tensor_scalar(out=gs[par][:], in0=pg_[par][:, 0:CAP], scalar1=bgu[:, ex_, fc:fc + 1],
                                                              scalar2=7.0, op0=ALU.add, op1=ALU.min), [pg_[par], bgu], [gs[par]])
                        kb.op("act", lambda e: e.activation(out=sg[par][:], in_=gs[par][:], func=AF.Sigmoid, scale=1.702),
                              [gs[par]], [sg[par]])
                        kb.op("dve", lambda e: e.tensor_scalar(out=us[par][:], in0=pu_[par][:, 0:CAP], scalar1=bgu[:, ex_, 8 + fc:9 + fc],
                                                              scalar2=7.0, op0=ALU.add, op1=ALU.min), [pu_[par], bgu], [us[par]])
                        kb.op("dve", lambda e: e.tensor_scalar(out=us[par][:], in0=us[par][:], scalar1=-7.0, scalar2=1.0,
                                                              op0=ALU.max, op1=ALU.add), [us[par]], [us[par]])
                        kb.op("pool", lambda e: e.tensor_tensor(out=gs[par][:], in0=gs[par][:], in1=sg[par][:], op=ALU.mult),
                              [gs[par], sg[par]], [gs[par]])
                        kb.op("pool", lambda e: e.tensor_tensor(out=actT[:, fc, :], in0=gs[par][:], in1=us[par][:], op=ALU.mult),
                              [gs[par], us[par]], [actT])
                wd = [wload(w_down[ex_, :, hf * 512:(hf + 1) * 512]) for hf in range(2)]
                for st in range(NST):
                    y = yt[st % 2]
                    for hf in range(2):
                        for fc in range(8):
                            kb.op("pe", lambda e: e.matmul(py_[hf][:], actT[:, fc, st * 128:(st + 1) * 128], wd[hf][:, fc, :],
                                                           start=(fc == 0), stop=(fc == 7)), [actT, wd[hf]], [py_[hf]])
                        kb.op("dve", lambda e: e.tensor_tensor(out=y[:, hf * 512:(hf + 1) * 512], in0=py_[hf][:],
                                                               in1=bdt[:, hf * 512:(hf + 1) * 512], op=ALU.add), [py_[hf], bdt], [y])
                    kb.dma("sp", ys_d[ex_ * CAP + st * 128: ex_ * CAP + (st + 1) * 128, :], y[:], reads=[y])
        fence()
        barrier()
        with ExitStack() as es:
            g2, b2 = [bcast_load(es, vecs[k, :], D, "lnv%d" % k) for k in (4, 5)]
            yks = [[kb.sb(es, [128, D], F32, "yk%d" % k) for k in range(4)] for _ in range(2)]
            h1ts = [kb.sb(es, [128, D], F32, "h1t") for _ in range(2)]
            acc = kb.sb(es, [128, D], F32, "accG")
            ot_ = kb.sb(es, [128, D], F32, "outG")
            stp = (kb.sb(es, [128, 2, 6], F32, "stG"), kb.sb(es, [128, 4], F32, "mvG"))
            for ot in range(NOT):
                yk, h1t = yks[ot % 2], h1ts[ot % 2]
                kb.dma("pool", h1t[:], h1_d[ot * 128:(ot + 1) * 128, :], writes=[h1t])
                for k in range(4):
                    kb.op("pool", lambda e: e.memset(yk[k][:], 0.0), [], [yk[k]])
                    kb.dma("pool", None, None, reads=[SLI], writes=[yk[k]], indirect=lambda g_: g_.indirect_dma_start(
                        out=yk[k][:], out_offset=None, in_=ys_d, in_offset=bass.IndirectOffsetOnAxis(ap=SLI[:, ot, k:k + 1], axis=0),
                        bounds_check=bc_reg, oob_is_err=False))
                kb.op("dve", lambda e: e.tensor_scalar(out=acc[:], in0=h1t[:], scalar1=float(ALPHA), scalar2=None, op0=ALU.mult),
                      [h1t], [acc])
                for k in range(4):
                    kb.op("dve", lambda e: e.scalar_tensor_tensor(out=acc[:], in0=yk[k][:], scalar=GK[:, ot, k:k + 1], in1=acc[:],
                                                                  op0=ALU.mult, op1=ALU.add), [yk[k], GK, acc], [acc])
                ln_full(acc, stp, g2, b2, ot_)
                kb.dma("sp", out[ot * 128:(ot + 1) * 128, :], ot_[:], reads=[ot_])
        finish()
    nc.used_inputs = used_inputs
    return nc


def _own_idx(T, j):
    c = np.arange(T // 64)[:, None] * 64 + 16 * j + np.arange(16)[None, :]
    return c.reshape(-1)


def _col(v, n):
    return np.ascontiguousarray(v.reshape(n, 128).T)


def prep(inputs, T):
    f = np.float32
    x = np.asarray(inputs["x"], f)
    B = x.shape[0]
    w_in = np.ascontiguousarray(np.asarray(inputs["w_in"], f)[0])
    b_in = np.asarray(inputs["b_in"], f)[0]
    conv_w = np.asarray(inputs["conv_w"], f)[0]
    conv_b = np.asarray(inputs["conv_b"], f)[0]
    cols = np.zeros((128, 128), f)
    cols[:, 0:8] = _col(np.asarray(inputs["ln_in_g"], f), 8)
    cols[:, 8:16] = _col(np.asarray(inputs["ln_in_b"], f), 8)
    cols[:, 16:32] = _col(b_in[0:2048], 16)
    cols[:, 32:48] = _col(conv_b, 16)
    conv_wc = np.ascontiguousarray(conv_w.reshape(4, 16, 128).transpose(2, 1, 0))
    pos = np.arange(T, dtype=np.float64)

    def rope_tab(half):
        inv = 10000.0 ** (-np.arange(half, dtype=np.float64) / half)
        ang = (pos[:, None].astype(f) * inv[None, :].astype(f)).astype(f)
        return np.stack([np.cos(ang), np.sin(ang)], axis=1).astype(f)

    ra, ri = rope_tab(64), rope_tab(32)
    p = np.arange(128)
    p64 = np.arange(64)
    tri64 = (p64[:, None] <= p64[None, :]).astype(f)
    ident = np.eye(128, dtype=f)
    pw2 = np.tile((2.0 ** -(np.arange(NBIS) + 1.0)).astype(f)[None, :], (128, 1))
    ustri = (p[:, None] < p[None, :]).astype(f)
    eoff = np.tile((np.arange(NEXP) * CAP).astype(f)[None, :], (128, 1))
    vecs = np.stack([np.asarray(inputs[k], f).reshape(-1) for k in
                     ("ln_in_g", "ln_in_b", "ln1_g", "ln1_b", "ln2_g", "ln2_b")])
    bg = np.asarray(inputs["b_gate"], f)[0]
    bu = np.asarray(inputs["b_up"], f)[0]
    bgu = np.zeros((128, NEXP, 16), f)
    for e_ in range(NEXP):
        bgu[:, e_, 0:8] = _col(bg[e_], 8)
        bgu[:, e_, 8:16] = _col(bu[e_], 8)
    m = np.arange(128)[:, None] // 16
    s = np.arange(512)[None, :] // 64
    admb = np.where(s <= m, 0.0, -3.0e38).astype(f)
    shared = dict(w_in=w_in, b_in=b_in[None, :], cols=cols, conv_wc=conv_wc, rope_a=ra, rope_i=ri, tri64=tri64,
                  ident=ident, pw2=pw2, admb=admb, m_norm_g=np.asarray(inputs["m_norm_g"], f).reshape(1, D),
                  w_out=np.ascontiguousarray(np.asarray(inputs["w_out"], f)[0]), vecs=vecs,
                  w_router=np.ascontiguousarray(np.asarray(inputs["w_router"], f)[0]),
                  b_router=np.asarray(inputs["b_router"], f).reshape(1, NEXP),
                  w_gate=np.ascontiguousarray(np.asarray(inputs["w_gate"], f)[0]),
                  w_up=np.ascontiguousarray(np.asarray(inputs["w_up"], f)[0]),
                  w_down=np.ascontiguousarray(np.asarray(inputs["w_down"], f)[0]),
                  bgu_col=bgu, b_down=np.ascontiguousarray(np.asarray(inputs["b_down"], f)[0]),
                  eoff=eoff, ustri=ustri)
    maps = []
    for c in range(8):
        b, j = (c // 4) % B, c % 4
        oi = _own_idx(T, j)
        sel = np.zeros((128, 32), f)
        trisel = np.zeros((8, 64, 128), f)
        for ch in range(2):
            for r in range(16):
                sel[ch * 64 + 16 * j + r, ch * 16 + r] = 1.0 / 16.0
        for cp in range(8):
            for r in range(16):
                trisel[cp, 0:16 * j + r + 1, cp * 16 + r] = 1.0
        cmask = (np.arange(64)[:, None] <= (16 * j + np.arange(16))[None, :]).astype(f)
        d = dict(shared)
        d.update(x_all=np.ascontiguousarray(x[b, :T]), x_own=np.ascontiguousarray(x[b, oi]),
                 rope_ao=np.ascontiguousarray(ra[oi]), rope_io=np.ascontiguousarray(ri[oi]),
                 sel=sel, trisel=trisel, cmask=cmask)
        maps.append(d)
    return maps


def assemble(results, T, B):
    out = np.zeros((B, T, D), np.float32)
    for c in range(8):
        b, j = c // 4, c % 4
        if b < B:
            out[b, _own_idx(T, j)] = results[c]["out"]
    return out


_NC_CACHE = {}


def kernel(**inputs):
    T = inputs["x"].shape[1]
    B = inputs["x"].shape[0]
    if T not in _NC_CACHE:
        _NC_CACHE[T] = build(T)
    nc = _NC_CACHE[T]
    maps = [{k: m[k] for k in nc.used_inputs} for m in prep(inputs, T)]
    res = run_bass_kernel_spmd(nc, maps, core_ids=list(range(8)))
    return assemble(res.results, T, B)
```

```python
import numpy as np
from contextlib import ExitStack
import concourse.bass as bass
import concourse.mybir as mybir
from concourse.bass_utils import run_bass_kernel_spmd

F32 = mybir.dt.float32
F32R = mybir.dt.float32r
BF16 = mybir.dt.bfloat16
I32 = mybir.dt.int32
U32 = mybir.dt.uint32
AF = mybir.ActivationFunctionType
ALU = mybir.AluOpType
AX = mybir.AxisListType

D = 1024
INW = 9808
NEXP = 32
CAP = 384
NBIS = 22
NDMASEM = 40
LN_EPS = 1e-5
ALPHA = 2.0 ** 0.25

O_MQ, O_MK, O_MV, O_MO, O_MI, O_MF = 0, 1024, 2048, 3072, 4096, 4100
O_AQ, O_AK, O_AV, O_XQ, O_XK, O_XW, O_GM, O_GA = 4104, 5128, 6152, 7176, 7688, 7752, 7760, 8784


class Buf:
    __slots__ = ("w", "r")

    def __init__(self):
        self.w = {}
        self.r = {}


class Tl:
    def __init__(self, t):
        self.t = t
        self.b = Buf()

    def __getitem__(self, k):
        return self.t[k]


class KB:
    def __init__(self, nc, es):
        self.nc = nc
        self.engs = {"pe": nc.tensor, "act": nc.scalar, "dve": nc.vector, "pool": nc.gpsimd, "sp": nc.sync}
        self.sem = {k: es.enter_context(nc.semaphore("sem_" + k)) for k in self.engs}
        self.cnt = {k: 0 for k in self.engs}
        self.seen = {}
        self.dsem = [es.enter_context(nc.semaphore("dq%d" % i)) for i in range(NDMASEM)]
        self.dval = [0] * NDMASEM
        self.qpool = {"sp": list(range(0, 20)), "pool": list(range(20, 36)), "act": list(range(36, 40))}
        self.qrr = {"sp": 0, "pool": 0, "act": 0}
        self.uid = 0

    def sb(self, es, shape, dt, name=None):
        self.uid += 1
        return Tl(es.enter_context(self.nc.sbuf_tensor("%s_%d" % (name or "t", self.uid), list(shape), dt)))

    def ps(self, es, shape, dt=F32, name=None):
        self.uid += 1
        return Tl(es.enter_context(self.nc.psum_tensor("%s_%d" % (name or "p", self.uid), list(shape), dt)))

    def _wait(self, e, tok):
        sem, val, key = tok
        if self.seen.get((e, key), 0) >= val:
            return
        self.seen[(e, key)] = val
        self.engs[e].wait_ge(sem, val)

    def _deps(self, e, reads, writes):
        toks = []
        for b in reads:
            toks.extend(b.b.w.values())
        for b in writes:
            toks.extend(b.b.w.values())
            toks.extend(b.b.r.values())
        for t in toks:
            if t[2] == "pe" and e == "pe":
                continue
            self._wait(e, t)

    def _mark(self, tok, reads, writes):
        for b in reads:
            b.b.r[tok[2]] = tok
        for b in writes:
            b.b.w[tok[2]] = tok
            b.b.r = {}

    def op(self, e, fn, reads=(), writes=()):
        self._deps(e, reads, writes)
        ins = fn(self.engs[e])
        self.cnt[e] += 1
        ins.then_inc(self.sem[e], 1)
        self._mark((self.sem[e], self.cnt[e], e), reads, writes)

    def dma(self, q, out, in_, reads=(), writes=(), indirect=None):
        pl = self.qpool[q]
        i = pl[self.qrr[q] % len(pl)]
        self.qrr[q] += 1
        key = "d%d" % i
        if self.dval[i] > 0:
            self._wait(q, (self.dsem[i], self.dval[i], key))
        self._deps(q, reads, writes)
        if indirect is None:
            ins = self.engs[q].dma_start(out=out, in_=in_)
        else:
            ins = indirect(self.engs[q])
        self.dval[i] += 16
        ins.then_inc(self.dsem[i], 16)
        self._mark((self.dsem[i], self.dval[i], key), reads, writes)

    def barrier_all(self, bufs):
        for e in self.engs:
            self._deps(e, bufs, ())


def _r(ap):
    return ap.bitcast(F32R)


def _f(ap):
    return ap.bitcast(F32)


def build(T, debug=None, upto="all"):
    NT = T // 128
    NG = T // 512
    NO = T // 4
    NOT = NO // 128
    NCH = T // 64
    nc = bass.Bass("TRN2", target_bir_lowering=False)

    used_inputs = []

    def din(name, shape, dt=F32):
        used_inputs.append(name)
        return nc.dram_tensor(name, list(shape), dt, kind="ExternalInput").ap()

    def dscr(name, shape, dt=F32):
        return nc.dram_tensor(name, list(shape), dt, kind="Internal").ap()

    x_all = din("x_all", [T, D])
    x_own = din("x_own", [NO, D])
    w_in = din("w_in", [D, INW], F32R)
    b_in = din("b_in", [1, INW])
    cols = din("cols", [128, 128])
    conv_wc = din("conv_wc", [128, 16, 4])
    rope_a = din("rope_a", [T, 2, 64])
    rope_i = din("rope_i", [T, 2, 32])
    rope_ao = din("rope_ao", [NO, 2, 64])
    rope_io = din("rope_io", [NO, 2, 32])
    sel = din("sel", [128, 32], F32R)
    trisel = din("trisel", [8, 64, 128])
    tri64 = din("tri64", [64, 64])
    cmask = din("cmask", [64, 16])
    ident = din("ident", [128, 128])
    pw2 = din("pw2", [128, NBIS])
    admb = din("admb", [128, 512])
    m_norm_g = din("m_norm_g", [1, D])
    w_out = din("w_out", [D, D], F32R)
    vecs = din("vecs", [6, D])
    w_router = din("w_router", [D, NEXP])
    b_router = din("b_router", [1, NEXP])
    if upto == "all":
        w_gate = din("w_gate", [NEXP, D, D], F32R)
        w_up = din("w_up", [NEXP, D, D], F32R)
        w_down = din("w_down", [NEXP, D, D], F32R)
    bgu_col = din("bgu_col", [128, NEXP, 16])
    b_down = din("b_down", [NEXP, D])
    eoff = din("eoff", [128, NEXP])
    ustri = din("ustri", [128, 128])
    out = nc.dram_tensor("out", [NO, D], F32, kind="ExternalOutput").ap()

    xnT_d = dscr("xnT_d", [8, 128, T])
    xnTo_d = dscr("xnTo_d", [8, 128, NO])
    kT_d = dscr("kT_d", [8, 128, T])
    ktok_d = dscr("ktok_d", [T, D])
    qTo_d = dscr("qTo_d", [8, 128, NO])
    v_d = dscr("v_d", [T, D])
    ikT_d = dscr("ikT_d", [64, T])
    g_d = dscr("g_d", [T, 8])
    mg_d = dscr("mg_d", [NO, D])
    akT_d = dscr("akT_d", [8, 128, T], BF16)
    av_d = dscr("av_d", [8, 128, T // 128, 130], BF16)
    aqT_d = dscr("aqT_d", [NOT, 128, 8, 128], BF16)
    iqT_d = dscr("iqT_d", [NOT, 64, 8, 128])
    og_d = dscr("og_d", [NO, D])
    ga_d = dscr("ga_d", [NO, D])
    ym_d = dscr("ym_d", [NO, D])
    h1_d = dscr("h1_d", [NO, D])
    xs_d = dscr("xs_d", [NEXP * CAP, D])
    ys_d = dscr("ys_d", [NEXP * CAP, D])
    dbg = {}
    if debug:
        for nm, shp in debug.items():
            dbg[nm] = nc.dram_tensor("dbg_" + nm, list(shp), F32, kind="ExternalOutput").ap()

    with ExitStack() as es0:
        kb = KB(nc, es0)
        V = nc.vector
        c_cols = kb.sb(es0, [128, 128], F32, "cols")
        c_ident = kb.sb(es0, [128, 128], F32, "ident")
        c_identb = kb.sb(es0, [128, 128], BF16, "identb")
        kb.dma("sp", c_cols[:], cols, writes=[c_cols])
        kb.dma("sp", c_ident[:], ident, writes=[c_ident])
        kb.op("dve", lambda e: e.tensor_copy(out=c_identb[:], in_=c_ident[:]), [c_ident], [c_identb])
        GCOL, BCOL, BQK, CBC = 0, 8, 16, 32

        def layernorm_stats(es, xt, tmp_pool):
            st, mv = tmp_pool
            kb.op("dve", lambda e: e.bn_stats(out=st[:, 0, :], in_=xt[:, 0:512]), [xt], [st])
            kb.op("dve", lambda e: e.bn_stats(out=st[:, 1, :], in_=xt[:, 512:1024]), [xt], [st])
            kb.op("dve", lambda e: e.bn_aggr(out=mv[:, 0:2], in_=st[:].rearrange("p a b -> p (a b)")), [st], [mv])
            kb.op("dve", lambda e: e.tensor_scalar(out=mv[:, 2:3], in0=mv[:, 1:2], scalar1=LN_EPS, scalar2=None,
                                                  op0=ALU.add), [mv], [mv])
            kb.op("act", lambda e: e.activation(out=mv[:, 2:3], in_=mv[:, 2:3], func=AF.Sqrt), [mv], [mv])
            kb.op("dve", lambda e: e.reciprocal(out=mv[:, 3:4], in_=mv[:, 2:3]), [mv], [mv])
            return mv[:, 0:1], mv[:, 3:4]

        def phase_ln_T(xsrc, ntiles, dst):
            with ExitStack() as es:
                xts = [kb.sb(es, [128, D], F32, "xt") for _ in range(3)]
                xhs = [kb.sb(es, [128, D], F32, "xh") for _ in range(2)]
                sts = [(kb.sb(es, [128, 2, 6], F32, "st"), kb.sb(es, [128, 4], F32, "mv")) for _ in range(2)]
                xns = [kb.sb(es, [128, 8, 512], F32, "xn") for _ in range(2)]
                pts = [kb.ps(es, [128, 512], F32, "pt") for _ in range(4)]
                for tt in range(ntiles):
                    xt, xh, stp, xn = xts[tt % 3], xhs[tt % 2], sts[tt % 2], xns[(tt // 4) % 2]
                    t4 = tt % 4
                    kb.dma("pool", xt[:], xsrc[tt * 128:(tt + 1) * 128, :], writes=[xt])
                    mean, rstd = layernorm_stats(es, xt, stp)
                    kb.op("dve", lambda e: e.tensor_scalar(out=xh[:], in0=xt[:], scalar1=mean, scalar2=rstd,
                                                          op0=ALU.subtract, op1=ALU.mult), [xt, stp[1]], [xh])
                    for half in range(2):
                        pt = pts[(tt % 2) * 2 + half]
                        for k in range(4):
                            dc = half * 4 + k
                            kb.op("pe", lambda e: e.transpose(out=pt[:, k * 128:(k + 1) * 128],
                                                              in_=xh[:, dc * 128:(dc + 1) * 128], identity=c_ident[:]),
                                  [xh, c_ident], [pt])
                        for k in range(4):
                            dc = half * 4 + k
                            kb.op("act", lambda e: e.activation(out=xn[:, dc, t4 * 128:(t4 + 1) * 128], in_=pt[:, k * 128:(k + 1) * 128],
                                                                func=AF.Identity, scale=c_cols[:, GCOL + dc:GCOL + dc + 1],
                                                                bias=c_cols[:, BCOL + dc:BCOL + dc + 1]),
                                  [pt, c_cols], [xn])
                    if t4 == 3:
                        g4 = tt // 4
                        kb.dma("sp", dst[:, :, g4 * 512:(g4 + 1) * 512].rearrange("c p t -> p c t"), xn[:], reads=[xn])

        def fence():
            for e in kb.engs:
                for i in range(NDMASEM):
                    if kb.dval[i] > 0:
                        kb._wait(e, (kb.dsem[i], kb.dval[i], "d%d" % i))

        def barrier():
            fence()
            for e in kb.engs:
                for e2 in kb.engs:
                    if kb.cnt[e2] > 0 and e2 != e:
                        kb._wait(e, (kb.sem[e2], kb.cnt[e2], e2))

        phase_ln_T(x_all, NT, xnT_d)
        barrier()
        phase_ln_T(x_own, NOT, xnTo_d)

        barrier()

        def w_blocks(W, ncols, specs):
            nb = (ncols + 511) // 512
            Wb = [Tl(None) for _ in range(nb)]
            pieces = []
            for (src0, n, dst0) in specs:
                o = 0
                while o < n:
                    d = dst0 + o
                    m = min(512 - (d % 512), n - o)
                    pieces.append((src0 + o, m, d))
                    o += m
            pieces.sort(key=lambda t: t[2])

            def emit(sel_):
                for (s0, m, d0) in pieces:
                    if sel_(d0 // 512):
                        kb.dma("pool", W[:, :, d0:d0 + m], w_in[:, s0:s0 + m].rearrange("(c p) n -> p c n", p=128),
                               writes=[Wb[d0 // 512]])
            return Wb, (lambda: emit(lambda b_: b_ == 0)), (lambda: emit(lambda b_: b_ > 0))

        with ExitStack() as es:
            W = kb.sb(es, [128, 8, 2048], F32R, "Wqk")
            Wb, w_first, w_rest = w_blocks(W, 2048, [(0, 2048, 0)])
            w_first()
            c_cw = kb.sb(es, [128, 16, 4], F32, "cw")
            kb.dma("sp", c_cw[:], conv_wc, writes=[c_cw])
            c_sel = kb.sb(es, [128, 32], F32R, "sel")
            kb.dma("pool", c_sel[:], sel, writes=[c_sel])
            xgs = [kb.sb(es, [128, 8, 512], F32R, "xg") for _ in range(2)]
            pre = kb.sb(es, [128, 16, 516], F32, "pre")
            preb = [Tl(None) for _ in range(16)]
            kb.op("dve", lambda e: e.memset(pre[:], 0.0), [], [pre] + preb)
            accs = [kb.sb(es, [128, 512], F32, "acc") for _ in range(2)]
            sqs = [kb.sb(es, [128, 512], F32, "sq") for _ in range(2)]
            qtok = kb.sb(es, [128, 4, D], F32R, "qtok")
            ktok = kb.sb(es, [128, 4, D], F32, "ktok")
            qTo = kb.sb(es, [128, 8, 128], F32, "qTo")
            pms = [kb.ps(es, [128, 512], F32, "pm") for _ in range(3)]
            ptr = [kb.ps(es, [128, 512], F32, "ptr") for _ in range(2)]
            psels = [kb.ps(es, [128, 512], F32, "psel") for _ in range(2)]
            pend_a1 = []
            for g in range(NG):
                xg = xgs[g % 2]
                kb.dma("pool", xg[:], _r(xnT_d[:, :, g * 512:(g + 1) * 512].rearrange("c p t -> p c t")), writes=[xg])
                if g == 0:
                    w_rest()
                pend_silu = []

                def emit_silu(cc):
                    acc, sq = accs[cc % 2], sqs[cc % 2]
                    kb.op("act", lambda e: e.activation(out=sq[:], in_=acc[:], func=AF.Silu), [acc], [sq])
                    if cc >= 8:
                        kb.dma("sp", kT_d[cc - 8, :, g * 512:(g + 1) * 512], sq[:], reads=[sq])

                    def tail(cc=cc, sq=sq):
                        pt = ptr[cc % 2]
                        for tk in range(4):
                            kb.op("pe", lambda e: e.transpose(out=pt[:, tk * 128:(tk + 1) * 128],
                                                              in_=sq[:, tk * 128:(tk + 1) * 128], identity=c_ident[:]),
                                  [sq, c_ident], [pt])
                        dst = qtok if cc < 8 else ktok
                        c8 = cc % 8
                        kb.op("dve", lambda e: e.tensor_copy(out=dst[:, :, c8 * 128:(c8 + 1) * 128],
                                                             in_=pt[:].rearrange("p (k c) -> p k c", k=4)), [pt], [dst])
                    pend_a1.append(tail)

                for cc in range(16):
                    pm = pms[cc % 3]
                    acc = accs[cc % 2]
                    pb = preb[cc]
                    for dc in range(8):
                        kb.op("pe", lambda e: e.matmul(pm[:], W[:, dc, cc * 128:(cc + 1) * 128], xg[:, dc, :],
                                                       start=(dc == 0), stop=(dc == 7)), [Wb[cc // 4], xg], [pm])
                    while pend_a1:
                        pend_a1.pop(0)()
                    kb.op("act", lambda e: e.activation(out=pre[:, cc, 0:4], in_=pre[:, cc, 512:516], func=AF.Identity),
                          [pb], [pb])
                    kb.op("act", lambda e: e.activation(out=pre[:, cc, 4:516], in_=pm[:], func=AF.Identity,
                                                        bias=c_cols[:, BQK + cc:BQK + cc + 1]), [pm, c_cols], [pb])
                    kb.op("act", lambda e: e.activation(out=acc[:], in_=pre[:, cc, 4:516], func=AF.Identity,
                                                        scale=c_cw[:, cc, 3:4], bias=c_cols[:, CBC + cc:CBC + cc + 1]),
                          [pb, c_cw, c_cols], [acc])
                    for j in range(3):
                        kb.op("dve", lambda e: e.scalar_tensor_tensor(out=acc[:], in0=pre[:, cc, 1 + j:513 + j],
                                                                      scalar=c_cw[:, cc, j:j + 1], in1=acc[:],
                                                                      op0=ALU.mult, op1=ALU.add), [pb, c_cw, acc], [acc])
                    while pend_silu:
                        emit_silu(pend_silu.pop(0))
                    pend_silu.append(cc)
                while pend_silu:
                    emit_silu(pend_silu.pop(0))
                while pend_a1:
                    pend_a1.pop(0)()
                kb.dma("sp", ktok_d[g * 512:(g + 1) * 512, :].rearrange("(k p) n -> p k n", p=128), ktok[:], reads=[ktok])
                for tk in range(4):
                    psl = psels[tk % 2]
                    for c8 in range(8):
                        kb.op("pe", lambda e: e.matmul(psl[:, c8 * 32:(c8 + 1) * 32], qtok[:, tk, c8 * 128:(c8 + 1) * 128],
                                                       c_sel[:], start=True, stop=True), [qtok, c_sel], [psl])
                    kb.op("act", lambda e: e.activation(out=qTo[:, :, tk * 32:(tk + 1) * 32],
                                                        in_=psl[:, 0:256].rearrange("p (c m) -> p c m", c=8),
                                                        func=AF.Identity), [psl], [qTo])
                kb.dma("sp", qTo_d[:, :, g * 128:(g + 1) * 128].rearrange("c p t -> p c t"), qTo[:], reads=[qTo])
        fence()
        def finish():
            fence()
            for e in kb.engs:
                for e2 in kb.engs:
                    if kb.cnt[e2] > 0:
                        kb._wait(e, (kb.sem[e2], kb.cnt[e2], e2))

        if upto == "A":
            for nm, src in (("kT", kT_d[:, :, :].rearrange("c p t -> (c p) t")), ("qTo", qTo_d[:, :, :].rearrange("c p t -> (c p) t")),
                            ("ktok", ktok_d)):
                if nm in dbg:
                    kb.dma("sp", dbg[nm], src)
            finish()
            nc.used_inputs = used_inputs
            return nc
        def bcast_load(es, src_row_ap, n, name):
            t = kb.sb(es, [128, n], F32, name)
            kb.dma("sp", t[:], src_row_ap.partition_broadcast(128), writes=[t])
            return t

        def rope(es, src, dst, cos, sin, nh, half, tmp):
            shp = [128, nh, half]
            cb = cos.unsqueeze(1).to_broadcast(shp)
            sb_ = sin.unsqueeze(1).to_broadcast(shp)
            x1, x2 = src[0], src[1]
            t = tmp
            kb.op("dve", lambda e: e.tensor_tensor(out=t[:, :, 0:half], in0=x1, in1=cb, op=ALU.mult), src[2], [t])
            kb.op("dve", lambda e: e.tensor_tensor(out=t[:, :, half:2 * half], in0=x2, in1=sb_, op=ALU.mult), src[2], [t])
            kb.op("dve", lambda e: e.tensor_tensor(out=dst[0], in0=t[:, :, 0:half], in1=t[:, :, half:2 * half],
                                                   op=ALU.subtract), [t], dst[2])
            kb.op("dve", lambda e: e.tensor_tensor(out=t[:, :, 0:half], in0=x2, in1=cb, op=ALU.mult), src[2], [t])
            kb.op("dve", lambda e: e.tensor_tensor(out=t[:, :, half:2 * half], in0=x1, in1=sb_, op=ALU.mult), src[2], [t])
            kb.op("dve", lambda e: e.tensor_tensor(out=dst[1], in0=t[:, :, 0:half], in1=t[:, :, half:2 * half],
                                                   op=ALU.add), [t], dst[2])

        WQ = kb.sb(es0, [128, NOT, 8], F32, "WQ")
        barrier()
        with ExitStack() as es:
            NC2 = 1096
            W = kb.sb(es, [128, 8, NC2], F32R, "W2")
            Wb, w_first, w_rest = w_blocks(W, NC2, [(O_MV, 1024, 0), (O_MI, 8, 1024), (O_XK, 64, 1032)])
            w_first()
            bias = kb.sb(es, [128, NC2], F32, "bias2")
            kb.dma("sp", bias[:, 0:1024], b_in[0, O_MV:O_MV + 1024].partition_broadcast(128), writes=[bias])
            kb.dma("sp", bias[:, 1024:1032], b_in[0, O_MI:O_MI + 8].partition_broadcast(128), writes=[bias])
            kb.dma("sp", bias[:, 1032:1096], b_in[0, O_XK:O_XK + 64].partition_broadcast(128), writes=[bias])
            xgs = [kb.sb(es, [128, 8, 512], F32R, "xg") for _ in range(2)]
            vts = [kb.sb(es, [128, 4, D], F32, "vt") for _ in range(2)]
            gxs = [kb.sb(es, [128, 4, 72], F32, "gx") for _ in range(2)]
            rps = [kb.sb(es, [128, 4, 2, 32], F32, "rp") for _ in range(2)]
            xkr = kb.sb(es, [128, 4, 64], F32, "xkr")
            rtmp = kb.sb(es, [128, 4, 64], F32, "rtmp")
            ikg = kb.sb(es, [64, 512], F32, "ikg")
            pA = [kb.ps(es, [128, 512], F32, "pA") for _ in range(6)]
            pT = kb.ps(es, [128, 512], F32, "pT")
            for g in range(NG):
                xg, vt, gx, rp = xgs[g % 2], vts[g % 2], gxs[g % 2], rps[g % 2]
                kb.dma("pool", xg[:], _r(xnT_d[:, :, g * 512:(g + 1) * 512].rearrange("c p t -> p c t")), writes=[xg])
                if g == 0:
                    w_rest()
                kb.dma("sp", rp[:], rope_i[g * 512:(g + 1) * 512].rearrange("(k p) c d -> p k c d", p=128), writes=[rp])
                for tk in range(4):
                    ps3 = [pA[(tk % 2) * 3 + q] for q in range(3)]
                    for q, (c0, c1) in enumerate(((0, 512), (512, 1024), (1024, 1096))):
                        for dc in range(8):
                            kb.op("pe", lambda e: e.matmul(ps3[q][:, 0:c1 - c0], xg[:, dc, tk * 128:(tk + 1) * 128],
                                                           W[:, dc, c0:c1], start=(dc == 0), stop=(dc == 7)),
                                  [xg, Wb[q]], [ps3[q]])
                    kb.op("dve", lambda e: e.tensor_tensor(out=vt[:, tk, 0:512], in0=ps3[0][:], in1=bias[:, 0:512],
                                                           op=ALU.add), [ps3[0], bias], [vt])
                    kb.op("dve", lambda e: e.tensor_tensor(out=vt[:, tk, 512:1024], in0=ps3[1][:], in1=bias[:, 512:1024],
                                                           op=ALU.add), [ps3[1], bias], [vt])
                    kb.op("dve", lambda e: e.tensor_tensor(out=gx[:, tk, :], in0=ps3[2][:, 0:72], in1=bias[:, 1024:1096],
                                                           op=ALU.add), [ps3[2], bias], [gx])
                kb.dma("sp", v_d[g * 512:(g + 1) * 512, :].rearrange("(k p) n -> p k n", p=128), vt[:], reads=[vt])
                kb.dma("sp", g_d[g * 512:(g + 1) * 512, :].rearrange("(k p) n -> p k n", p=128), gx[:, :, 0:8], reads=[gx])
                for tk in range(4):
                    cosv, sinv = rp[:, tk, 0, :], rp[:, tk, 1, :]
                    x1, x2 = gx[:, tk, 8:40], gx[:, tk, 40:72]
                    kb.op("dve", lambda e: e.tensor_tensor(out=rtmp[:, tk, 0:32], in0=x1, in1=cosv, op=ALU.mult), [gx, rp], [rtmp])
                    kb.op("dve", lambda e: e.tensor_tensor(out=rtmp[:, tk, 32:64], in0=x2, in1=sinv, op=ALU.mult), [gx, rp], [rtmp])
                    kb.op("dve", lambda e: e.tensor_tensor(out=xkr[:, tk, 0:32], in0=rtmp[:, tk, 0:32], in1=rtmp[:, tk, 32:64],
                                                           op=ALU.subtract), [rtmp], [xkr])
                    kb.op("dve", lambda e: e.tensor_tensor(out=rtmp[:, tk, 0:32], in0=x2, in1=cosv, op=ALU.mult), [gx, rp], [rtmp])
                    kb.op("dve", lambda e: e.tensor_tensor(out=rtmp[:, tk, 32:64], in0=x1, in1=sinv, op=ALU.mult), [gx, rp], [rtmp])
                    kb.op("dve", lambda e: e.tensor_tensor(out=xkr[:, tk, 32:64], in0=rtmp[:, tk, 0:32], in1=rtmp[:, tk, 32:64],
                                                           op=ALU.add), [rtmp], [xkr])
                for tk in range(4):
                    kb.op("pe", lambda e: e.transpose(out=pT[0:64, tk * 128:(tk + 1) * 128], in_=xkr[:, tk, :],
                                                      identity=c_ident[:]), [xkr, c_ident], [pT])
                kb.op("act", lambda e: e.activation(out=ikg[:], in_=pT[0:64, :], func=AF.Identity), [pT], [ikg])
                kb.dma("sp", ikT_d[:, g * 512:(g + 1) * 512], ikg[:], reads=[ikg])
        barrier()
        with ExitStack() as es:
            W = kb.sb(es, [128, 8, 2048], F32R, "W3")
            Wb, w_first, w_rest = w_blocks(W, 2048, [(O_AK, 2048, 0)])
            w_first()
            bias = bcast_load(es, b_in[0, O_AK:O_AK + 2048], 2048, "bias3")
            xgs = [kb.sb(es, [128, 8, 512], F32R, "xg") for _ in range(2)]
            rps = [kb.sb(es, [128, 4, 2, 64], F32, "rpa") for _ in range(2)]
            ak = kb.sb(es, [128, 8, 128], F32, "ak")
            akrs = [kb.sb(es, [128, 8, 128], BF16, "akr") for _ in range(2)]
            pend_a3 = []
            rtmp = kb.sb(es, [128, 8, 128], F32, "rtmp3")
            akTs = [kb.sb(es, [128, 8, 512], BF16, "akT") for _ in range(2)]
            vaugs = [kb.sb(es, [128, 8, 4, 130], BF16, "vaug") for _ in range(2)]
            for va in vaugs:
                kb.op("dve", lambda e: e.memset(va[:], 1.0), [], [va])
            pA = [kb.ps(es, [128, 512], F32, "pA3") for _ in range(4)]
            pTb = [kb.ps(es, [128, 1024], BF16, "pTb") for _ in range(2)]
            for g in range(NG):
                xg, rp, akT, va = xgs[g % 2], rps[g % 2], akTs[g % 2], vaugs[g % 2]
                kb.dma("pool", xg[:], _r(xnT_d[:, :, g * 512:(g + 1) * 512].rearrange("c p t -> p c t")), writes=[xg])
                if g == 0:
                    w_rest()
                kb.dma("sp", rp[:], rope_a[g * 512:(g + 1) * 512].rearrange("(k p) c d -> p k c d", p=128), writes=[rp])
                for tk in range(4):
                    akr = akrs[tk % 2]
                    for q in range(4):
                        for dc in range(8):
                            kb.op("pe", lambda e: e.matmul(pA[q][:], xg[:, dc, tk * 128:(tk + 1) * 128],
                                                           W[:, dc, q * 512:(q + 1) * 512], start=(dc == 0), stop=(dc == 7)),
                                  [xg, Wb[q]], [pA[q]])
                    while pend_a3:
                        pend_a3.pop(0)()
                    akf = ak[:].rearrange("p h d -> p (h d)")
                    for q in range(2):
                        kb.op("dve", lambda e: e.tensor_tensor(out=akf[:, q * 512:(q + 1) * 512], in0=pA[q][:],
                                                               in1=bias[:, q * 512:(q + 1) * 512], op=ALU.add),
                              [pA[q], bias], [ak])
                    for q in range(2):
                        kb.op("dve", lambda e: e.tensor_tensor(out=va[:, q * 4:(q + 1) * 4, tk, 0:128],
                                                               in0=pA[2 + q][:].rearrange("p (h d) -> p h d", h=4),
                                                               in1=bias[:, 1024 + q * 512:1024 + (q + 1) * 512].rearrange("p (h d) -> p h d", h=4),
                                                               op=ALU.add), [pA[2 + q], bias], [va])
                    rope(es, (ak[:, :, 0:64], ak[:, :, 64:128], [ak, rp]), (akr[:, :, 0:64], akr[:, :, 64:128], [akr]),
                         rp[:, tk, 0, :], rp[:, tk, 1, :], 8, 64, rtmp)
                    def tail(g=g, tk=tk, akr=akr, akT=akT, va=va):
                        ptb = pTb[tk % 2]
                        for h in range(8):
                            kb.op("pe", lambda e: e.transpose(out=ptb[:, h * 128:(h + 1) * 128], in_=akr[:, h, :],
                                                              identity=c_identb[:]), [akr, c_identb], [ptb])
                        kb.op("act", lambda e: e.activation(out=akT[:, :, tk * 128:(tk + 1) * 128],
                                                            in_=ptb[:].rearrange("p (h t) -> p h t", h=8), func=AF.Identity),
                              [ptb], [akT])
                        if tk == 3:
                            kb.dma("sp", akT_d[:, :, g * 512:(g + 1) * 512].rearrange("h p t -> p h t"), akT[:], reads=[akT])
                            kb.dma("sp", av_d[:, :, g * 4:(g + 1) * 4, :].rearrange("h p k c -> p h k c"), va[:], reads=[va])
                    pend_a3.append(tail)
            while pend_a3:
                pend_a3.pop(0)()
        def q_phase(col_specs, ncols, consume):
            barrier()
            with ExitStack() as es:
                W = kb.sb(es, [128, 8, ncols], F32R, "WQp")
                bias = kb.sb(es, [128, ncols], F32, "biasq")
                Wb, w_first, w_rest = w_blocks(W, ncols, col_specs)
                w_first()
                for (src0, n, dst0) in col_specs:
                    kb.dma("sp", bias[:, dst0:dst0 + n], b_in[0, src0:src0 + n].partition_broadcast(128), writes=[bias])
                xos = [kb.sb(es, [128, 8, 128], F32R, "xo") for _ in range(2)]
                nb = (ncols + 511) // 512
                pQ = [kb.ps(es, [128, 512], F32, "pQ") for _ in range(4)]
                pend_q = []
                for ot in range(NOT):
                    xo = xos[ot % 2]
                    kb.dma("pool", xo[:], _r(xnTo_d[:, :, ot * 128:(ot + 1) * 128].rearrange("c p t -> p c t")), writes=[xo])
                    if ot == 0:
                        w_rest()
                    for q in range(nb):
                        c0, c1 = q * 512, min(ncols, (q + 1) * 512)
                        for dc in range(8):
                            kb.op("pe", lambda e: e.matmul(pQ[q][:, 0:c1 - c0], xo[:, dc, :], W[:, dc, c0:c1],
                                                           start=(dc == 0), stop=(dc == 7)), [xo, Wb[q]], [pQ[q]])
                    while pend_q:
                        pend_q.pop(0)()
                    tl = consume(es, ot, pQ, bias)
                    if tl is not None:
                        pend_q.append(tl)
                while pend_q:
                    pend_q.pop(0)()

        barrier()
        with ExitStack() as esq:
            aq = kb.sb(esq, [128, 8, 128], F32, "aq")
            aqrs = [kb.sb(esq, [128, 8, 128], BF16, "aqr") for _ in range(2)]
            xq = kb.sb(esq, [128, 8, 64], F32, "xq")
            xqrs = [kb.sb(esq, [128, 8, 64], F32, "xqr") for _ in range(2)]
            rtq = kb.sb(esq, [128, 8, 128], F32, "rtq")
            rpa = kb.sb(esq, [128, 2, 64], F32, "rpao")
            rpi = kb.sb(esq, [128, 2, 32], F32, "rpio")
            aqT = kb.sb(esq, [128, 8, 128], BF16, "aqT")
            iqT = kb.sb(esq, [64, 8, 128], F32, "iqT")
            ptb = kb.ps(esq, [128, 1024], BF16, "ptbq")
            pti = [kb.ps(esq, [128, 512], F32, "ptiq") for _ in range(2)]

            def consume_q1(es, ot, pQ, bias):
                aqr, xqr = aqrs[ot % 2], xqrs[ot % 2]
                kb.dma("sp", rpa[:], rope_ao[ot * 128:(ot + 1) * 128], writes=[rpa])
                kb.dma("sp", rpi[:], rope_io[ot * 128:(ot + 1) * 128], writes=[rpi])
                aqf = aq[:].rearrange("p h d -> p (h d)")
                for q in range(2):
                    kb.op("dve", lambda e: e.tensor_tensor(out=aqf[:, q * 512:(q + 1) * 512], in0=pQ[q][:],
                                                           in1=bias[:, q * 512:(q + 1) * 512], op=ALU.add), [pQ[q], bias], [aq])
                kb.op("dve", lambda e: e.tensor_tensor(out=xq[:].rearrange("p h d -> p (h d)"), in0=pQ[2][:],
                                                       in1=bias[:, 1024:1536], op=ALU.add), [pQ[2], bias], [xq])
                kb.op("dve", lambda e: e.tensor_tensor(out=WQ[:, ot, :], in0=pQ[3][:, 0:8], in1=bias[:, 1536:1544],
                                                       op=ALU.add), [pQ[3], bias], [WQ])
                kb.op("dve", lambda e: e.tensor_scalar(out=WQ[:, ot, :], in0=WQ[:, ot, :], scalar1=float(8 ** -0.5 * 64 ** -0.5),
                                                      scalar2=None, op0=ALU.mult), [WQ], [WQ])
                rope(es, (aq[:, :, 0:64], aq[:, :, 64:128], [aq, rpa]), (aqr[:, :, 0:64], aqr[:, :, 64:128], [aqr]),
                     rpa[:, 0, :], rpa[:, 1, :], 8, 64, rtq)
                rope(es, (xq[:, :, 0:32], xq[:, :, 32:64], [xq, rpi]), (xqr[:, :, 0:32], xqr[:, :, 32:64], [xqr]),
                     rpi[:, 0, :], rpi[:, 1, :], 8, 32, rtq)
                def tail(ot=ot, aqr=aqr, xqr=xqr):
                    for h in range(8):
                        kb.op("pe", lambda e: e.transpose(out=ptb[:, h * 128:(h + 1) * 128], in_=aqr[:, h, :],
                                                          identity=c_identb[:]), [aqr, c_identb], [ptb])
                    kb.op("act", lambda e: e.activation(out=aqT[:], in_=ptb[:].rearrange("p (h t) -> p h t", h=8),
                                                        func=AF.Identity), [ptb], [aqT])
                    kb.dma("sp", aqT_d[ot], aqT[:], reads=[aqT])
                    for h in range(8):
                        kb.op("pe", lambda e: e.transpose(out=pti[h // 4][0:64, (h % 4) * 128:(h % 4 + 1) * 128],
                                                          in_=xqr[:, h, :], identity=c_ident[:]), [xqr, c_ident], [pti[h // 4]])
                    for q in range(2):
                        kb.op("act", lambda e: e.activation(out=iqT[:, q * 4:(q + 1) * 4, :],
                                                            in_=pti[q][0:64, :].rearrange("p (h t) -> p h t", h=4),
                                                            func=AF.Identity), [pti[q]], [iqT])
                    kb.dma("sp", iqT_d[ot], iqT[:], reads=[iqT])
                return tail

            q_phase([(O_AQ, 1024, 0), (O_XQ, 512, 1024), (O_XW, 8, 1536)], 1544, consume_q1)

        barrier()
        with ExitStack() as esq:
            t1 = kb.sb(esq, [128, 2048], F32, "t1")
            ogt = kb.sb(esq, [128, D], F32, "ogt")

            def consume_q2(es, ot, pQ, bias):
                for q in range(4):
                    kb.op("dve", lambda e: e.tensor_tensor(out=t1[:, q * 512:(q + 1) * 512], in0=pQ[q][:],
                                                           in1=bias[:, q * 512:(q + 1) * 512], op=ALU.add), [pQ[q], bias], [t1])
                kb.op("act", lambda e: e.activation(out=t1[:], in_=t1[:], func=AF.Sigmoid), [t1], [t1])
                kb.op("dve", lambda e: e.tensor_tensor(out=ogt[:], in0=t1[:, 0:1024], in1=t1[:, 1024:2048], op=ALU.mult),
                      [t1], [ogt])
                kb.dma("sp", og_d[ot * 128:(ot + 1) * 128, :], ogt[:], reads=[ogt])

            q_phase([(O_MO, 1024, 0), (O_GM, 1024, 1024)], 2048, consume_q2)

            def consume_q3(es, ot, pQ, bias):
                for q in range(2):
                    kb.op("dve", lambda e: e.tensor_tensor(out=t1[:, q * 512:(q + 1) * 512], in0=pQ[q][:],
                                                           in1=bias[:, q * 512:(q + 1) * 512], op=ALU.add), [pQ[q], bias], [t1])
                kb.op("act", lambda e: e.activation(out=ogt[:], in_=t1[:, 0:1024], func=AF.Sigmoid), [t1], [ogt])
                kb.dma("sp", ga_d[ot * 128:(ot + 1) * 128, :], ogt[:], reads=[ogt])

            q_phase([(O_GA, 1024, 0)], 1024, consume_q3)
        fence()
        if upto == "Q":
            for nm, src in (("v", v_d), ("ikT", ikT_d), ("og", og_d), ("ga", ga_d)):
                if nm in dbg:
                    kb.dma("sp", dbg[nm], src)
            finish()
            nc.used_inputs = used_inputs
            return nc
        barrier()
        with ExitStack() as es:
            G = kb.sb(es, [64, NCH, 8], F32, "G64")
            for c0 in range(0, NCH, 16):
                c1 = min(NCH, c0 + 16)
                kb.dma("sp", G[:, c0:c1, :], g_d[c0 * 64:c1 * 64, :].rearrange("(c s) n -> s c n", s=64), writes=[G])
            c_tri = kb.sb(es, [64, 64], F32, "tri64")
            kb.dma("sp", c_tri[:], tri64, writes=[c_tri])
            c_trs = kb.sb(es, [64, 8, 128], F32, "trisel")
            kb.dma("sp", c_trs[:], trisel.rearrange("c s m -> s c m"), writes=[c_trs])
            c_cm = kb.sb(es, [64, 16], F32, "cmask")
            kb.dma("sp", c_cm[:], cmask, writes=[c_cm])
            c_ones = kb.sb(es, [64, 128], F32, "ones64")
            kb.op("dve", lambda e: e.memset(c_ones[:], 1.0), [], [c_ones])
            mng = bcast_load(es, m_norm_g[0, :], D, "mng")
            NL = kb.sb(es, [64, NCH, 4], F32, "NL")
            NB = kb.sb(es, [64, NCH, 4], F32, "NB")
            NBL = kb.sb(es, [128, NCH, 4], F32, "NBL")
            DEC = kb.sb(es, [128, NCH, 4], F32, "DEC")
            WG = kb.sb(es, [64, NCH, 4], F32, "WG")
            CF = kb.sb(es, [64, NCH, 4], F32, "CF")
            RF = kb.sb(es, [128, NG, 4], F32, "RF")
            pg = [kb.ps(es, [128, 512], F32, "pgate") for _ in range(2)]
            kb.op("act", lambda e: e.activation(out=NL[:], in_=G[:, :, 4:8], func=AF.Exp, scale=-1.0), [G], [NL])
            kb.op("act", lambda e: e.activation(out=NL[:], in_=NL[:], func=AF.Ln, bias=1.0), [NL], [NL])
            NLf = NL[:].rearrange("s c h -> s (c h)")
            for c0 in range(0, NCH * 4, 512):
                c1 = min(NCH * 4, c0 + 512)
                kb.op("pe", lambda e: e.matmul(pg[0][0:64, 0:c1 - c0], c_tri[:], NLf[:, c0:c1], start=True, stop=True),
                      [c_tri, NL], [pg[0]])
                kb.op("dve", lambda e: e.tensor_copy(out=NB[:].rearrange("s c h -> s (c h)")[:, c0:c1], in_=pg[0][0:64, 0:c1 - c0]),
                      [pg[0]], [NB])
                kb.op("pe", lambda e: e.matmul(pg[1][:, 0:c1 - c0], c_ones[:], NLf[:, c0:c1], start=True, stop=True),
                      [c_ones, NL], [pg[1]])
                kb.op("dve", lambda e: e.tensor_copy(out=NBL[:].rearrange("s c h -> s (c h)")[:, c0:c1], in_=pg[1][:, 0:c1 - c0]),
                      [pg[1]], [NBL])
            kb.op("act", lambda e: e.activation(out=DEC[:], in_=NBL[:], func=AF.Exp, scale=-1.0), [NBL], [DEC])
            kb.op("dve", lambda e: e.tensor_tensor(out=CF[:], in0=G[:, :, 0:4], in1=NB[:], op=ALU.add), [G, NB], [CF])
            kb.op("dve", lambda e: e.tensor_tensor(out=WG[:], in0=CF[:], in1=NBL[0:64], op=ALU.subtract), [CF, NBL], [WG])
            kb.op("act", lambda e: e.activation(out=CF[:], in_=CF[:], func=AF.Exp), [CF], [CF])
            kb.op("act", lambda e: e.activation(out=WG[:], in_=WG[:], func=AF.Exp), [WG], [WG])
            for g in range(NG):
                for cp in range(8):
                    kb.op("pe", lambda e: e.matmul(pg[0][:, 0:4], c_trs[:, cp, :], NL[:, g * 8 + cp, :],
                                                   start=(cp == 0), stop=(cp == 7)), [c_trs, NL], [pg[0]])
                kb.op("act", lambda e: e.activation(out=RF[:, g, :], in_=pg[0][:, 0:4], func=AF.Exp, scale=-1.0), [pg[0]], [RF])
            kTs = [kb.sb(es, [128, 8, 512], F32R, "kTg") for _ in range(2)]
            kts = [kb.sb(es, [64, D], F32, "ktg") for _ in range(2)]
            kws = [kb.sb(es, [64, 4, 256], F32R, "kwg") for _ in range(3)]
            vgs = [kb.sb(es, [64, 4, 258], F32R, "vg") for _ in range(3)]
            qgs = [kb.sb(es, [128, 8, 128], F32R, "qg") for _ in range(2)]
            for vg in vgs:
                kb.op("dve", lambda e: e.memset(_f(vg[:]), 1.0), [], [vg])
            qpad = kb.sb(es, [128, 8, 8, 128], F32R, "qpad")
            spad = kb.sb(es, [64, 4, 8, 128], F32R, "spad")
            kb.op("dve", lambda e: e.memset(_f(qpad[:]), 0.0), [], [qpad])
            kb.op("dve", lambda e: e.memset(_f(spad[:]), 0.0), [], [spad])
            C32 = [kb.sb(es, [128, 2, 258], F32, "C32_%d" % h) for h in range(4)]
            Cr = [kb.sb(es, [128, 2, 258], F32R, "Cr_%d" % h) for h in range(4)]
            for h in range(4):
                kb.op("dve", lambda e: e.memset(C32[h][:], 0.0), [], [C32[h]])
                kb.op("dve", lambda e: e.memset(_f(Cr[h][:]), 0.0), [], [Cr[h]])
            spb = [[Tl(None) for _ in range(8)] for _ in range(4)]
            hm = kb.sb(es, [128, 4, 256], F32, "hm")
            ogts = [kb.sb(es, [128, D], F32, "ogtB") for _ in range(2)]
            zt = kb.sb(es, [128, D], F32, "zt")
            kb.op("dve", lambda e: e.memset(zt[:], 0.0), [], [zt])
            zrows = list(range(0, NEXP * CAP, 128))
            ymt = kb.sb(es, [128, D], F32, "ymt")
            sc = kb.sb(es, [128, 16], F32, "scB")
            stB = (kb.sb(es, [128, 1, 6], F32, "stB"), kb.sb(es, [128, 4], F32, "mvB"))
            pacc = [kb.ps(es, [128, 512], F32, "pacc") for _ in range(4)]
            pU = pg
            pS = kb.ps(es, [128, 512], F32, "pS")
            pS_s = [Tl(None), Tl(None)]
            pS_n = [Tl(None), Tl(None)]
            c_one2 = kb.sb(es, [64, 2], F32R, "one2")
            kb.op("dve", lambda e: e.memset(_f(c_one2[:]), 1.0), [], [c_one2])
            def group_loads(g):
                kT, qg = kTs[g % 2], qgs[g % 2]
                kb.dma("pool", kT[:], _r(kT_d[:, :, g * 512:(g + 1) * 512].rearrange("c p t -> p c t")), writes=[kT])
                kb.dma("pool", qg[:], _r(qTo_d[:, :, g * 128:(g + 1) * 128].rearrange("c p t -> p c t")), writes=[qg])
                kb.dma("sp", ogts[g % 2][:], og_d[g * 128:(g + 1) * 128, :], writes=[ogts[g % 2]])

            for g in range(NG):
                kT, qg, ogt = kTs[g % 2], qgs[g % 2], ogts[g % 2]
                if g == 0:
                    group_loads(0)
                for cp in range(8):
                    kb.op("pool", lambda e: e.tensor_copy(out=qpad[:, :, cp, cp * 16:(cp + 1) * 16], in_=qg[:, :, cp * 16:(cp + 1) * 16]),
                          [qg], [qpad])
                if g + 1 < NG:
                    group_loads(g + 1)
                nz = (len(zrows) + NG - 1) // NG
                for r0 in zrows[g * nz:(g + 1) * nz]:
                    kb.dma("sp", xs_d[r0:r0 + 128, :], zt[:], reads=[zt])
                def chunk_loads(c):
                    kt, kw, vg = kts[c % 2], kws[c % 3], vgs[c % 3]
                    kb.dma("sp", kt[:], ktok_d[c * 64:(c + 1) * 64, :], writes=[kt])
                    kb.dma("pool", vg[:, :, 0:256], _r(v_d[c * 64:(c + 1) * 64, :].rearrange("s (h d) -> s h d", h=4)), writes=[vg])
                    kb.op("pool", lambda e: e.tensor_tensor(out=kw[:], in0=kt[:].rearrange("s (h d) -> s h d", h=4),
                                                            in1=WG[:, c, :].unsqueeze(2).to_broadcast([64, 4, 256]),
                                                            op=ALU.mult), [kt, WG], [kw])

                def emit_S(cp, h):
                    c = g * 8 + cp
                    par = h % 2
                    for half in range(2):
                        kb.op("pe", lambda e: e.matmul(pS[0:64, par * 16:(par + 1) * 16], kT[:, h * 2 + half, cp * 64:(cp + 1) * 64],
                                                       qg[:, h * 2 + half, cp * 16:(cp + 1) * 16], start=(half == 0), stop=(half == 1)),
                              [kT, qg], [pS_s[par]])
                    kb.op("dve", lambda e: e.scalar_tensor_tensor(out=spad[:, h, cp, cp * 16:(cp + 1) * 16],
                                                                  in0=pS[0:64, par * 16:(par + 1) * 16],
                                                                  scalar=CF[:, c, h:h + 1], in1=c_cm[:], op0=ALU.mult, op1=ALU.mult),
                          [pS_s[par], CF, c_cm], [spb[h][cp]])

                steps = [(cp, h) for cp in range(8) for h in range(4)]
                emit_S(*steps[0])
                for si, (cp, h) in enumerate(steps):
                    c = g * 8 + cp
                    kt, kw, vg = kts[c % 2], kws[c % 3], vgs[c % 3]
                    if h == 0:
                        if c == 0:
                            chunk_loads(0)
                        if c + 1 < NCH:
                            chunk_loads(c + 1)
                    if si + 1 < len(steps):
                        emit_S(*steps[si + 1])
                    par = h % 2
                    kb.op("pe", lambda e: e.matmul(pacc[h][:, 0:258], spad[:, h, cp, :], vg[:, h, :],
                                                   start=(cp == 0), stop=False), [spb[h][cp], vg], [pacc[h]])
                    for half in range(2):
                        kb.op("pe", lambda e: e.matmul(pacc[h][:, 0:258], qpad[:, h * 2 + half, cp, :], Cr[h][:, half, :],
                                                       start=False, stop=(cp == 7 and half == 1)), [qpad, Cr[h]], [pacc[h]])
                    for half in range(2):
                        kb.op("pe", lambda e: e.matmul(pU[par][:, half * 256:(half + 1) * 256], kw[:, h, half * 128:(half + 1) * 128],
                                                       vg[:, h, 0:256], start=True, stop=True), [kw, vg], [pU[par]])
                        kb.op("pe", lambda e: e.matmul(pS[:, 64 + par * 4 + half * 2:64 + par * 4 + half * 2 + 2],
                                                       kw[:, h, half * 128:(half + 1) * 128], c_one2[:], start=True, stop=True),
                              [kw, c_one2], [pS_n[par]])
                    kb.op("dve", lambda e: e.scalar_tensor_tensor(out=C32[h][:, :, 0:256], in0=C32[h][:, :, 0:256],
                                                                  scalar=DEC[:, c, h:h + 1],
                                                                  in1=pU[par][:].rearrange("p (a d) -> p a d", a=2),
                                                                  op0=ALU.mult, op1=ALU.add), [C32[h], DEC, pU[par]], [C32[h]])
                    kb.op("dve", lambda e: e.scalar_tensor_tensor(out=C32[h][:, :, 256:258], in0=C32[h][:, :, 256:258],
                                                                  scalar=DEC[:, c, h:h + 1],
                                                                  in1=pS[:, 64 + par * 4:64 + par * 4 + 4].rearrange("p (a d) -> p a d", a=2),
                                                                  op0=ALU.mult, op1=ALU.add), [C32[h], DEC, pS_n[par]], [C32[h]])
                    kb.op("act", lambda e: e.activation(out=Cr[h][:], in_=C32[h][:], func=AF.Identity), [C32[h]], [Cr[h]])
                for h in range(4):
                    kb.op("dve", lambda e: e.tensor_scalar(out=sc[:, 0:1], in0=pacc[h][:, 256:257], scalar1=RF[:, g, h:h + 1],
                                                          scalar2=None, op0=ALU.mult), [pacc[h], RF], [sc])
                    kb.op("dve", lambda e: e.tensor_scalar(out=sc[:, 3:4], in0=sc[:, 0:1], scalar1=-1.0, scalar2=None,
                                                          op0=ALU.mult), [sc], [sc])
                    kb.op("dve", lambda e: e.tensor_tensor(out=sc[:, 0:1], in0=sc[:, 0:1], in1=sc[:, 3:4], op=ALU.max), [sc], [sc])
                    kb.op("dve", lambda e: e.tensor_scalar(out=sc[:, 0:1], in0=sc[:, 0:1], scalar1=1.0, scalar2=None,
                                                          op0=ALU.max), [sc], [sc])
                    kb.op("dve", lambda e: e.reciprocal(out=sc[:, 1:2], in_=sc[:, 0:1]), [sc], [sc])
                    kb.op("dve", lambda e: e.tensor_tensor(out=sc[:, 2:3], in0=sc[:, 1:2], in1=RF[:, g, h:h + 1], op=ALU.mult),
                          [sc, RF], [sc])
                    kb.op("act", lambda e: e.activation(out=hm[:, h, :], in_=pacc[h][:, 0:256], func=AF.Identity, scale=sc[:, 2:3]),
                          [pacc[h], sc], [hm])
                    kb.op("dve", lambda e: e.bn_stats(out=stB[0][:, 0, :], in_=hm[:, h, :]), [hm], [stB[0]])
                    kb.op("dve", lambda e: e.bn_aggr(out=stB[1][:, 0:2], in_=stB[0][:, 0, :]), [stB[0]], [stB[1]])
                    kb.op("dve", lambda e: e.tensor_scalar(out=stB[1][:, 2:3], in0=stB[1][:, 1:2], scalar1=LN_EPS, scalar2=None,
                                                          op0=ALU.add), [stB[1]], [stB[1]])
                    kb.op("act", lambda e: e.activation(out=stB[1][:, 2:3], in_=stB[1][:, 2:3], func=AF.Sqrt), [stB[1]], [stB[1]])
                    kb.op("dve", lambda e: e.reciprocal(out=stB[1][:, 3:4], in_=stB[1][:, 2:3]), [stB[1]], [stB[1]])
                    kb.op("dve", lambda e: e.tensor_scalar(out=ymt[:, h * 256:(h + 1) * 256], in0=hm[:, h, :], scalar1=stB[1][:, 0:1],
                                                          scalar2=stB[1][:, 3:4], op0=ALU.subtract, op1=ALU.mult), [hm, stB[1]], [ymt])
                kb.op("dve", lambda e: e.tensor_tensor(out=ymt[:], in0=ymt[:], in1=mng[:], op=ALU.mult), [ymt, mng], [ymt])
                kb.op("dve", lambda e: e.tensor_tensor(out=ymt[:], in0=ymt[:], in1=ogt[:], op=ALU.mult), [ymt, ogt], [ymt])
                kb.dma("sp", ym_d[g * 128:(g + 1) * 128, :], ymt[:], reads=[ymt])
        fence()
        if upto == "B":
            if "ym" in dbg:
                kb.dma("sp", dbg["ym"], ym_d)
            finish()
            nc.used_inputs = used_inputs
            return nc
        barrier()
        with ExitStack() as es:
            c_adm = kb.sb(es, [128, 512], F32, "admb")
            kb.dma("sp", c_adm[:], admb, writes=[c_adm])
            c_pw = kb.sb(es, [128, NBIS], F32, "pw2")
            kb.dma("sp", c_pw[:], pw2, writes=[c_pw])
            NMAX = T
            score = kb.sb(es, [128, NMAX], F32, "score")
            msk = kb.sb(es, [128, NMAX], BF16, "msk")
            mbT = kb.sb(es, [128, NMAX // 128, 128], BF16, "mbT")
            Rb = [kb.sb(es, [128, 2, 512], F32R, "Rb") for _ in range(4)]
            Dw = kb.sb(es, [128, 8, 128], F32R, "Dw")
            iqs = [kb.sb(es, [64, 8, 128], F32R, "iq") for _ in range(2)]
            aqs = [kb.sb(es, [128, 8, 128], BF16, "aqs") for _ in range(2)]
            iks = [kb.sb(es, [64, 512], F32R, "ik") for _ in range(2)]
            KhT = [kb.sb(es, [128, NMAX], BF16, "KhT") for _ in range(2)]
            Vh = [kb.sb(es, [128, NMAX // 128, 130], BF16, "Vh") for _ in range(2)]
            PT = [kb.sb(es, [128, 4, 128], BF16, "PT") for _ in range(2)]
            bs = kb.sb(es, [128, 8 + NBIS], F32, "bs")
            rs8 = kb.sb(es, [128, 8], F32, "rs8")
            ya = kb.sb(es, [128, 8, 128], F32, "ya")
            gat = kb.sb(es, [128, D], F32, "gat")
            ymt = kb.sb(es, [128, D], F32, "ymtD")
            PS = [kb.ps(es, [128, 512], F32, "PDs") for _ in range(2)]
            PSC = [kb.ps(es, [128, 512], F32, "PDsc") for _ in range(2)]
            LG = [kb.ps(es, [128, 512], F32, "PDlg") for _ in range(2)]
            PO = kb.ps(es, [128, 512], F32, "PDo")
            PO_t = [Tl(None), Tl(None)]
            Pb = kb.ps(es, [128, 1024], BF16, "PDb")
            qscale = float(128 ** -0.5)

            def kv_load(i, h):
                N = 512 * (i + 1)
                Kt, Vt = KhT[h % 2], Vh[h % 2]
                kb.dma("sp", Kt[:, 0:N], akT_d[h, :, 0:N], writes=[Kt])
                kb.dma("sp", Vt[:, 0:N // 128, :], av_d[h, :, 0:N // 128, :], writes=[Vt])

            def stage1(i):
                N = 512 * (i + 1)
                iq = iqs[i % 2]
                th = []

                def t_load():
                    kb.dma("pool", iq[:], _r(iqT_d[i]), writes=[iq])
                    for h in range(8):
                        kb.op("pool", lambda e: e.tensor_scalar(out=Dw[:, h, :], in0=c_ident[:], scalar1=WQ[:, i, h:h + 1], scalar2=None,
                                                               op0=ALU.mult), [c_ident, WQ], [Dw])
                th.append((1.0, t_load))
                units = [(kt, hp) for kt in range(i + 1) for hp in range(4)]

                def emit_S(u):
                    kt, hp = units[u]
                    ik = iks[kt % 2]
                    if hp == 0:
                        kb.dma("pool", ik[:], _r(ikT_d[:, kt * 512:(kt + 1) * 512]), writes=[ik])
                    R_ = Rb[u % 4]
                    for j in range(2):
                        h = hp * 2 + j
                        pS_ = PS[j]
                        kb.op("pe", lambda e: e.matmul(pS_[:], iq[:, h, :], ik[:], start=True, stop=True), [iq, ik], [pS_])
                        if j == 0:
                            kb.op("act", lambda e: e.activation(out=R_[:, j, :], in_=pS_[:], func=AF.Relu), [pS_], [R_])
                        else:
                            kb.op("dve", lambda e: e.tensor_scalar(out=R_[:, j, :], in0=pS_[:], scalar1=0.0, scalar2=None,
                                                                  op0=ALU.max), [pS_], [R_])

                def emit_Sc(u):
                    kt, hp = units[u]
                    psc = PSC[kt % 2]
                    R_ = Rb[u % 4]
                    for j in range(2):
                        h = hp * 2 + j
                        kb.op("pe", lambda e: e.matmul(psc[:], Dw[:, h, :], R_[:, j, :], start=(h == 0), stop=(h == 7)),
                              [Dw, R_], [psc])
                    if hp == 3:
                        kb.op("act", lambda e: e.activation(out=score[:, kt * 512:(kt + 1) * 512], in_=psc[:], func=AF.Identity),
                              [psc], [score])

                th.append((1.0, lambda: emit_S(0)))
                for u in range(len(units)):
                    def t_unit(u=u):
                        if u + 1 < len(units):
                            emit_S(u + 1)
                        emit_Sc(u)
                    th.append((1.3, t_unit))

                def t_prep():
                    kb.op("dve", lambda e: e.tensor_reduce(out=bs[:, 0:1], in_=score[:, 0:N], axis=AX.X, op=ALU.max), [score], [bs])
                    kb.op("dve", lambda e: e.tensor_reduce(out=bs[:, 1:2], in_=score[:, 0:N], axis=AX.X, op=ALU.min), [score], [bs])
                    kb.op("dve", lambda e: e.tensor_scalar(out=bs[:, 2:3], in0=bs[:, 1:2], scalar1=-1.0, scalar2=None, op0=ALU.add), [bs], [bs])
                    kb.op("dve", lambda e: e.tensor_tensor(out=bs[:, 3:4], in0=bs[:, 0:1], in1=bs[:, 2:3], op=ALU.subtract), [bs], [bs])
                    kb.op("dve", lambda e: e.tensor_scalar(out=bs[:, 8:8 + NBIS], in0=c_pw[:], scalar1=bs[:, 3:4], scalar2=None,
                                                          op0=ALU.mult), [bs, c_pw], [bs])
                    kb.op("dve", lambda e: e.tensor_tensor(out=score[:, N - 512:N], in0=score[:, N - 512:N], in1=c_adm[:], op=ALU.add),
                          [score, c_adm], [score])
                th.append((2.0 * N / 960.0 + 1.0, t_prep))

                def t_bis(n):
                    kb.op("dve", lambda e: e.tensor_tensor(out=bs[:, 4:5], in0=bs[:, 2:3], in1=bs[:, 8 + n:9 + n], op=ALU.add), [bs], [bs])
                    kb.op("dve", lambda e: e.tensor_scalar(out=msk[:, 0:N], in0=score[:, 0:N], scalar1=bs[:, 4:5], scalar2=0.0,
                                                          op0=ALU.is_gt, op1=ALU.add, accum_out=bs[:, 5:6]), [score, bs], [msk, bs])
                    kb.op("dve", lambda e: e.tensor_scalar(out=bs[:, 6:7], in0=bs[:, 5:6], scalar1=255.5, scalar2=None, op0=ALU.is_gt),
                          [bs], [bs])
                    kb.op("dve", lambda e: e.scalar_tensor_tensor(out=bs[:, 2:3], in0=bs[:, 8 + n:9 + n], scalar=bs[:, 6:7],
                                                                  in1=bs[:, 2:3], op0=ALU.mult, op1=ALU.add), [bs], [bs])
                for n in range(NBIS):
                    th.append((N / 960.0 + 0.5, lambda n=n: t_bis(n)))

                def t_final():
                    kb.op("dve", lambda e: e.tensor_scalar(out=msk[:, 0:N], in0=score[:, 0:N], scalar1=bs[:, 2:3], scalar2=None,
                                                          op0=ALU.is_gt), [score, bs], [msk])
                th.append((N / 960.0 + 0.2, t_final))
                return th

            def mask_T(i):
                N = 512 * (i + 1)
                for k8 in range(0, N // 128, 8):
                    for kb_ in range(8):
                        kk = k8 + kb_
                        if kk >= N // 128:
                            break
                        kb.op("pe", lambda e: e.transpose(out=Pb[:, kb_ * 128:(kb_ + 1) * 128], in_=msk[:, kk * 128:(kk + 1) * 128],
                                                          identity=c_identb[:]), [msk, c_identb], [Pb])
                    nn = min(8, N // 128 - k8)
                    kb.op("act", lambda e: e.activation(out=mbT[:, k8:k8 + nn, :],
                                                        in_=Pb[:, 0:nn * 128].rearrange("p (k q) -> p k q", k=nn),
                                                        func=AF.Identity, scale=30000.0, bias=-30000.0), [Pb], [mbT])

            def stage2(i):
                N = 512 * (i + 1)
                aq_ = aqs[i % 2]
                th = []

                def t_loads():
                    if i == 0:
                        kb.dma("sp", aqs[0][:], aqT_d[0], writes=[aqs[0]])
                        kv_load(0, 0)
                    if i + 1 < NG:
                        kb.dma("sp", aqs[(i + 1) % 2][:], aqT_d[i + 1], writes=[aqs[(i + 1) % 2]])
                    kb.dma("sp", gat[:], ga_d[i * 128:(i + 1) * 128, :], writes=[gat])
                    kb.dma("sp", ymt[:], ym_d[i * 128:(i + 1) * 128, :], writes=[ymt])
                th.append((0.2, t_loads))

                def emit_lg(h, k4):
                    Kt = KhT[h % 2]
                    lg = LG[k4 % 2]
                    pt = PT[k4 % 2]
                    for j4 in range(4):
                        kk = k4 * 4 + j4
                        kb.op("pe", lambda e: e.matmul(lg[:, j4 * 128:(j4 + 1) * 128], Kt[:, kk * 128:(kk + 1) * 128], aq_[:, h, :],
                                                       start=True, stop=False), [Kt, aq_], [lg])
                        kb.op("pe", lambda e: e.matmul(lg[:, j4 * 128:(j4 + 1) * 128], c_identb[:], mbT[:, kk, :],
                                                       start=False, stop=True), [c_identb, mbT], [lg])
                    kb.op("act", lambda e: e.activation(out=pt[:].rearrange("p k q -> p (k q)"), in_=lg[:], func=AF.Exp, scale=qscale),
                          [lg], [pt])

                def emit_pv(h, k4):
                    Vt = Vh[h % 2]
                    pt = PT[k4 % 2]
                    par = h % 2
                    O = PO[:, par * 256:par * 256 + 130]
                    for j4 in range(4):
                        kk = k4 * 4 + j4
                        kb.op("pe", lambda e: e.matmul(O, pt[:, j4, :], Vt[:, kk, :], start=(kk == 0),
                                                       stop=(kk == N // 128 - 1)), [pt, Vt], [PO_t[par]])
                    if k4 == N // 512 - 1:
                        kb.op("dve", lambda e: e.reciprocal(out=rs8[:, h:h + 1], in_=PO[:, par * 256 + 128:par * 256 + 129]),
                              [PO_t[par]], [rs8])
                        kb.op("dve", lambda e: e.tensor_scalar(out=ya[:, h, :], in0=PO[:, par * 256:par * 256 + 128],
                                                              scalar1=rs8[:, h:h + 1], scalar2=None, op0=ALU.mult),
                              [PO_t[par], rs8], [ya])

                for h in range(8):
                    def t_first(h=h):
                        if h < 7:
                            kv_load(i, h + 1)
                        elif i + 1 < NG:
                            kv_load(i + 1, 0)
                        emit_lg(h, 0)
                    th.append((0.9, t_first))
                    for k4 in range(N // 512):
                        def t_blk(h=h, k4=k4):
                            if k4 + 1 < N // 512:
                                emit_lg(h, k4 + 1)
                            emit_pv(h, k4)
                        th.append((0.75, t_blk))

                def t_merge():
                    kb.op("dve", lambda e: e.tensor_tensor(out=gat[:], in0=gat[:], in1=ya[:].rearrange("p h d -> p (h d)"), op=ALU.mult),
                          [gat, ya], [gat])
                    kb.op("dve", lambda e: e.tensor_tensor(out=gat[:], in0=gat[:], in1=ymt[:], op=ALU.add), [gat, ymt], [gat])
                    kb.dma("sp", mg_d[i * 128:(i + 1) * 128, :], gat[:], reads=[gat])
                th.append((2.0, t_merge))
                return th

            def run_merged(A, Bl):
                ta = sum(w for w, _ in A) or 1.0
                tb = sum(w for w, _ in Bl) or 1.0
                ia = ib = 0
                da = db = 0.0
                while ia < len(A) or ib < len(Bl):
                    if ib >= len(Bl) or (ia < len(A) and da / ta <= db / tb):
                        w, f = A[ia]
                        ia += 1
                        da += w
                    else:
                        w, f = Bl[ib]
                        ib += 1
                        db += w
                    f()

            for _, f in stage1(0):
                f()
            mask_T(0)
            for i in range(NG):
                run_merged(stage2(i), stage1(i + 1) if i + 1 < NG else [])
                if i + 1 < NG:
                    mask_T(i + 1)
        fence()
        if upto == "D":
            if "mg" in dbg:
                kb.dma("sp", dbg["mg"], mg_d)
            finish()
            nc.used_inputs = used_inputs
            return nc
        barrier()
        NSL = NEXP * CAP
        GK = kb.sb(es0, [128, NOT, 4], F32, "GK")
        bc_reg = nc.gpsimd.to_reg(NSL - 1)
        SLI = kb.sb(es0, [128, NOT, 4], I32, "SLI")

        def ln_full(xt, stp, gt, bt, outt):
            mean, rstd = layernorm_stats(None, xt, stp)
            kb.op("dve", lambda e: e.tensor_scalar(out=outt[:], in0=xt[:], scalar1=mean, scalar2=rstd,
                                                  op0=ALU.subtract, op1=ALU.mult), [xt, stp[1]], [outt])
            kb.op("dve", lambda e: e.tensor_tensor(out=outt[:], in0=outt[:], in1=gt[:], op=ALU.mult), [outt, gt], [outt])
            kb.op("dve", lambda e: e.tensor_tensor(out=outt[:], in0=outt[:], in1=bt[:], op=ALU.add), [outt, bt], [outt])

        with ExitStack() as es:
            Wo = kb.sb(es, [128, 8, D], F32R, "Wo")
            for dc in range(8):
                kb.dma("pool", Wo[:, dc, :], w_out[dc * 128:(dc + 1) * 128, :], writes=[Wo])
            g0, b0, g1, b1 = [bcast_load(es, vecs[k, :], D, "lnv%d" % k) for k in range(4)]
            Wr = kb.sb(es, [128, 8, NEXP], F32, "Wr")
            kb.dma("sp", Wr[:], w_router.rearrange("(c p) n -> p c n", p=128), writes=[Wr])
            br = bcast_load(es, b_router[0, :], NEXP, "br")
            c_us = kb.sb(es, [128, 128], F32, "ustri")
            kb.dma("sp", c_us[:], ustri, writes=[c_us])
            c_on = kb.sb(es, [128, 128], F32, "ones128")
            kb.op("dve", lambda e: e.memset(c_on[:], 1.0), [], [c_on])
            c_eo = kb.sb(es, [128, NEXP], F32, "eoff")
            kb.dma("sp", c_eo[:], eoff, writes=[c_eo])
            MK = kb.sb(es, [128, NOT, NEXP], F32, "MK")
            mgt = kb.sb(es, [128, D], F32, "mgt")
            mT = kb.sb(es, [128, 8, 128], F32R, "mT")
            xt = kb.sb(es, [128, D], F32, "xtE")
            h0 = kb.sb(es, [128, D], F32, "h0")
            rs = kb.sb(es, [128, D], F32, "rs")
            h1 = kb.sb(es, [128, D], F32, "h1")
            h1T = kb.sb(es, [128, 8, 128], F32, "h1T")
            stp = (kb.sb(es, [128, 2, 6], F32, "stE"), kb.sb(es, [128, 4], F32, "mvE"))
            lgt = kb.sb(es, [128, NEXP], F32, "lgt")
            ex = kb.sb(es, [128, NEXP], F32, "ex")
            gts = kb.sb(es, [128, NEXP], F32, "gts")
            slf = kb.sb(es, [128, NEXP], F32, "slf")
            m8 = kb.sb(es, [128, 8], F32, "m8")
            sm = kb.sb(es, [128, 8], F32, "sm")
            slk = kb.sb(es, [128, 4], F32, "slk")
            junk = kb.sb(es, [128, NEXP], F32, "junk")
            pt = [kb.ps(es, [128, 512], F32, "ptE") for _ in range(2)]
            po = [kb.ps(es, [128, 512], F32, "poE") for _ in range(2)]
            pr = kb.ps(es, [128, 512], F32, "prE")
            pp = kb.ps(es, [128, 512], F32, "ppE")
            fence()
            for ot in range(NOT):
                kb.dma("sp", mgt[:], mg_d[ot * 128:(ot + 1) * 128, :], writes=[mgt])
                kb.dma("sp", xt[:], x_own[ot * 128:(ot + 1) * 128, :], writes=[xt])
                for half in range(2):
                    for k in range(4):
                        dc = half * 4 + k
                        kb.op("pe", lambda e: e.transpose(out=pt[half][:, k * 128:(k + 1) * 128], in_=mgt[:, dc * 128:(dc + 1) * 128],
                                                          identity=c_ident[:]), [mgt, c_ident], [pt[half]])
                    kb.op("act", lambda e: e.activation(out=mT[:, half * 4:(half + 1) * 4, :],
                                                        in_=pt[half][:].rearrange("p (k t) -> p k t", k=4), func=AF.Identity),
                          [pt[half]], [mT])
                for q in range(2):
                    for dc in range(8):
                        kb.op("pe", lambda e: e.matmul(po[q][:], mT[:, dc, :], Wo[:, dc, q * 512:(q + 1) * 512],
                                                       start=(dc == 0), stop=(dc == 7)), [mT, Wo], [po[q]])
                ln_full(xt, stp, g0, b0, h0)
                for q in range(2):
                    kb.op("dve", lambda e: e.scalar_tensor_tensor(out=rs[:, q * 512:(q + 1) * 512], in0=h0[:, q * 512:(q + 1) * 512],
                                                                  scalar=float(ALPHA), in1=po[q][:], op0=ALU.mult, op1=ALU.add),
                          [h0, po[q]], [rs])
                ln_full(rs, stp, g1, b1, h1)
                kb.dma("sp", h1_d[ot * 128:(ot + 1) * 128, :], h1[:], reads=[h1])
                for half in range(2):
                    for k in range(4):
                        dc = half * 4 + k
                        kb.op("pe", lambda e: e.transpose(out=pt[half][:, k * 128:(k + 1) * 128], in_=h1[:, dc * 128:(dc + 1) * 128],
                                                          identity=c_ident[:]), [h1, c_ident], [pt[half]])
                    kb.op("act", lambda e: e.activation(out=h1T[:, half * 4:(half + 1) * 4, :],
                                                        in_=pt[half][:].rearrange("p (k t) -> p k t", k=4), func=AF.Identity),
                          [pt[half]], [h1T])
                for dc in range(8):
                    kb.op("pe", lambda e: e.matmul(pr[:, 0:NEXP], h1T[:, dc, :], Wr[:, dc, :], start=(dc == 0), stop=(dc == 7)),
                          [h1T, Wr], [pr])
                kb.op("dve", lambda e: e.tensor_tensor(out=lgt[:], in0=pr[:, 0:NEXP], in1=br[:], op=ALU.add), [pr, br], [lgt])
                kb.op("dve", lambda e: e.max(out=m8[:], in_=lgt[:]), [lgt], [m8])
                kb.op("dve", lambda e: e.tensor_scalar(out=MK[:, ot, :], in0=lgt[:], scalar1=m8[:, 3:4], scalar2=None, op0=ALU.is_ge),
                      [lgt, m8], [MK])
                kb.op("dve", lambda e: e.tensor_scalar(out=sm[:, 0:1], in0=m8[:, 0:1], scalar1=-1.0, scalar2=None, op0=ALU.mult), [m8], [sm])
                kb.op("act", lambda e: e.activation(out=ex[:], in_=lgt[:], func=AF.Exp, bias=sm[:, 0:1]), [lgt, sm], [ex])
                kb.op("dve", lambda e: e.scalar_tensor_tensor(out=ex[:], in0=ex[:], scalar=1.0, in1=MK[:, ot, :], op0=ALU.mult, op1=ALU.mult,
                                                              accum_out=sm[:, 1:2]), [ex, MK], [ex, sm])
                kb.op("dve", lambda e: e.reciprocal(out=sm[:, 2:3], in_=sm[:, 1:2]), [sm], [sm])
                kb.op("dve", lambda e: e.tensor_scalar(out=gts[:], in0=ex[:], scalar1=sm[:, 2:3], scalar2=None, op0=ALU.mult), [ex, sm], [gts])
                kb.op("pe", lambda e: e.matmul(pp[:, 0:NEXP], c_us[:], MK[:, ot, :], start=True, stop=(ot == 0)), [c_us, MK], [pp])
                for o2 in range(ot):
                    kb.op("pe", lambda e: e.matmul(pp[:, 0:NEXP], c_on[:], MK[:, o2, :], start=False, stop=(o2 == ot - 1)), [c_on, MK], [pp])
                kb.op("dve", lambda e: e.tensor_scalar(out=junk[:], in0=pp[:, 0:NEXP], scalar1=float(CAP) - 0.5, scalar2=None, op0=ALU.is_lt),
                      [pp], [junk])
                kb.op("dve", lambda e: e.tensor_tensor(out=junk[:], in0=junk[:], in1=MK[:, ot, :], op=ALU.mult), [junk, MK], [junk])
                kb.op("dve", lambda e: e.tensor_tensor(out=slf[:], in0=pp[:, 0:NEXP], in1=c_eo[:], op=ALU.add), [pp, c_eo], [slf])
                kb.op("dve", lambda e: e.tensor_scalar(out=slf[:], in0=slf[:], scalar1=-1.0e6, scalar2=None, op0=ALU.add), [slf], [slf])
                kb.op("dve", lambda e: e.tensor_tensor(out=slf[:], in0=slf[:], in1=junk[:], op=ALU.mult), [slf, junk], [slf])
                kb.op("dve", lambda e: e.tensor_scalar(out=slf[:], in0=slf[:], scalar1=1.0e6, scalar2=None, op0=ALU.add), [slf], [slf])
                for k in range(4):
                    kb.op("dve", lambda e: e.scalar_tensor_tensor(out=junk[:], in0=lgt[:], scalar=m8[:, k:k + 1], in1=slf[:],
                                                                  op0=ALU.is_equal, op1=ALU.mult, accum_out=slk[:, k:k + 1]),
                          [lgt, m8, slf], [junk, slk])
                    kb.op("dve", lambda e: e.scalar_tensor_tensor(out=junk[:], in0=lgt[:], scalar=m8[:, k:k + 1], in1=gts[:],
                                                                  op0=ALU.is_equal, op1=ALU.mult, accum_out=GK[:, ot, k:k + 1]),
                          [lgt, m8, gts], [junk, GK])
                kb.op("dve", lambda e: e.tensor_copy(out=SLI[:, ot, :], in_=slk[:]), [slk], [SLI])
                for k in range(4):
                    kb.dma("pool", None, None, reads=[h1, SLI], indirect=lambda g_: g_.indirect_dma_start(
                        out=xs_d, out_offset=bass.IndirectOffsetOnAxis(ap=SLI[:, ot, k:k + 1], axis=0), in_=h1[:], in_offset=None,
                        bounds_check=bc_reg, oob_is_err=False))
        fence()
        if upto == "E":
            if "h1" in dbg:
                kb.dma("sp", dbg["h1"], h1_d)
            finish()
            nc.used_inputs = used_inputs
            return nc
        barrier()
        with ExitStack() as es:
            NST = CAP // 128
            WS = [kb.sb(es, [128, 8, 512], F32, "WS%d" % k) for k in range(3)]
            WB = [kb.sb(es, [128, 8, 512], F32R, "WB%d" % k) for k in range(4)]
            xsb = [kb.sb(es, [128, NST, D], F32, "xsb") for _ in range(2)]
            xT = kb.sb(es, [128, 8, CAP], F32R, "xTe")
            actT = kb.sb(es, [128, 8, CAP], F32R, "actT")
            bgu = kb.sb(es, [128, NEXP, 16], F32, "bgu")
            kb.dma("sp", bgu[:], bgu_col, writes=[bgu])
            bd = [kb.sb(es, [128, D], F32, "bd") for _ in range(2)]
            gs = [kb.sb(es, [128, CAP], F32, "gs") for _ in range(2)]
            sg = [kb.sb(es, [128, CAP], F32, "sg") for _ in range(2)]
            us = [kb.sb(es, [128, CAP], F32, "us") for _ in range(2)]
            yt = [kb.sb(es, [128, D], F32, "yt") for _ in range(2)]
            pg_ = [kb.ps(es, [128, 512], F32, "pgF") for _ in range(2)]
            pu_ = [kb.ps(es, [128, 512], F32, "puF") for _ in range(2)]
            py_ = [kb.ps(es, [128, 512], F32, "pyF") for _ in range(2)]
            ptx = [kb.ps(es, [128, 512], F32, "ptxF") for _ in range(2)]
            wcnt = [0]

            def wload(src_ap):
                k = wcnt[0]
                wcnt[0] += 1
                st_, t = WS[k % 3], WB[k % 4]
                kb.dma("sp", st_[:], _f(src_ap).rearrange("(c p) n -> p c n", p=128), writes=[st_])
                if k % 2 == 0:
                    kb.op("act", lambda e: e.activation(out=t[:], in_=st_[:], func=AF.Identity), [st_], [t])
                else:
                    kb.op("dve", lambda e: e.tensor_copy(out=t[:], in_=st_[:]), [st_], [t])
                return t

            def xs_load(ex_):
                kb.dma("sp", xsb[ex_ % 2][:], xs_d[ex_ * CAP:(ex_ + 1) * CAP, :].rearrange("(k p) n -> p k n", p=128), writes=[xsb[ex_ % 2]])
                kb.dma("sp", bd[ex_ % 2][:], b_down[ex_, :].partition_broadcast(128), writes=[bd[ex_ % 2]])

            xs_load(0)
            for ex_ in range(NEXP):
                xs = xsb[ex_ % 2]
                bdt = bd[ex_ % 2]
                for dc in range(8):
                    px = ptx[dc % 2]
                    for st in range(NST):
                        kb.op("pe", lambda e: e.transpose(out=px[:, st * 128:(st + 1) * 128], in_=xs[:, st, dc * 128:(dc + 1) * 128],
                                                          identity=c_ident[:]), [xs, c_ident], [px])
                    if dc % 2 == 0:
                        kb.op("act", lambda e: e.activation(out=xT[:, dc, :], in_=px[:, 0:CAP], func=AF.Identity), [px], [xT])
                    else:
                        kb.op("dve", lambda e: e.tensor_copy(out=xT[:, dc, :], in_=px[:, 0:CAP]), [px], [xT])
                if ex_ + 1 < NEXP:
                    xs_load(ex_ + 1)
                for hf in range(2):
                    wg_ = wload(w_gate[ex_, :, hf * 512:(hf + 1) * 512])
                    wu_ = wload(w_up[ex_, :, hf * 512:(hf + 1) * 512])
                    for f4 in range(4):
                        fc = hf * 4 + f4
                        par = fc % 2
                        for dc in range(8):
                            kb.op("pe", lambda e: e.matmul(pg_[par][:, 0:CAP], wg_[:, dc, f4 * 128:(f4 + 1) * 128], xT[:, dc, :],
                                                           start=(dc == 0), stop=(dc == 7)), [wg_, xT], [pg_[par]])
                        for dc in range(8):
                            kb.op("pe", lambda e: e.matmul(pu_[par][:, 0:CAP], wu_[:, dc, f4 * 128:(f4 + 1) * 128], xT[:, dc, :],
                                                           start=(dc == 0), stop=(dc == 7)), [wu_, xT], [pu_[par]])
                        kb.op("dve", lambda e: e.tensor_scalar(out=gs[par][:], in0=pg_[par][:, 0:CAP], scalar1=bgu[:, ex_, fc:fc + 1],
                                                              scalar2=7.0, op0=ALU.add, op1=ALU.min), [pg_[par], bgu], [gs[par]])
                        kb.op("act", lambda e: e.activation(out=sg[par][:], in_=gs[par][:], func=AF.Sigmoid, scale=1.702),
                              [gs[par]], [sg[par]])
                        kb.op("dve", lambda e: e.tensor_scalar(out=us[par][:], in0=pu_[par][:, 0:CAP], scalar1=bgu[:, ex_, 8 + fc:9 + fc],
                                                              scalar2=7.0, op0=ALU.add, op1=ALU.min), [pu_[par], bgu], [us[par]])
                        kb.op("dve", lambda e: e.tensor_scalar(out=us[par][:], in0=us[par][:], scalar1=-7.0, scalar2=1.0,
                                                              op0=ALU.max, op1=ALU.add), [us[par]], [us[par]])
                        kb.op("pool", lambda e: e.tensor_tensor(out=gs[par][:], in0=gs[par][:], in1=sg[par][:], op=ALU.mult),
                              [gs[par], sg[par]], [gs[par]])
                        kb.op("pool", lambda e: e.tensor_tensor(out=actT[:, fc, :], in0=gs[par][:], in1=us[par][:], op=ALU.mult),
                              [gs[par], us[par]], [actT])
                wd = [wload(w_down[ex_, :, hf * 512:(hf + 1) * 512]) for hf in range(2)]
                for st in range(NST):
                    y = yt[st % 2]
                    for hf in range(2):
                        for fc in range(8):
                            kb.op("pe", lambda e: e.matmul(py_[hf][:], actT[:, fc, st * 128:(st + 1) * 128], wd[hf][:, fc, :],
                                                           start=(fc == 0), stop=(fc == 7)), [actT, wd[hf]], [py_[hf]])
                        kb.op("dve", lambda e: e.tensor_tensor(out=y[:, hf * 512:(hf + 1) * 512], in0=py_[hf][:],
                                                               in1=bdt[:, hf * 512:(hf + 1) * 512], op=ALU.add), [py_[hf], bdt], [y])
                    kb.dma("sp", ys_d[ex_ * CAP + st * 128: ex_ * CAP + (st + 1) * 128, :], y[:], reads=[y])
        fence()
        barrier()
        with ExitStack() as es:
            g2, b2 = [bcast_load(es, vecs[k, :], D, "lnv%d" % k) for k in (4, 5)]
            yks = [[kb.sb(es, [128, D], F32, "yk%d" % k) for k in range(4)] for _ in range(2)]
            h1ts = [kb.sb(es, [128, D], F32, "h1t") for _ in range(2)]
            acc = kb.sb(es, [128, D], F32, "accG")
            ot_ = kb.sb(es, [128, D], F32, "outG")
            stp = (kb.sb(es, [128, 2, 6], F32, "stG"), kb.sb(es, [128, 4], F32, "mvG"))
            for ot in range(NOT):
                yk, h1t = yks[ot % 2], h1ts[ot % 2]
                kb.dma("pool", h1t[:], h1_d[ot * 128:(ot + 1) * 128, :], writes=[h1t])
                for k in range(4):
                    kb.op("pool", lambda e: e.memset(yk[k][:], 0.0), [], [yk[k]])
                    kb.dma("pool", None, None, reads=[SLI], writes=[yk[k]], indirect=lambda g_: g_.indirect_dma_start(
                        out=yk[k][:], out_offset=None, in_=ys_d, in_offset=bass.IndirectOffsetOnAxis(ap=SLI[:, ot, k:k + 1], axis=0),
                        bounds_check=bc_reg, oob_is_err=False))
                kb.op("dve", lambda e: e.tensor_scalar(out=acc[:], in0=h1t[:], scalar1=float(ALPHA), scalar2=None, op0=ALU.mult),
                      [h1t], [acc])
                for k in range(4):
                    kb.op("dve", lambda e: e.scalar_tensor_tensor(out=acc[:], in0=yk[k][:], scalar=GK[:, ot, k:k + 1], in1=acc[:],
                                                                  op0=ALU.mult, op1=ALU.add), [yk[k], GK, acc], [acc])
                ln_full(acc, stp, g2, b2, ot_)
                kb.dma("sp", out[ot * 128:(ot + 1) * 128, :], ot_[:], reads=[ot_])
        finish()
    nc.used_inputs = used_inputs
    return nc


def _own_idx(T, j):
    c = np.arange(T // 64)[:, None] * 64 + 16 * j + np.arange(16)[None, :]
    return c.reshape(-1)


def _col(v, n):
    return np.ascontiguousarray(v.reshape(n, 128).T)


def prep(inputs, T):
    f = np.float32
    x = np.asarray(inputs["x"], f)
    B = x.shape[0]
    w_in = np.ascontiguousarray(np.asarray(inputs["w_in"], f)[0])
    b_in = np.asarray(inputs["b_in"], f)[0]
    conv_w = np.asarray(inputs["conv_w"], f)[0]
    conv_b = np.asarray(inputs["conv_b"], f)[0]
    cols = np.zeros((128, 128), f)
    cols[:, 0:8] = _col(np.asarray(inputs["ln_in_g"], f), 8)
    cols[:, 8:16] = _col(np.asarray(inputs["ln_in_b"], f), 8)
    cols[:, 16:32] = _col(b_in[0:2048], 16)
    cols[:, 32:48] = _col(conv_b, 16)
    conv_wc = np.ascontiguousarray(conv_w.reshape(4, 16, 128).transpose(2, 1, 0))
    pos = np.arange(T, dtype=np.float64)

    def rope_tab(half):
        inv = 10000.0 ** (-np.arange(half, dtype=np.float64) / half)
        ang = (pos[:, None].astype(f) * inv[None, :].astype(f)).astype(f)
        return np.stack([np.cos(ang), np.sin(ang)], axis=1).astype(f)

    ra, ri = rope_tab(64), rope_tab(32)
    p = np.arange(128)
    p64 = np.arange(64)
    tri64 = (p64[:, None] <= p64[None, :]).astype(f)
    ident = np.eye(128, dtype=f)
    pw2 = np.tile((2.0 ** -(np.arange(NBIS) + 1.0)).astype(f)[None, :], (128, 1))
    ustri = (p[:, None] < p[None, :]).astype(f)
    eoff = np.tile((np.arange(NEXP) * CAP).astype(f)[None, :], (128, 1))
    vecs = np.stack([np.asarray(inputs[k], f).reshape(-1) for k in
                     ("ln_in_g", "ln_in_b", "ln1_g", "ln1_b", "ln2_g", "ln2_b")])
    bg = np.asarray(inputs["b_gate"], f)[0]
    bu = np.asarray(inputs["b_up"], f)[0]
    bgu = np.zeros((128, NEXP, 16), f)
    for e_ in range(NEXP):
        bgu[:, e_, 0:8] = _col(bg[e_], 8)
        bgu[:, e_, 8:16] = _col(bu[e_], 8)
    m = np.arange(128)[:, None] // 16
    s = np.arange(512)[None, :] // 64
    admb = np.where(s <= m, 0.0, -3.0e38).astype(f)
    shared = dict(w_in=w_in, b_in=b_in[None, :], cols=cols, conv_wc=conv_wc, rope_a=ra, rope_i=ri, tri64=tri64,
                  ident=ident, pw2=pw2, admb=admb, m_norm_g=np.asarray(inputs["m_norm_g"], f).reshape(1, D),
                  w_out=np.ascontiguousarray(np.asarray(inputs["w_out"], f)[0]), vecs=vecs,
                  w_router=np.ascontiguousarray(np.asarray(inputs["w_router"], f)[0]),
                  b_router=np.asarray(inputs["b_router"], f).reshape(1, NEXP),
                  w_gate=np.ascontiguousarray(np.asarray(inputs["w_gate"], f)[0]),
                  w_up=np.ascontiguousarray(np.asarray(inputs["w_up"], f)[0]),
                  w_down=np.ascontiguousarray(np.asarray(inputs["w_down"], f)[0]),
                  bgu_col=bgu, b_down=np.ascontiguousarray(np.asarray(inputs["b_down"], f)[0]),
                  eoff=eoff, ustri=ustri)
    maps = []
    for c in range(8):
        b, j = (c // 4) % B, c % 4
        oi = _own_idx(T, j)
        sel = np.zeros((128, 32), f)
        trisel = np.zeros((8, 64, 128), f)
        for ch in range(2):
            for r in range(16):
                sel[ch * 64 + 16 * j + r, ch * 16 + r] = 1.0 / 16.0
        for cp in range(8):
            for r in range(16):
                trisel[cp, 0:16 * j + r + 1, cp * 16 + r] = 1.0
        cmask = (np.arange(64)[:, None] <= (16 * j + np.arange(16))[None, :]).astype(f)
        d = dict(shared)
        d.update(x_all=np.ascontiguousarray(x[b, :T]), x_own=np.ascontiguousarray(x[b, oi]),
                 rope_ao=np.ascontiguousarray(ra[oi]), rope_io=np.ascontiguousarray(ri[oi]),
                 sel=sel, trisel=trisel, cmask=cmask)
        maps.append(d)
    return maps


def assemble(results, T, B):
    out = np.zeros((B, T, D), np.float32)
    for c in range(8):
        b, j = c // 4, c % 4
        if b < B:
            out[b, _own_idx(T, j)] = results[c]["out"]
    return out


_NC_CACHE = {}


def kernel(**inputs):
    T = inputs["x"].shape[1]
    B = inputs["x"].shape[0]
    if T not in _NC_CACHE:
        _NC_CACHE[T] = build(T)
    nc = _NC_CACHE[T]
    maps = [{k: m[k] for k in nc.used_inputs} for m in prep(inputs, T)]
    res = run_bass_kernel_spmd(nc, maps, core_ids=list(range(8)))
    return assemble(res.results, T, B)
```
